# Optimizing a Trainium2 kernel written in Bass

```python
import math
import jax
import jax.numpy as jnp
from jax import lax
import numpy as np

D_MODEL = 1024
BATCH = 2
SEQ = 8192
DEPTH = 2

GRID_W = 64
CTX_LEN = 256
N_ADA = 6
N_BRANCH = 3
BRANCH_WIDTH = D_MODEL
POOL_WIDTH = BRANCH_WIDTH
POOL_GROUPS = 4
POOL_WINDOWS = (2, 4, 8, 16)
FOURIER_WIDTH = BRANCH_WIDTH
FOURIER_GROUPS = 4
SSM_INNER = BRANCH_WIDTH
SSM_HEAD_DIM = 64
SSM_HEADS = SSM_INNER // SSM_HEAD_DIM
SSM_GROUPS = 4
SSM_STATE = 128
SSM_CONV = 5
SSM_CHUNK = 128
CONV_DIM = SSM_INNER + 2 * SSM_GROUPS * SSM_STATE
OFF_FOURIER = POOL_WIDTH
OFF_Z = OFF_FOURIER + FOURIER_WIDTH
OFF_XBC = OFF_Z + SSM_INNER
OFF_DT = OFF_XBC + CONV_DIM
OFF_GATE = OFF_DT + 2 * SSM_HEADS
IN_COLS = OFF_GATE + N_BRANCH * D_MODEL
N_EXPERTS = 32
TOP_K = 4
D_FF = D_MODEL
SWIGLU_ALPHA = 1.702
SWIGLU_LIMIT = 7.0
MOE_BLOCK = 256
RMS_EPS = 1e-6

kernel_name = 'hybrid_pool_fourier_ssd_moe_dit'


def rms_norm(x, w):
    xf = x.astype(jnp.float32)
    y = xf * lax.rsqrt(jnp.mean(xf * xf, axis=-1, keepdims=True) + RMS_EPS)
    return y * w.astype(jnp.float32)


def modulate(h, shift, scale):
    return h * (1.0 + scale) + shift


def _window_bounds(n, w):
    t = jnp.arange(n)
    lo = jnp.clip(t - w // 2, 0, n)
    hi = jnp.clip(t + w - w // 2, 0, n)
    return lo, hi


def _pool1d(u, w):
    n = u.shape[1]
    s = jnp.pad(jnp.cumsum(u, axis=1), ((0, 0), (1, 0), (0, 0)))
    lo, hi = _window_bounds(n, w)
    return (s[:, hi] - s[:, lo]) / (hi - lo).astype(u.dtype)[None, :, None]


def _pool2d(u, w):
    rows, cols = u.shape[1], u.shape[2]
    s = jnp.pad(jnp.cumsum(jnp.cumsum(u, axis=1), axis=2), ((0, 0), (1, 0), (1, 0), (0, 0)))
    r0, r1 = _window_bounds(rows, w)
    c0, c1 = _window_bounds(cols, w)
    tot = (s[:, r1][:, :, c1] - s[:, r0][:, :, c1]
           - s[:, r1][:, :, c0] + s[:, r0][:, :, c0])
    cnt = ((r1 - r0)[:, None] * (c1 - c0)[None, :]).astype(u.dtype)
    return tot / cnt[None, :, :, None]


def pool_mixer(u, pool_w, pool_scale, grid):
    b, n, _ = u.shape
    gw = POOL_WIDTH // POOL_GROUPS
    ug = u.reshape(b, n, POOL_GROUPS, gw)
    outs = []
    for g, w in enumerate(POOL_WINDOWS):
        v = ug[:, :, g]
        if grid:
            rows = n // GRID_W
            p = _pool2d(v.reshape(b, rows, GRID_W, gw), w).reshape(b, n, gw)
        else:
            p = _pool1d(v, w)
        outs.append(p - v)
    d = jnp.stack(outs, axis=2)
    y = jnp.einsum('blgc,gcd->blgd', d, pool_w).reshape(b, n, POOL_WIDTH)
    return y * pool_scale


def fourier_mixer(u):
    b, n, _ = u.shape
    ug = u.astype(jnp.float32).reshape(b, n, FOURIER_GROUPS, FOURIER_WIDTH // FOURIER_GROUPS)
    f = jnp.fft.fft2(ug, axes=(1, 3), norm='ortho').real
    return f.reshape(b, n, FOURIER_WIDTH).astype(jnp.float32)


def depthwise_conv(u, w, bias):
    k = w.shape[0]
    out = lax.conv_general_dilated(
        u, w.astype(jnp.float32)[:, None, :], window_strides=(1,),
        padding=[(k // 2, k - 1 - k // 2)],
        dimension_numbers=('NWC', 'WIO', 'NWC'), feature_group_count=u.shape[-1])
    return out + bias.astype(jnp.float32)


def _segsum(a):
    t = a.shape[-1]
    cs = jnp.cumsum(a, axis=-1)
    diff = cs[..., :, None] - cs[..., None, :]
    return jnp.where(jnp.tril(jnp.ones((t, t), dtype=bool)), diff, -jnp.inf)


def ssd_scan(x, dt, a, bm, cm, init_state):
    b, n, h, p = x.shape
    q = SSM_CHUNK
    nc = n // q
    xd = (x * dt[..., None]).reshape(b, nc, q, h, p)
    ad = (dt * a).reshape(b, nc, q, h).transpose(0, 3, 1, 2)
    bc = bm.reshape(b, nc, q, h, -1)
    cc = cm.reshape(b, nc, q, h, -1)
    a_cum = jnp.cumsum(ad, axis=-1)
    lmat = jnp.exp(_segsum(ad))
    cb = jnp.einsum('bclhn,bcshn->bhcls', cc, bc)
    y_diag = jnp.einsum('bhcls,bcshp->bclhp', cb * lmat, xd)
    decay_states = jnp.exp(a_cum[..., -1:] - a_cum).transpose(0, 2, 3, 1)
    chunk_states = jnp.einsum('bclhn,bclhp->bchpn', bc, xd * decay_states[..., None])
    chunk_decay = jnp.exp(a_cum[..., -1])

    def step(s, inp):
        st, dec = inp
        return s * dec[..., None, None] + st, s

    final, starts = lax.scan(step, init_state,
                             (chunk_states.transpose(1, 0, 2, 3, 4), chunk_decay.transpose(2, 0, 1)))
    starts = starts.transpose(1, 0, 2, 3, 4)
    y_off = (jnp.einsum('bclhn,bchpn->bclhp', cc, starts)
             * jnp.exp(a_cum).transpose(0, 2, 3, 1)[..., None])
    return (y_diag + y_off).reshape(b, n, h, p), final


def _bidir_ssd(xs, bm, cm, dt, a, init_f, init_b):
    y_f, fin_f = ssd_scan(xs, dt[:, :, 0], a[0], bm, cm, init_f)
    rev = lambda t: jnp.flip(t, axis=1)
    y_b, fin_b = ssd_scan(rev(xs), rev(dt[:, :, 1]), a[1], rev(bm), rev(cm), init_b)
    return y_f + rev(y_b), fin_f, fin_b


def _ssm_inputs(xbc, dt_raw, conv_w, conv_b, dt_bias):
    b, n, _ = xbc.shape
    v = jax.nn.silu(depthwise_conv(xbc, conv_w, conv_b))
    gn = SSM_GROUPS * SSM_STATE
    rep = SSM_HEADS // SSM_GROUPS
    xs = v[..., :SSM_INNER].reshape(b, n, SSM_HEADS, SSM_HEAD_DIM)
    bm = jnp.repeat(v[..., SSM_INNER:SSM_INNER + gn].reshape(b, n, SSM_GROUPS, SSM_STATE), rep, axis=2)
    cm = jnp.repeat(v[..., SSM_INNER + gn:].reshape(b, n, SSM_GROUPS, SSM_STATE), rep, axis=2)
    dt = jax.nn.softplus(dt_raw.reshape(b, n, 2, SSM_HEADS) + dt_bias.astype(jnp.float32))
    return xs, bm, cm, dt


def _ssm_out(y, xs, z, d_skip, norm_w):
    b, n = y.shape[:2]
    y = (y + d_skip.astype(jnp.float32)[:, None] * xs).reshape(b, n, SSM_INNER)
    return rms_norm(y * jax.nn.silu(z), norm_w)


def merge_branches(ya, yb, yc, gate_raw, w_branch, w_out):
    b, n, _ = ya.shape
    ys = jnp.stack([ya, yb, yc], axis=2)
    proj = jnp.einsum('blkw,kwd->blkd', ys, w_branch)
    g = jax.nn.sigmoid(gate_raw.reshape(b, n, N_BRANCH, D_MODEL))
    return jnp.sum(g * proj, axis=2) @ w_out


def token_mixer(hl, hc, w_in, pool_w, pool_scale, conv_w, conv_b, dt_bias, a_log, d_skip,
                ssm_norm_w, w_branch, w_out, need_ctx):
    ul = (hl @ w_in).astype(jnp.float32)
    uc = (hc @ w_in).astype(jnp.float32)
    a = -jnp.exp(a_log.astype(jnp.float32))
    b = hl.shape[0]
    xs_c, bm_c, cm_c, dt_c = _ssm_inputs(uc[..., OFF_XBC:OFF_DT], uc[..., OFF_DT:OFF_GATE],
                                         conv_w, conv_b, dt_bias)
    xs_l, bm_l, cm_l, dt_l = _ssm_inputs(ul[..., OFF_XBC:OFF_DT], ul[..., OFF_DT:OFF_GATE],
                                         conv_w, conv_b, dt_bias)
    zero = jnp.zeros((b, SSM_HEADS, SSM_HEAD_DIM, SSM_STATE), jnp.float32)
    y_c, st_f, st_b = _bidir_ssd(xs_c, bm_c, cm_c, dt_c, a, zero, zero)
    y_l, _, _ = _bidir_ssd(xs_l, bm_l, cm_l, dt_l, a, st_f, st_b)

    def branches(u, y_ssm, xs, grid):
        ya = pool_mixer(u[..., :OFF_FOURIER], pool_w, pool_scale, grid)
        yb = fourier_mixer(u[..., OFF_FOURIER:OFF_Z])
        yc = _ssm_out(y_ssm, xs, u[..., OFF_Z:OFF_XBC], d_skip, ssm_norm_w)
        return merge_branches(ya, yb, yc, u[..., OFF_GATE:], w_branch, w_out)

    out_l = branches(ul, y_l, xs_l, True)
    out_c = branches(uc, y_c, xs_c, False) if need_ctx else None
    return out_l, out_c


def moe(h, router_w, router_b, w1, b1, w2, b2):
    t, d = h.shape
    logits = (h @ router_w + router_b).astype(jnp.float32)
    top_val, top_idx = lax.top_k(logits, TOP_K)
    wts = jax.nn.softmax(top_val, axis=-1)
    flat_e = top_idx.reshape(-1)
    flat_tok = jnp.repeat(jnp.arange(t, dtype=jnp.int32), TOP_K)
    flat_w = wts.reshape(-1)
    order = jnp.argsort(flat_e)
    se, stok, sw = flat_e[order], flat_tok[order], flat_w[order]
    counts = jnp.zeros((N_EXPERTS,), jnp.int32).at[flat_e].add(1)
    starts = jnp.cumsum(counts) - counts
    padded = (counts + MOE_BLOCK - 1) // MOE_BLOCK * MOE_BLOCK
    pends = jnp.cumsum(padded)
    pstarts = pends - padded
    dest = pstarts[se] + jnp.arange(t * TOP_K, dtype=jnp.int32) - starts[se]
    n_blocks = -(-(t * TOP_K) // MOE_BLOCK) + N_EXPERTS
    n_slots = n_blocks * MOE_BLOCK
    slot_tok = jnp.full((n_slots,), t, jnp.int32).at[dest].set(stok)
    slot_w = jnp.zeros((n_slots,), jnp.float32).at[dest].set(sw)
    block_e = jnp.clip(jnp.searchsorted(pends, jnp.arange(n_blocks) * MOE_BLOCK, side='right'),
                       0, N_EXPERTS - 1)
    hp = jnp.concatenate([h, jnp.zeros((1, d), h.dtype)], axis=0)
    xs = hp[slot_tok].reshape(n_blocks, MOE_BLOCK, d)

    def expert_block(args):
        xb, e = args
        gu = xb @ w1[e] + b1[e]
        gate = jnp.minimum(gu[:, :D_FF], SWIGLU_LIMIT)
        lin = jnp.clip(gu[:, D_FF:], -SWIGLU_LIMIT, SWIGLU_LIMIT)
        act = gate * jax.nn.sigmoid(SWIGLU_ALPHA * gate) * (lin + 1.0)
        return (act @ w2[e] + b2[e]).astype(jnp.float32)

    ys = lax.map(expert_block, (xs, block_e)).reshape(n_slots, d)
    out = jnp.zeros((t + 1, d), jnp.float32).at[slot_tok].add(ys * slot_w[:, None])
    return out[:t]


def setup_inputs(seed: int = 0) -> dict:
    key = jax.random.key(seed)
    ks = iter(jax.random.split(key, 32))
    nrm = lambda shape, scale: scale * jax.random.normal(next(ks), shape, jnp.float32)
    L, D = DEPTH, D_MODEL
    pg = POOL_WIDTH // POOL_GROUPS
    x = nrm((BATCH, SEQ, D), 1.0)
    c = nrm((BATCH, D), 1.0)
    ctx = nrm((BATCH, CTX_LEN, D), 1.0)
    c_ctx = nrm((D,), 1.0)
    w_ada = nrm((L, D, N_ADA * D), 0.5 * D ** -0.5)
    b_ada = nrm((L, N_ADA * D), 0.02)
    norm1_w = 1.0 + nrm((L, D), 0.02)
    norm2_w = 1.0 + nrm((L, D), 0.02)
    w_in = nrm((L, D, IN_COLS), D ** -0.5)
    pool_w = nrm((L, POOL_GROUPS, pg, pg), pg ** -0.5)
    pool_scale = 1.0 + nrm((L, POOL_WIDTH), 0.1)
    conv_w = nrm((L, SSM_CONV, CONV_DIM), SSM_CONV ** -0.5)
    conv_b = nrm((L, CONV_DIM), 0.02)
    dt0 = jnp.exp(jax.random.uniform(next(ks), (L, 2, SSM_HEADS), jnp.float32,
                                     minval=math.log(1e-3), maxval=math.log(1e-1)))
    dt_bias = dt0 + jnp.log(-jnp.expm1(-dt0))
    a_log = jnp.log(jax.random.uniform(next(ks), (L, 2, SSM_HEADS), jnp.float32, minval=1.0, maxval=16.0))
    d_skip = 1.0 + nrm((L, SSM_HEADS), 0.1)
    ssm_norm_w = 1.0 + nrm((L, SSM_INNER), 0.02)
    w_branch = nrm((L, N_BRANCH, BRANCH_WIDTH, D), BRANCH_WIDTH ** -0.5)
    w_out = nrm((L, D, D), D ** -0.5)
    router_w = nrm((L, D, N_EXPERTS), D ** -0.5)
    router_b = nrm((L, N_EXPERTS), 0.01)
    moe_w1 = nrm((L, N_EXPERTS, D, 2 * D_FF), D ** -0.5)
    moe_b1 = nrm((L, N_EXPERTS, 2 * D_FF), 0.02)
    moe_w2 = nrm((L, N_EXPERTS, D_FF, D), D_FF ** -0.5)
    moe_b2 = nrm((L, N_EXPERTS, D), 0.02)
    final_norm_w = 1.0 + nrm((D,), 0.02)
    return {'x': x, 'c': c, 'ctx': ctx, 'c_ctx': c_ctx, 'w_ada': w_ada, 'b_ada': b_ada,
            'norm1_w': norm1_w, 'norm2_w': norm2_w, 'w_in': w_in, 'pool_w': pool_w,
            'pool_scale': pool_scale, 'conv_w': conv_w, 'conv_b': conv_b, 'dt_bias': dt_bias,
            'a_log': a_log, 'd_skip': d_skip, 'ssm_norm_w': ssm_norm_w, 'w_branch': w_branch,
            'w_out': w_out, 'router_w': router_w, 'router_b': router_b, 'moe_w1': moe_w1,
            'moe_b1': moe_b1, 'moe_w2': moe_w2, 'moe_b2': moe_b2, 'final_norm_w': final_norm_w}


def reference(x, c, ctx, c_ctx, w_ada, b_ada, norm1_w, norm2_w, w_in, pool_w, pool_scale,
              conv_w, conv_b, dt_bias, a_log, d_skip, ssm_norm_w, w_branch, w_out,
              router_w, router_b, moe_w1, moe_b1, moe_w2, moe_b2, final_norm_w):
    b, n, d = x.shape
    lat, cx = x, ctx
    for i in range(DEPTH):
        last = i == DEPTH - 1
        mod_l = (jax.nn.silu(c.astype(jnp.float32)) @ w_ada[i] + b_ada[i]).reshape(b, N_ADA, 1, d)
        mod_c = (jax.nn.silu(c_ctx.astype(jnp.float32)) @ w_ada[i] + b_ada[i]).reshape(N_ADA, 1, 1, d)
        hl = modulate(rms_norm(lat, norm1_w[i]), mod_l[:, 0], mod_l[:, 1])
        hc = modulate(rms_norm(cx, norm1_w[i]), mod_c[0], mod_c[1])
        ol, oc = token_mixer(hl, hc, w_in[i], pool_w[i], pool_scale[i], conv_w[i], conv_b[i],
                             dt_bias[i], a_log[i], d_skip[i], ssm_norm_w[i], w_branch[i], w_out[i],
                             not last)
        lat = lat + (mod_l[:, 2] * ol).astype(lat.dtype)
        hl = modulate(rms_norm(lat, norm2_w[i]), mod_l[:, 3], mod_l[:, 4])
        if last:
            fl = moe(hl.reshape(-1, d), router_w[i], router_b[i], moe_w1[i], moe_b1[i],
                     moe_w2[i], moe_b2[i])
        else:
            cx = cx + (mod_c[2] * oc).astype(cx.dtype)
            hc = modulate(rms_norm(cx, norm2_w[i]), mod_c[3], mod_c[4])
            f = moe(jnp.concatenate([hl.reshape(-1, d), hc.reshape(-1, d)], axis=0), router_w[i],
                    router_b[i], moe_w1[i], moe_b1[i], moe_w2[i], moe_b2[i])
            fl = f[:b * n]
            cx = cx + (mod_c[5] * f[b * n:].reshape(b, -1, d)).astype(cx.dtype)
        lat = lat + (mod_l[:, 5] * fl.reshape(b, n, d)).astype(lat.dtype)
    return rms_norm(lat, final_norm_w).astype(x.dtype)
```

```python
import numpy as np
import ml_dtypes
import concourse.bass as bass
import concourse.mybir as mybir
from concourse.bass_utils import run_bass_kernel_spmd

F32 = mybir.dt.float32
BF16 = mybir.dt.bfloat16
ALU = mybir.AluOpType
AF = mybir.ActivationFunctionType
AX = mybir.AxisListType

D = 1024
B = 2
SEQ = 8192
CTX = 256
NCORE = 8
TS_T = 2176
TS_NT = 17
EPS = 1e-6
NBF = ml_dtypes.bfloat16


_DRAM_CACHE = {}


class Prog:
    ENGS = ("pe", "act", "dve", "pool", "sp")
    NDMA = 40
    EPOCH = 30000

    def __init__(self, nc=None, prefix=""):
        self.nc = nc if nc is not None else bass.Bass("TRN2", target_bir_lowering=False)
        self.prefix = prefix
        self.q = {e: [] for e in self.ENGS}
        self.cnt = {e: 0 for e in self.ENGS}
        self.pending = {e: False for e in self.ENGS}
        self.sems = {}
        self.cur = {}
        self._ctx = []
        self._sem_handles = []
        for e in self.ENGS:
            self._new_epoch(e)
        self.dma_sems = [self._sem(f"{self.prefix}dq{i}") for i in range(self.NDMA)]
        self.dma_use = [0] * self.NDMA
        self.dma_i = 0
        self.seen = {e: {} for e in self.ENGS}
        self.last_w = {}
        self.readers = {}
        self.n_ins = 0

    def _sem(self, name):
        h = self.nc.alloc_semaphore(name=name)
        self._sem_handles.append(h)
        return h

    def _enter(self, cm):
        v = cm.__enter__()
        self._ctx.append(cm)
        return v

    def _new_epoch(self, e):
        k = len([1 for n in self.sems if n.startswith(e + "_")])
        s = self._sem(f"{self.prefix}{e}_{k}")
        self.sems[f"{e}_{k}"] = s
        self.cur[e] = (f"{e}_{k}", s)
        self.cnt[e] = 0

    def sbuf(self, name, shape, dt):
        return self._enter(self.nc.sbuf_tensor(self.prefix + name, list(shape), dt))

    def psum(self, name, shape, dt=F32):
        return self._enter(self.nc.psum_tensor(self.prefix + name, list(shape), dt))

    def dram(self, name, shape, dt, kind):
        cache = _DRAM_CACHE.setdefault(id(self.nc), {})
        if name not in cache:
            cache[name] = self.nc.dram_tensor(name, list(shape), dt, kind=kind).ap()
        return cache[name]

    def _deps(self, reads, writes):
        toks = []
        for k in reads:
            if k in self.last_w:
                toks.append(self.last_w[k])
        for k in writes:
            if k in self.last_w:
                toks.append(self.last_w[k])
            toks.extend(self.readers.get(k, []))
        return toks

    def _waits(self, eng, toks):
        best = {}
        for (name, sem, val) in toks:
            if val > best.get(name, (None, 0))[1]:
                best[name] = (sem, val)
        out = []
        for name, (sem, val) in best.items():
            if name.startswith("pe_") and eng == "pe":
                continue
            if self.seen[eng].get(name, 0) >= val:
                continue
            self.seen[eng][name] = val
            out.append((sem, val))
        return out

    def _record(self, tok, reads, writes):
        for k in reads:
            self.readers.setdefault(k, []).append(tok)
        for k in writes:
            self.last_w[k] = tok
            self.readers[k] = []

    def op(self, eng, fn, reads=(), writes=(), inc=True):
        writes = list(writes) + [k for k in reads if k.startswith("ps")]
        reads = [k for k in reads if not k.startswith("ps")]
        toks = self._deps(reads, writes)
        waits = self._waits(eng, toks)
        name, sem = self.cur[eng]
        tok = (name, sem, self.cnt[eng] + 1)
        self.q[eng].append((waits, fn, (sem if inc else None)))
        self._record(tok, reads, writes)
        self.n_ins += 1
        if inc:
            self.cnt[eng] += 1
            self.pending[eng] = False
            if self.cnt[eng] >= self.EPOCH:
                self._new_epoch(eng)
        else:
            self.pending[eng] = True

    def dma(self, out, in_, reads=(), writes=(), eng="sp"):
        slot = self.dma_i % self.NDMA
        self.dma_i += 1
        sem = self.dma_sems[slot]
        toks = self._deps(reads, writes)
        if self.dma_use[slot] > 0:
            toks.append((f"dq{slot}", sem, 16 * self.dma_use[slot]))
        waits = self._waits(eng, toks)
        self.dma_use[slot] += 1
        tok = (f"dq{slot}", sem, 16 * self.dma_use[slot])
        self.q[eng].append((waits, lambda e: e.dma_start(out=out, in_=in_), ("dma", sem)))
        self._record(tok, reads, writes)
        self.n_ins += 1
        return tok

    def finish(self, final_toks):
        nc = self.nc
        fw = self._waits("sp", list(final_toks))
        self.q["sp"].append((fw, None, None))
        for e in self.ENGS:
            assert not self.pending[e], f"engine {e} has trailing un-inc'ed instructions"
        qs = self.q
        with nc.Block() as block:
            def run(engobj, lst):
                for waits, fn, inc in lst:
                    for sem, val in waits:
                        engobj.wait_ge(sem, val)
                    if fn is None:
                        continue
                    ins = fn(engobj)
                    if inc is None:
                        continue
                    if isinstance(inc, tuple):
                        ins.then_inc(inc[1], 16)
                    else:
                        ins.then_inc(inc, 1)

            @block.sync
            def _(e):
                run(e, qs["sp"])

            @block.tensor
            def _(e):
                run(e, qs["pe"])

            @block.scalar
            def _(e):
                run(e, qs["act"])

            @block.vector
            def _(e):
                run(e, qs["dve"])

            @block.gpsimd
            def _(e):
                run(e, qs["pool"])
        for cm in reversed(self._ctx):
            cm.__exit__(None, None, None)
        if self.prefix:
            nc.clear_and_free_semaphores(self._sem_handles)
            nc.all_engine_barrier()
        return nc


def run_spmd(prog_nc, in_maps):
    res = run_bass_kernel_spmd(prog_nc, in_maps, core_ids=list(range(NCORE)))
    return res.results


def build_s0():
    P = Prog()
    nc = P.nc
    NCOL = 768
    vT = P.dram("vT", [128, 8, 3], F32, "ExternalInput")
    w = P.dram("w", [2, 1024, NCOL], F32, "ExternalInput")
    bb = P.dram("bb", [2, 3, NCOL], F32, "ExternalInput")
    out = P.dram("mod", [2, 3, NCOL], F32, "ExternalOutput")
    vt = P.sbuf("vt", [128, 8, 3], F32)
    sv = P.sbuf("sv", [128, 8, 3], F32)
    wt = P.sbuf("wt", [128, 2, 8, NCOL], F32)
    bt = P.sbuf("bt", [3, 2, NCOL], F32)
    ot = P.sbuf("ot", [3, 2, NCOL], F32)
    ps = [P.psum(f"ps{i}", [3, 384]) for i in range(4)]
    P.dma(vt[:], vT, writes=["vt"])
    for l in range(2):
        P.dma(wt[:, l], w[l].rearrange("(c p) n -> p c n", p=128), writes=[f"wt{l}"])
        P.dma(bt[:, l], bb[l], writes=[f"bt{l}"])
    P.op("act", lambda e: e.activation(out=sv[:], in_=vt[:], func=AF.Silu), reads=["vt"], writes=["sv"])
    toks = []
    for l in range(2):
        for h in range(2):
            pst = ps[l * 2 + h]
            for kc in range(8):
                P.op("pe", lambda e, kc=kc, l=l, h=h, pst=pst: e.matmul(
                    pst[:], lhsT=sv[:, kc, :], rhs=wt[:, l, kc, h * 384:(h + 1) * 384],
                    start=(kc == 0), stop=(kc == 7)),
                    reads=["sv", f"wt{l}"], writes=[f"ps{l}{h}"], inc=(kc == 7))
            P.op("dve", lambda e, l=l, h=h, pst=pst: e.tensor_tensor(
                out=ot[:, l, h * 384:(h + 1) * 384], in0=pst[:], in1=bt[:, l, h * 384:(h + 1) * 384], op=ALU.add),
                reads=[f"ps{l}{h}", f"bt{l}"], writes=[f"ot{l}{h}"])
        toks.append(P.dma(out[l], ot[:, l], reads=[f"ot{l}0", f"ot{l}1"], writes=[f"out{l}"]))
    return P.finish(toks)


def run_s0(c, c_ctx, w_ada, b_ada):
    v = np.concatenate([c, c_ctx[None]], axis=0)
    vT = np.ascontiguousarray(v.T.reshape(8, 128, 3).transpose(1, 0, 2))
    nc = build_s0()
    maps = []
    for core in range(NCORE):
        sl = slice(core * 768, (core + 1) * 768)
        maps.append({
            "vT": vT,
            "w": np.ascontiguousarray(w_ada[:, :, sl]),
            "bb": np.ascontiguousarray(np.broadcast_to(b_ada[:, None, sl], (2, 3, 768))),
        })
    res = run_spmd(nc, maps)
    mod = np.concatenate([r["mod"] for r in res], axis=2)
    return mod.reshape(2, 3, 6, D)


def emit_norm_T(P, lat_tile, key_lat, modA, modB, which, hT_dst, hT_key, ident, ps_pair, tagi, scr):
    junk, ssq, rstd, xn = scr
    t = tagi
    P.op("act", lambda e: e.activation(out=junk[:], in_=lat_tile, func=AF.Square, accum_out=ssq[:]),
         reads=[key_lat], writes=["junk", "ssq"])
    P.op("act", lambda e: e.activation(out=rstd[:], in_=ssq[:], func=AF.Sqrt, bias=float(D * EPS)),
         reads=["ssq"], writes=["rstd"])
    P.op("dve", lambda e: e.reciprocal(out=rstd[:], in_=rstd[:]), reads=["rstd"], writes=["rstd"])
    P.op("dve", lambda e: e.tensor_scalar(out=xn[:], in0=lat_tile, scalar1=rstd[:, 0:1], scalar2=None, op0=ALU.mult),
         reads=[key_lat, "rstd"], writes=["xn"])
    for half in range(2):
        ps = ps_pair[half]
        for j in range(4):
            c = half * 4 + j
            P.op("pe", lambda e, c=c, j=j, ps=ps: e.transpose(ps[:, j * 128:(j + 1) * 128], xn[:, c * 128:(c + 1) * 128], ident[:]),
                 reads=["xn", "ident"], writes=[f"psT{half}"], inc=(j == 3))
        for j in range(4):
            c = half * 4 + j
            eng = "act" if j % 2 == 0 else "dve"
            if eng == "act":
                P.op("act", lambda e, c=c, j=j, ps=ps: e.activation(
                    out=hT_dst[:, c, :], in_=ps[:, j * 128:(j + 1) * 128], func=AF.Identity,
                    scale=modA[:, c, which:which + 1], bias=modB[:, c, which:which + 1]),
                    reads=[f"psT{half}", "modAB"], writes=[hT_key + f"_{c}"])
            else:
                P.op("dve", lambda e, c=c, j=j, ps=ps: e.tensor_scalar(
                    out=hT_dst[:, c, :], in0=ps[:, j * 128:(j + 1) * 128],
                    scalar1=modA[:, c, which:which + 1], scalar2=modB[:, c, which:which + 1],
                    op0=ALU.mult, op1=ALU.add),
                    reads=[f"psT{half}", "modAB"], writes=[hT_key + f"_{c}"])


def load_modAB(P, nwT_d, scT_d, shT_d, name=""):
    nw = P.sbuf(name + "nw", [128, 8], F32)
    modA = P.sbuf(name + "modA", [128, 8, 2], F32)
    modB = P.sbuf(name + "modB", [128, 8, 2], F32)
    P.dma(nw[:], nwT_d, writes=[name + "nw"])
    P.dma(modA[:], scT_d, writes=[name + "modA0"])
    P.dma(modB[:], shT_d, writes=["modAB_B" + name])
    for wch in range(2):
        P.op("dve", lambda e, wch=wch: e.scalar_tensor_tensor(
            out=modA[:, :, wch], in0=modA[:, :, wch], scalar=1.0, in1=nw[:], op0=ALU.add, op1=ALU.mult),
            reads=[name + "nw", name + "modA0"], writes=[name + "modA0"])
    P.op("dve", lambda e: e.tensor_scalar(out=modA[:], in0=modA[:], scalar1=32.0, scalar2=None, op0=ALU.mult),
         reads=[name + "modA0", "modAB_B" + name], writes=["modAB"])
    return modA, modB


def build_s1(with_partials):
    P = Prog()
    xin = P.dram("xin", [TS_T, D], F32, "ExternalInput")
    nwT = P.dram("nwT", [128, 8], F32, "ExternalInput")
    scT = P.dram("scT", [128, 8, 2], F32, "ExternalInput")
    shT = P.dram("shT", [128, 8, 2], F32, "ExternalInput")
    identd = P.dram("ident", [128, 128], F32, "ExternalInput")
    fwb = P.dram("fwb", [128, D], F32, "ExternalInput")
    if with_partials:
        part = P.dram("part", [NCORE, TS_T, D], BF16, "ExternalInput")
        g2b = P.dram("g2b", [2, 128, D], F32, "ExternalInput")
        lat_o = P.dram("lat_o", [TS_T, D], F32, "ExternalOutput")
        fin_o = P.dram("fin_o", [TS_T, D], F32, "ExternalOutput")
    hT_o = P.dram("hT_o", [D, TS_T], BF16, "ExternalOutput")

    ident = P.sbuf("ident_s", [128, 128], F32)
    P.dma(ident[:], identd, writes=["ident"])
    modA, modB = load_modAB(P, nwT, scT, shT)
    fw = P.sbuf("fw", [128, D], F32)
    P.dma(fw[:], fwb, writes=["fw"])
    if with_partials:
        g2 = P.sbuf("g2", [128, 2, D], F32)
        P.dma(g2[:], g2b.rearrange("w p d -> p w d"), writes=["g2"])
    hT = P.sbuf("hT", [128, 8, TS_T], BF16)
    lat = [P.sbuf(f"lat{i}", [128, D], F32) for i in range(2)]
    pt = ([P.sbuf("pt0", [128, D], F32)] + [P.sbuf(f"pt{i}", [128, D], BF16) for i in range(1, 4)]) if with_partials else None
    fin = [P.sbuf(f"fin{i}", [128, D], F32) for i in range(2)] if with_partials else None
    scr = (P.sbuf("junk", [128, D], BF16), P.sbuf("ssq", [128, 1], F32), P.sbuf("rstd", [128, 1], F32),
           P.sbuf("xn", [128, D], F32))
    ps_pair = [P.psum(f"psT{i}", [128, 512]) for i in range(2)]
    toks = []
    for t in range(TS_NT):
        which = 1 if t == TS_NT - 1 else 0
        lt = lat[t % 2]
        kl = f"lat{t % 2}"
        P.dma(lt[:], xin[t * 128:(t + 1) * 128, :], writes=[kl])
        if with_partials:
            for c in range(NCORE):
                bi = 1 + (c % 3)
                pb = pt[bi]
                P.dma(pb[:], part[c, t * 128:(t + 1) * 128, :], writes=[f"pt{bi}"])
                if c == 0:
                    P.op("dve", lambda e, pb=pb: e.tensor_copy(out=pt[0][:], in_=pb[:]), reads=[f"pt{bi}"], writes=["pt0"])
                    continue
                P.op("dve", lambda e, pb=pb: e.tensor_tensor(out=pt[0][:], in0=pt[0][:], in1=pb[:], op=ALU.add),
                     reads=[f"pt{bi}", "pt0"], writes=["pt0"])
            P.op("dve", lambda e, which=which: e.tensor_tensor(out=pt[0][:], in0=pt[0][:], in1=g2[:, which, :], op=ALU.mult),
                 reads=["pt0", "g2"], writes=["pt0"])
            P.op("dve", lambda e, lt=lt: e.tensor_tensor(out=lt[:], in0=lt[:], in1=pt[0][:], op=ALU.add),
                 reads=["pt0", kl], writes=[kl])
            toks.append(P.dma(lat_o[t * 128:(t + 1) * 128, :], lt[:], reads=[kl], writes=[f"lato{t}"]))
        emit_norm_T(P, lt[:], kl, modA, modB, which, hT[:, :, t * 128:(t + 1) * 128], f"hT{t}", ident, ps_pair, t, scr)
        if with_partials:
            fb = fin[t % 2]
            P.op("dve", lambda e, fb=fb: e.scalar_tensor_tensor(out=fb[:], in0=scr[3][:], scalar=32.0, in1=fw[:],
                                                                  op0=ALU.mult, op1=ALU.mult),
                 reads=["xn", "fw"], writes=[f"fin{t % 2}"])
            toks.append(P.dma(fin_o[t * 128:(t + 1) * 128, :], fb[:], reads=[f"fin{t % 2}"], writes=[f"fino{t}"]))
    for c in range(8):
        toks.append(P.dma(hT_o[c * 128:(c + 1) * 128, :], hT[:, c, :],
                          reads=[f"hT{t}_{c}" for t in range(TS_NT)], writes=[f"hTo{c}"]))
    return P.finish(toks)


def ssd_consts():
    tri_f = np.triu(np.ones((128, 128), np.float32))
    tri_b = np.tril(np.ones((128, 128), np.float32))
    NEG = -30000.0
    mf = np.where(np.arange(128)[None, :] >= np.arange(128)[:, None], 0.0, NEG).astype(np.float32)
    mb = np.where(np.arange(128)[None, :] <= np.arange(128)[:, None], 0.0, NEG).astype(np.float32)
    c = np.zeros((128, 6, 128), np.float32)
    c[:, 0] = tri_f; c[:, 1] = tri_b; c[:, 2] = -tri_f; c[:, 3] = -tri_b
    c[:, 4] = np.eye(128); c[:, 5] = 1.0
    m4 = np.stack([np.tile(mf, (1, 4)), np.tile(mb, (1, 4))], 1)
    return c, m4.astype(np.float32)


def build_s2c(nlat, nc=None, pfx=""):
    P = Prog(nc, pfx)
    TT = CTX + nlat
    NB = TT // 256
    NCH = TT // 128
    hTp = P.dram("hTp", [D, NB, 260], BF16, "ExternalInput")
    wxbc_d = P.dram("wxbc", [D, 512], F32, "ExternalInput")
    wz_d = P.dram("wz", [D, 256], F32, "ExternalInput")
    wdt_d = P.dram("wdt40", [D, 40], F32, "ExternalInput")
    cw_d = P.dram("convwT", [128, 4, 5], F32, "ExternalInput")
    cb_d = P.dram("convbT", [128, 4], F32, "ExternalInput")
    dtb_d = P.dram("dtb40", [40, 1], F32, "ExternalInput")
    alog_d = P.dram("alog40", [40, 1], F32, "ExternalInput")
    dsk_d = P.dram("dskb", [128, 4], F32, "ExternalInput")
    nw_d = P.dram("snwT", [128, 2], F32, "ExternalInput")
    c6_d = P.dram("c6", [128, 6, 128], F32, "ExternalInput")
    m4_d = P.dram("m4", [128, 2, 512], F32, "ExternalInput")
    ycT_o = P.dram("ycT_o", [256, TT], BF16, "ExternalOutput")
    ssq_o = P.dram("ssq_o", [1, TT], F32, "ExternalOutput")

    c6 = P.sbuf("c6s", [128, 6, 128], F32)
    m4 = P.sbuf("m4s", [128, 2, 512], F32)
    P.dma(c6[:], c6_d, writes=["c6"])
    P.dma(m4[:], m4_d, writes=["m4"])
    identb = P.sbuf("identb", [128, 128], BF16)
    P.op("dve", lambda e: e.tensor_copy(out=identb[:], in_=c6[:, 4, :]), reads=["c6"], writes=["identb"])
    TRI = [c6[:, 0, :], c6[:, 1, :]]
    NTRI = [c6[:, 2, :], c6[:, 3, :]]
    IDF = c6[:, 4, :]
    ONES = c6[:, 5, :]
    wst = P.sbuf("wst", [128, 8, 512], F32)
    wsc = P.sbuf("wsc", [128, 4, 512], F32)
    wxb = P.sbuf("wxb", [128, 8, 512], BF16)
    wzb = P.sbuf("wzb", [128, 8, 256], BF16)
    wdtb = P.sbuf("wdtb", [128, 8, 40], BF16)
    P.dma(wst[:], wxbc_d.rearrange("(c p) n -> p c n", p=128), writes=["wst"])
    P.op("dve", lambda e: e.tensor_copy(out=wxb[:], in_=wst[:]), reads=["wst"], writes=["wxb"])
    P.dma(wst[:, :, 0:256], wz_d.rearrange("(c p) n -> p c n", p=128), writes=["wst"])
    P.op("dve", lambda e: e.tensor_copy(out=wzb[:], in_=wst[:, :, 0:256]), reads=["wst"], writes=["wzb"])
    P.dma(wst[:, :, 256:296], wdt_d.rearrange("(c p) n -> p c n", p=128), writes=["wst"])
    P.op("dve", lambda e: e.tensor_copy(out=wdtb[:], in_=wst[:, :, 256:296]), reads=["wst"], writes=["wdtb"])
    ALIAS = ["abc0", "E0", "t10", "t30", "ST0", "cbT0", "zs0", "zs1", "g0", "g1", "sq0", "sq1"]
    P.op("dve", lambda e: e.memset(wst[:], 0.0), writes=["wst"] + ALIAS)
    cw = P.sbuf("cw", [128, 4, 5], F32)
    cb = P.sbuf("cb", [128, 4], F32)
    dtb = P.sbuf("dtb", [40, 1], F32)
    A40 = P.sbuf("A40", [40, 1], F32)
    dsk = P.sbuf("dsk", [128, 4], F32)
    snw = P.sbuf("snw", [128, 2], F32)
    P.dma(cw[:], cw_d, writes=["cw"])
    P.dma(cb[:], cb_d, writes=["cb"])
    P.dma(dtb[:], dtb_d, writes=["dtb"])
    P.dma(A40[:], alog_d, writes=["A40"])
    P.dma(dsk[:], dsk_d, writes=["dsk"])
    P.dma(snw[:], nw_d, writes=["snw"])
    P.op("act", lambda e: e.activation(out=A40[:], in_=A40[:], func=AF.Exp), reads=["A40"], writes=["A40"])
    P.op("dve", lambda e: e.tensor_scalar(out=A40[:], in0=A40[:], scalar1=-1.0, scalar2=None, op0=ALU.mult),
         reads=["A40"], writes=["A40"])

    xbcT = P.sbuf("xbcT", [128, 4, TT], BF16)
    dt40 = P.sbuf("dt40", [40, 256], F32)
    dtmp = P.sbuf("dtmp", [40, 256], F32)
    dta = P.sbuf("dta", [128, NCH, 16], F32)
    yacc = P.sbuf("yacc", [128, NCH, 256], BF16)
    cacc = [P.sbuf(f"cacc{i}", [128, 256], F32) for i in range(2)]
    hb = [P.sbuf(f"hb{i}", [128, 8, 260], BF16) for i in range(2)]
    psA = [P.psum(f"psA{i}", [128, 512]) for i in range(2)]
    psD = P.psum("psD", [128, 512])
    psY = P.psum("psY", [128, 512])
    psS = [P.psum(f"psS{i}", [128, 512]) for i in range(2)]
    psTb = [P.psum(f"psTb{i}", [128, 1024], BF16) for i in range(2)]

    for blk in range(NB):
        h = hb[blk % 2]
        hk = f"hb{blk % 2}"
        P.dma(h[:], hTp.rearrange("(c p) b t -> p c b t", p=128)[:, :, blk, :], writes=[hk])
        for cc in range(4):
            ps = psA[cc % 2]
            ca = cacc[cc % 2]; ck = f"cacc{cc % 2}"
            for kc in range(8):
                P.op("pe", lambda e, ps=ps, kc=kc, cc=cc, h=h: e.matmul(
                    ps[:, 0:260], lhsT=wxb[:, kc, cc * 128:(cc + 1) * 128], rhs=h[:, kc, :], start=(kc == 0), stop=(kc == 7)),
                    reads=[hk, "wxb"], writes=[f"psA{cc % 2}"], inc=(kc == 7))
            P.op("dve", lambda e, ps=ps, ca=ca, cc=cc: e.tensor_scalar(out=ca[:], in0=ps[:, 0:256], scalar1=cw[:, cc, 0:1], scalar2=None, op0=ALU.mult),
                 reads=[f"psA{cc % 2}", "cw"], writes=[ck])
            for k in range(1, 5):
                P.op("dve", lambda e, ps=ps, ca=ca, cc=cc, k=k: e.scalar_tensor_tensor(out=ca[:], in0=ps[:, k:k + 256], scalar=cw[:, cc, k:k + 1], in1=ca[:],
                                                                                       op0=ALU.mult, op1=ALU.add),
                     reads=[f"psA{cc % 2}", "cw", ck], writes=[ck])
            P.op("act", lambda e, ca=ca, cc=cc, blk=blk: e.activation(
                out=xbcT[:, cc, blk * 256:(blk + 1) * 256], in_=ca[:], func=AF.Silu, bias=cb[:, cc:cc + 1]),
                reads=[ck, "cb"], writes=[f"xbcT{blk}"])
        ps = psA[0]
        for kc in range(8):
            P.op("pe", lambda e, ps=ps, kc=kc, h=h: e.matmul(ps[0:40, 256:512], lhsT=wdtb[:, kc, :], rhs=h[:, kc, 2:258],
                                                              start=(kc == 0), stop=(kc == 7)),
                 reads=[hk, "wdtb"], writes=["psA0"], inc=(kc == 7))
        P.op("act", lambda e, ps=ps: e.activation(out=dtmp[:], in_=ps[0:40, 256:512], func=AF.Exp, bias=dtb[:, 0:1]),
             reads=["psA0", "dtb"], writes=["dtmp"])
        P.op("act", lambda e: e.activation(out=dt40[:], in_=dtmp[:], func=AF.Ln, bias=1.0),
             reads=["dtmp"], writes=["dt40"])
        P.op("dve", lambda e: e.tensor_scalar(out=dt40[32:40, :], in0=dt40[32:40, :], scalar1=A40[32:40, 0:1], scalar2=None, op0=ALU.mult),
             reads=["dt40", "A40"], writes=["dt40"])
        for j in range(2):
            ch = blk * 2 + j
            P.op("pe", lambda e, j=j: e.transpose(psS[0][:, j * 64:j * 64 + 40], dt40[:, j * 128:(j + 1) * 128], IDF[0:40, 0:40]),
                 reads=["dt40", "c6"], writes=["psS0"])
            P.op("dve", lambda e, j=j, ch=ch: e.tensor_copy(out=dta[:, ch, :].rearrange("p (a b) -> p a b", a=2),
                                                            in_=psS[0][:, j * 64:j * 64 + 64].rearrange("p (a b) -> p a b", a=2)[:, :, 0:8]),
                 reads=["psS0"], writes=[f"dta{ch}"])

    scr = [wst, wsc]
    sfx = ["0", "1"]
    P.op("dve", lambda e: e.memset(wsc[:], 0.0), writes=["abc1", "E1", "t11", "t31", "ST1", "cbT1"])
    tiles = []
    for d in range(2):
        w_ = scr[d]
        tiles.append(dict(
            abc=w_[:, 0, :].rearrange("p (h l) -> p h l", h=4), E=w_[:, 1, :].rearrange("p (h l) -> p h l", h=4),
            t1=w_[:, 2, 0:256].rearrange("p (h d) -> p h d", h=4), t3=w_[:, 2, 256:512].rearrange("p (h d) -> p h d", h=4),
            ST=w_[:, 3, 0:256].rearrange("p (h d) -> p h d", h=4), cbT=w_[:, 3, 256:384],
            STb=P.sbuf(f"STb{d}", [128, 4, 64], BF16), cs=P.sbuf(f"cs{d}", [128, 12], F32), ecs=P.sbuf(f"ecs{d}", [128, 12], F32),
            MT=P.sbuf(f"MT{d}", [128, 4, 128], BF16), xtok=P.sbuf(f"xtok{d}", [128, 4, 64], BF16), btok=P.sbuf(f"btok{d}", [128, 128], BF16),
            xdt=P.sbuf(f"xdt{d}", [128, 4, 64], BF16), xdd=P.sbuf(f"xdd{d}", [128, 4, 64], BF16),
            pD=(psD if d == 0 else psA[0]), kD=("psD" if d == 0 else "psA0"),
            pY=(psY if d == 0 else psA[1]), kY=("psY" if d == 0 else "psA1"),
            pS=psS[d], kS=f"psS{d}", pT=psTb[d], kT=f"psTb{d}"))
    written = set()

    def ssd_iter(d, ch):
        T = tiles[d]; x = sfx[d]
        sl = slice(ch * 128, (ch + 1) * 128)
        blkkey = f"xbcT{ch // 2}"
        a4 = dta[:, ch, 8 + 4 * d:12 + 4 * d]
        dt4 = dta[:, ch, 4 * d:4 * d + 4]
        pD, pY, pS, pT = T["pD"], T["pY"], T["pS"], T["pT"]
        kD, kY, kS, kT = T["kD"], T["kY"], T["kS"], T["kT"]
        for j in range(3):
            P.op("pe", lambda e, j=j: e.transpose(pT[:, j * 128:(j + 1) * 128], xbcT[:, j, sl], identb[:]),
                 reads=[blkkey, "identb"], writes=[kT], inc=(j == 2))
        yield
        P.op("act", lambda e: e.copy(out=T["xtok"][:].rearrange("p h d -> p (h d)"), in_=pT[:, 0:256]), reads=[kT], writes=["xtok" + x])
        yield
        P.op("act", lambda e: e.copy(out=T["btok"][:], in_=pT[:, 256:384]), reads=[kT], writes=["btok" + x])
        yield
        P.op("pe", lambda e: e.matmul(pS[:, 384:512], lhsT=xbcT[:, 2, sl], rhs=xbcT[:, 3, sl], start=True, stop=True),
             reads=[blkkey], writes=[kS])
        yield
        P.op("act", lambda e: e.copy(out=T["cbT"], in_=pS[:, 384:512]), reads=[kS], writes=["cbT" + x])
        yield
        P.op("pe", lambda e: e.matmul(pS[:, 256:260], lhsT=TRI[d], rhs=a4, start=True, stop=True),
             reads=[f"dta{ch}", "c6"], writes=[kS], inc=False)
        P.op("pe", lambda e: e.matmul(pS[:, 260:264], lhsT=ONES, rhs=a4, start=True, stop=True),
             reads=[f"dta{ch}", "c6"], writes=[kS])
        yield
        P.op("dve", lambda e: e.tensor_copy(out=T["abc"], in_=a4.unsqueeze(2).broadcast_to([128, 4, 128])),
             reads=[f"dta{ch}"], writes=["abc" + x])
        yield
        for hh in range(4):
            P.op("pe", lambda e, hh=hh: e.matmul(pD[:, hh * 128:(hh + 1) * 128], lhsT=T["abc"][:, hh, :], rhs=TRI[d],
                                                 start=(hh == 0), stop=False),
                 reads=["abc" + x, "c6"], writes=[kD], inc=False)
        P.op("pe", lambda e: e.matmul(pD[:, :], lhsT=NTRI[d], rhs=T["abc"].rearrange("p h l -> p (h l)"), start=False, stop=False),
             reads=["abc" + x, "c6"], writes=[kD], inc=False)
        P.op("pe", lambda e: e.matmul(pD[:, :], lhsT=IDF, rhs=m4[:, d, :], start=False, stop=True),
             reads=["m4", "c6"], writes=[kD])
        yield
        P.op("act", lambda e: e.activation(out=T["E"].rearrange("p h l -> p (h l)"), in_=pD[:, :], func=AF.Exp),
             reads=[kD], writes=["E" + x])
        yield
        P.op("dve", lambda e: e.tensor_tensor(out=T["MT"][:], in0=T["E"], in1=T["cbT"].unsqueeze(1).broadcast_to([128, 4, 128]), op=ALU.mult),
             reads=["E" + x, "cbT" + x], writes=["MT" + x])
        yield
        cs, ecs = T["cs"], T["ecs"]
        P.op("dve", lambda e: e.tensor_copy(out=cs[:, 0:4], in_=pS[:, 256:260]), reads=[kS], writes=["cs" + x])
        P.op("dve", lambda e: e.tensor_copy(out=cs[:, 8:12], in_=pS[:, 260:264]), reads=[kS], writes=["cs" + x])
        P.op("dve", lambda e: e.tensor_tensor(out=cs[:, 4:8], in0=cs[:, 8:12], in1=cs[:, 0:4], op=ALU.subtract),
             reads=["cs" + x], writes=["cs" + x])
        yield
        P.op("act", lambda e: e.activation(out=ecs[:], in_=cs[:], func=AF.Exp), reads=["cs" + x], writes=["ecs" + x])
        yield
        P.op("dve", lambda e: e.tensor_tensor(out=T["xdt"][:], in0=T["xtok"][:], in1=dt4.unsqueeze(2).broadcast_to([128, 4, 64]), op=ALU.mult),
             reads=["xtok" + x, f"dta{ch}"], writes=["xdt" + x])
        yield
        P.op("pool", lambda e: e.tensor_tensor(out=T["xdd"][:], in0=T["xdt"][:], in1=ecs[:, 4:8].unsqueeze(2).broadcast_to([128, 4, 64]), op=ALU.mult),
             reads=["xdt" + x, "ecs" + x], writes=["xdd" + x])
        yield
        for hh in range(4):
            P.op("pe", lambda e, hh=hh: e.matmul(pY[:, hh * 64:(hh + 1) * 64], lhsT=T["MT"][:, hh, :], rhs=T["xdt"][:, hh, :],
                                                 start=(hh == 0), stop=(hh == 3)),
                 reads=["MT" + x, "xdt" + x], writes=[kY], inc=(hh == 3))
        P.op("pe", lambda e: e.matmul(pY[:, 256:512], lhsT=xbcT[:, 3, sl], rhs=T["STb"][:].rearrange("p h d -> p (h d)"),
                                      start=True, stop=True),
             reads=[blkkey, "STb" + x], writes=[kY])
        yield
        t1 = T["t1"]; t3 = T["t3"]
        P.op("dve", lambda e: e.tensor_tensor(out=t1, in0=pY[:, 256:512].rearrange("p (h d) -> p h d", h=4),
                                              in1=ecs[:, 0:4].unsqueeze(2).broadcast_to([128, 4, 64]), op=ALU.mult),
             reads=[kY, "ecs" + x], writes=["t1" + x])
        P.op("dve", lambda e: e.tensor_tensor(out=t1, in0=t1, in1=pY[:, 0:256].rearrange("p (h d) -> p h d", h=4), op=ALU.add),
             reads=[kY, "t1" + x], writes=["t1" + x])
        yield
        ya = yacc[:, ch, :].rearrange("p (h d) -> p h d", h=4)
        if d == 0:
            P.op("pool", lambda e: e.tensor_tensor(out=t3, in0=T["xtok"][:], in1=dsk[:].unsqueeze(2).broadcast_to([128, 4, 64]), op=ALU.mult),
                 reads=["xtok" + x, "dsk"], writes=["t3" + x])
            P.op("pool", lambda e: e.tensor_tensor(out=t1, in0=t1, in1=t3, op=ALU.add), reads=["t1" + x, "t3" + x], writes=["t1" + x])
        if ch not in written:
            written.add(ch)
            P.op("dve", lambda e: e.tensor_copy(out=ya, in_=t1), reads=["t1" + x], writes=[f"yacc{ch}"])
        else:
            P.op("dve", lambda e: e.tensor_tensor(out=ya, in0=ya, in1=t1, op=ALU.add), reads=["t1" + x, f"yacc{ch}"], writes=[f"yacc{ch}"])
        yield
        P.op("pe", lambda e: e.matmul(pS[:, 0:256], lhsT=T["btok"][:], rhs=T["xdd"][:].rearrange("p h d -> p (h d)"), start=True, stop=True),
             reads=["btok" + x, "xdd" + x], writes=[kS])
        yield
        ST = T["ST"]
        P.op("dve", lambda e: e.tensor_tensor(out=ST, in0=ST, in1=ecs[:, 8:12].unsqueeze(2).broadcast_to([128, 4, 64]), op=ALU.mult),
             reads=["ST" + x, "ecs" + x], writes=["ST" + x])
        P.op("dve", lambda e: e.tensor_tensor(out=ST, in0=ST, in1=pS[:, 0:256].rearrange("p (h d) -> p h d", h=4), op=ALU.add),
             reads=["ST" + x, kS], writes=["ST" + x])
        yield
        P.op("act", lambda e: e.copy(out=T["STb"][:], in_=ST), reads=["ST" + x], writes=["STb" + x])
        yield

    orders = [list(range(NCH)), [1, 0] + list(range(NCH - 1, 1, -1))]
    for d in range(2):
        P.op("dve", lambda e, d=d: e.memset(tiles[d]["STb"][:], 0.0), writes=["STb" + sfx[d]])
    for s_ in range(NCH):
        gens = [ssd_iter(0, orders[0][s_]), ssd_iter(1, orders[1][s_])]
        alive = [True, True]
        while any(alive):
            for gi in range(2):
                if alive[gi]:
                    try:
                        next(gens[gi])
                    except StopIteration:
                        alive[gi] = False

    ycTb = [P.sbuf(f"ycTb{i}", [128, 2, 256], BF16) for i in range(2)]
    ssqb = [P.sbuf(f"ssqb{i}", [1, 256], F32) for i in range(2)]
    toks = []
    zs = wst[:, 4, :].rearrange("p (c t) -> p c t", c=2)
    g = wst[:, 5, :].rearrange("p (c t) -> p c t", c=2)
    sq = wst[:, 6, :].rearrange("p (c t) -> p c t", c=2)
    for blk in range(NB):
        h = hb[blk % 2]
        hk = f"hb{blk % 2}"
        P.dma(h[:], hTp.rearrange("(c p) b t -> p c b t", p=128)[:, :, blk, :], writes=[hk])
        for cc in range(2):
            ps = psA[cc]
            for kc in range(8):
                P.op("pe", lambda e, ps=ps, kc=kc, cc=cc, h=h: e.matmul(ps[:, 0:256], lhsT=wzb[:, kc, cc * 128:(cc + 1) * 128], rhs=h[:, kc, 2:258],
                                                                       start=(kc == 0), stop=(kc == 7)),
                     reads=[hk, "wzb"], writes=[f"psA{cc}"], inc=(kc == 7))
            P.op("act", lambda e, ps=ps, cc=cc: e.activation(out=zs[:, cc, :], in_=ps[:, 0:256], func=AF.Silu),
                 reads=[f"psA{cc}"], writes=[f"zs{cc}"])
        for j in range(2):
            ch = blk * 2 + j
            for cc in range(2):
                P.op("pe", lambda e, j=j, cc=cc, ch=ch: e.transpose(psTb[0][:, (cc * 2 + j) * 128:(cc * 2 + j + 1) * 128],
                                                                   yacc[:, ch, cc * 128:(cc + 1) * 128], identb[:]),
                     reads=[f"yacc{ch}", "identb"], writes=["psTb0"], inc=(j == 1 and cc == 1))
        for cc in range(2):
            P.op("dve", lambda e, cc=cc: e.tensor_tensor(out=g[:, cc, :], in0=psTb[0][:, cc * 256:(cc + 1) * 256], in1=zs[:, cc, :], op=ALU.mult),
                 reads=["psTb0", f"zs{cc}"], writes=[f"g{cc}"])
            P.op("act", lambda e, cc=cc: e.activation(out=sq[:, cc, :], in_=g[:, cc, :], func=AF.Square),
                 reads=[f"g{cc}"], writes=[f"sq{cc}"])
            P.op("dve", lambda e, cc=cc, blk=blk: e.tensor_scalar(out=ycTb[blk % 2][:, cc, :], in0=g[:, cc, :],
                                                                   scalar1=snw[:, cc:cc + 1], scalar2=None, op0=ALU.mult),
                 reads=[f"g{cc}", "snw"], writes=[f"ycTb{blk % 2}"])
        for cc in range(2):
            P.op("pe", lambda e, cc=cc: e.matmul(psD[0:1, 0:256], lhsT=ONES[:, 0:1], rhs=sq[:, cc, :], start=(cc == 0), stop=(cc == 1)),
                 reads=[f"sq{cc}", "c6"], writes=["psD"], inc=(cc == 1))
        P.op("act", lambda e, blk=blk: e.copy(out=ssqb[blk % 2][:], in_=psD[0:1, 0:256]),
             reads=["psD"], writes=[f"ssqb{blk % 2}"])
        toks.append(P.dma(ycT_o.rearrange("(c p) t -> p c t", p=128)[:, :, blk * 256:(blk + 1) * 256], ycTb[blk % 2][:],
                          reads=[f"ycTb{blk % 2}"], writes=[f"yo{blk}"]))
        toks.append(P.dma(ssq_o[:, blk * 256:(blk + 1) * 256], ssqb[blk % 2][:], reads=[f"ssqb{blk % 2}"], writes=[f"so{blk}"]))
    return P.finish(toks)


POOL_WINDOWS = (2, 4, 8, 16)


def _win(n, w):
    t = np.arange(n)
    return np.clip(t - w // 2, 0, n), np.clip(t + w - w // 2, 0, n)


def pool_consts(w, gw):
    lo, hi = _win(128, w)
    Ar = ((np.arange(128)[:, None] >= lo[None, :]) & (np.arange(128)[:, None] < hi[None, :])).astype(np.float32)
    BL = np.zeros((128, 16, 128), np.float32)
    for di, dl in enumerate(range(-8, 8)):
        if -(w // 2) <= dl <= w - w // 2 - 1:
            BL[:, di, :] = Ar
    normrow_l = np.broadcast_to((1.0 / (hi - lo))[None, :], (128, 128)).astype(np.float32)
    clo, chi = _win(gw, w)
    invc = np.broadcast_to((1.0 / (chi - clo))[None, :], (128, gw)).astype(np.float32)
    tlo, thi = _win(256, w)
    BC = np.zeros((128, 4, 128), np.float32)
    normrow_c = np.zeros((128, 2, 128), np.float32)
    for js in range(2):
        for jd in range(2):
            ts = 2 * np.arange(128)[:, None] + js
            td = 2 * np.arange(128)[None, :] + jd
            BC[:, js * 2 + jd, :] = ((ts >= tlo[td]) & (ts < thi[td])).astype(np.float32)
    for jd in range(2):
        td = 2 * np.arange(128) + jd
        normrow_c[:, jd, :] = (1.0 / (thi[td] - tlo[td]))[None, :]
    return {"BL": BL, "BC": BC, "normrow_l": np.ascontiguousarray(normrow_l), "invc": np.ascontiguousarray(invc),
            "normrow_c": normrow_c}


def build_s2a(gw, nc=None, pfx=""):
    P = Prog(nc, pfx)
    NT = gw + 2
    TT = NT * 128
    hTc = P.dram("hTc", [D, TT], BF16, "ExternalInput")
    wp_d = P.dram("wp", [D, 256], F32, "ExternalInput")
    pw_d = P.dram("pw", [256, 256], F32, "ExternalInput")
    ps_d = P.dram("pscT", [128, 2], F32, "ExternalInput")
    BL_d = P.dram("BL", [128, 16, 128], F32, "ExternalInput")
    BC_d = P.dram("BC", [128, 4, 128], F32, "ExternalInput")
    nl_d = P.dram("normrow_l", [128, 128], F32, "ExternalInput")
    ic_d = P.dram("invc", [128, gw], F32, "ExternalInput")
    ncx_d = P.dram("normrow_c", [128, 2, 128], F32, "ExternalInput")
    yaT_o = P.dram("yaT_o", [256, TT], BF16, "ExternalOutput")

    def load_cast(name, src_ap, shape):
        st = P.sbuf(name + "_st", shape, F32)
        bf = P.sbuf(name, shape, BF16)
        P.dma(st[:], src_ap, writes=[name + "_st"])
        P.op("dve", lambda e: e.tensor_copy(out=bf[:], in_=st[:]), reads=[name + "_st"], writes=[name])
        return bf
    wpb = load_cast("wpb", wp_d.rearrange("(c p) n -> p c n", p=128), [128, 8, 256])
    pwb = load_cast("pwb", pw_d.rearrange("(c p) n -> p c n", p=128), [128, 2, 256])
    BLb = load_cast("BLb", BL_d, [128, 16, 128])
    BCb = load_cast("BCb", BC_d, [128, 4, 128])
    psc = P.sbuf("psc", [128, 2], F32)
    nl = P.sbuf("nl", [128, 128], F32)
    ic = P.sbuf("ic", [128, gw], F32)
    ncx = P.sbuf("ncx", [128, 2, 128], F32)
    for t_, d_, k_ in ((psc, ps_d, "pscale"), (nl, nl_d, "nl"), (ic, ic_d, "ic"), (ncx, ncx_d, "ncx")):
        P.dma(t_[:], d_, writes=[k_])
    Xp = P.sbuf("Xp", [128, NT, 256], BF16)
    XpT = P.sbuf("XpT", [128, 2, TT], BF16)
    yaT = P.sbuf("yaT", [128, 2, TT], BF16)
    dT = [P.sbuf(f"dT{i}", [128, 2, 128], BF16) for i in range(2)]
    tmp = P.sbuf("ptmp", [128, 128], F32)
    hb = [P.sbuf(f"hbp{i}", [128, 8, 128], BF16) for i in range(2)]
    psX = [P.psum(f"psX{i}", [128, 512]) for i in range(2)]
    psXT = [P.psum(f"psXT{i}", [128, 512]) for i in range(2)]
    psP = [P.psum(f"psP{i}", [128, 512]) for i in range(2)]
    psO = [P.psum(f"psO{i}", [128, 512]) for i in range(2)]
    for ti in range(NT):
        h = hb[ti % 2]; hk = f"hbp{ti % 2}"
        P.dma(h[:], hTc.rearrange("(c p) t -> p c t", p=128)[:, :, ti * 128:(ti + 1) * 128], writes=[hk])
        ps = psX[ti % 2]
        for kc in range(8):
            P.op("pe", lambda e, ps=ps, kc=kc, h=h: e.matmul(ps[:, 0:256], lhsT=h[:, kc, :], rhs=wpb[:, kc, :], start=(kc == 0), stop=(kc == 7)),
                 reads=[hk, "wpb"], writes=[f"psX{ti % 2}"], inc=(kc == 7))
        P.op("act", lambda e, ps=ps, ti=ti: e.copy(out=Xp[:, ti, :], in_=ps[:, 0:256]), reads=[f"psX{ti % 2}"], writes=[f"Xp{ti}"])
        ps2 = psXT[ti % 2]
        for cc in range(2):
            for kc in range(8):
                P.op("pe", lambda e, ps2=ps2, kc=kc, cc=cc, h=h: e.matmul(ps2[:, cc * 128:(cc + 1) * 128], lhsT=wpb[:, kc, cc * 128:(cc + 1) * 128],
                                                                         rhs=h[:, kc, :], start=(kc == 0), stop=(kc == 7)),
                     reads=[hk, "wpb"], writes=[f"psXT{ti % 2}"], inc=(kc == 7 and cc == 1))
        P.op("dve", lambda e, ps2=ps2, ti=ti: e.tensor_copy(out=XpT[:, :, ti * 128:(ti + 1) * 128],
                                                            in_=ps2[:, 0:256].rearrange("p (c t) -> p c t", c=2)),
             reads=[f"psXT{ti % 2}"], writes=[f"XpT{ti}"])
    for ti in range(NT):
        if ti < gw:
            srcs = [(ti + dl, BLb[:, dl + 8, :]) for dl in range(-8, 8) if 0 <= ti + dl < gw]
            nrow = nl[:]
            sc = ic[:, ti:ti + 1]
        else:
            jd = ti - gw
            srcs = [(gw + js, BCb[:, js * 2 + jd, :]) for js in range(2)]
            nrow = ncx[:, jd, :]
            sc = None
        d = dT[ti % 2]; dk = f"dT{ti % 2}"
        for cc in range(2):
            ps = psP[cc]
            for i, (src, bm) in enumerate(srcs):
                P.op("pe", lambda e, ps=ps, src=src, bm=bm, cc=cc, i=i, n=len(srcs): e.matmul(
                    ps[:, 0:128], lhsT=Xp[:, src, cc * 128:(cc + 1) * 128], rhs=bm, start=(i == 0), stop=(i == n - 1)),
                    reads=[f"Xp{src}", "BLb", "BCb"], writes=[f"psP{cc}"], inc=(i == len(srcs) - 1))
            if sc is not None:
                P.op("dve", lambda e, ps=ps, sc=sc, nrow=nrow: e.scalar_tensor_tensor(out=tmp[:], in0=ps[:, 0:128], scalar=sc, in1=nrow,
                                                                                        op0=ALU.mult, op1=ALU.mult),
                     reads=[f"psP{cc}", "ic", "nl"], writes=["ptmp"])
            else:
                P.op("dve", lambda e, ps=ps, nrow=nrow: e.tensor_tensor(out=tmp[:], in0=ps[:, 0:128], in1=nrow, op=ALU.mult),
                     reads=[f"psP{cc}", "ncx"], writes=["ptmp"])
            P.op("dve", lambda e, d=d, cc=cc, ti=ti: e.tensor_tensor(out=d[:, cc, :], in0=tmp[:], in1=XpT[:, cc, ti * 128:(ti + 1) * 128], op=ALU.subtract),
                 reads=["ptmp", f"XpT{ti}"], writes=[dk])
        for dc in range(2):
            ps = psO[dc]
            for cc in range(2):
                P.op("pe", lambda e, ps=ps, cc=cc, dc=dc, d=d: e.matmul(ps[:, 0:128], lhsT=pwb[:, cc, dc * 128:(dc + 1) * 128], rhs=d[:, cc, :],
                                                                       start=(cc == 0), stop=(cc == 1)),
                     reads=[dk, "pwb"], writes=[f"psO{dc}"], inc=(cc == 1))
            P.op("act", lambda e, ps=ps, dc=dc, ti=ti: e.activation(out=yaT[:, dc, ti * 128:(ti + 1) * 128], in_=ps[:, 0:128], func=AF.Identity,
                                                                    scale=psc[:, dc:dc + 1]),
                 reads=[f"psO{dc}", "pscale"], writes=[f"yaT{ti}"])
    toks = [P.dma(yaT_o[dc * 128:(dc + 1) * 128, :], yaT[:, dc, :], reads=[f"yaT{t}" for t in range(NT)], writes=[f"yao{dc}"]) for dc in range(2)]
    return P.finish(toks)


def fourier_consts(n2):
    n = 128 * n2
    j1 = np.arange(128)[:, None]; k1 = np.arange(128)[None, :]
    MA = np.zeros((128, n2, 256), np.float32)
    for j2 in range(n2):
        ang = -2 * np.pi * ((j1 * k1) / 128.0 + (j2 * k1) / float(n))
        MA[:, j2, :128] = np.cos(ang); MA[:, j2, 128:] = np.sin(ang)
    kl = 128 // n2
    MC = np.zeros((128, 2, 128), np.float32)
    sc = 1.0 / np.sqrt(n * 256.0)
    for a in range(kl):
        for j2 in range(n2):
            for k2 in range(n2):
                ang = -2 * np.pi * j2 * k2 / n2
                MC[a * n2 + j2, 0, a * n2 + k2] = np.cos(ang) * sc
                MC[a * n2 + j2, 1, a * n2 + k2] = -np.sin(ang) * sc
    return MA, MC


def fourier_fb():
    c = np.arange(256)[:, None]; mm = np.arange(256)[None, :]
    ang = -2 * np.pi * c * mm / 256.0
    C_, S_ = np.cos(ang), np.sin(ang)
    FB = np.zeros((128, 2, 2, 512), np.float32)
    for cc in range(2):
        sl = slice(cc * 128, (cc + 1) * 128)
        FB[:, 0, cc, :256] = C_[sl]; FB[:, 0, cc, 256:] = S_[sl]
        FB[:, 1, cc, :256] = -S_[sl]; FB[:, 1, cc, 256:] = C_[sl]
    return FB


def build_s2b(n2l, nc=None, pfx=""):
    P = Prog(nc, pfx)
    NT = n2l + 2
    TT = NT * 128
    hTc = P.dram("hTc", [D, TT], BF16, "ExternalInput")
    wf_d = P.dram("wf", [D, 256], F32, "ExternalInput")
    MAl_d = P.dram("MA_l", [128, n2l, 256], F32, "ExternalInput")
    MAc_d = P.dram("MA_c", [128, 2, 256], F32, "ExternalInput")
    MCl_d = P.dram("MC_l", [128, 2, 128], F32, "ExternalInput")
    MCc_d = P.dram("MC_c", [128, 2, 128], F32, "ExternalInput")
    FB_d = P.dram("FB", [128, 2, 2, 512], F32, "ExternalInput")
    ybT_o = P.dram("ybT_o", [256, TT], BF16, "ExternalOutput")

    stg = [P.sbuf(f"stg{i}", [128, 2048], F32) for i in range(2)]
    nst = [0]

    def load_cast(name, src_ap, shape):
        bf = P.sbuf(name, shape, BF16)
        A = shape[1]
        rest = int(np.prod(shape[2:]))
        grp = max(1, 2048 // rest)
        for a0 in range(0, A, grp):
            g_ = min(grp, A - a0)
            s = stg[nst[0] % 2]; sk = f"stg{nst[0] % 2}"; nst[0] += 1
            if len(shape) == 3:
                sv = s[:, 0:g_ * rest].rearrange("p (a b) -> p a b", a=g_)
            else:
                sv = s[:, 0:g_ * rest].rearrange("p (a b c) -> p a b c", a=g_, b=shape[2])
            P.dma(sv, src_ap[:, a0:a0 + g_], writes=[sk])
            P.op("dve", lambda e, sv=sv, a0=a0, g_=g_: e.tensor_copy(out=bf[:, a0:a0 + g_], in_=sv), reads=[sk], writes=[name])
        return bf
    wfb = load_cast("wfb", wf_d.rearrange("(c p) n -> p c n", p=128), [128, 8, 256])
    MAl = load_cast("MAl", MAl_d, [128, n2l, 256])
    MAc = load_cast("MAc", MAc_d, [128, 2, 256])
    MCl = load_cast("MCl", MCl_d, [128, 2, 128])
    MCc = load_cast("MCc", MCc_d, [128, 2, 128])
    FBb = load_cast("FBb", FB_d, [128, 2, 2, 512])
    Xf = P.sbuf("Xf", [128, NT, 256], BF16)
    Zl = P.sbuf("Zl", [128, 2, 2, 128 * n2l], BF16)
    Zc = P.sbuf("Zc", [128, 2, 2, 256], BF16)
    ybT = P.sbuf("ybT", [128, 2, TT], BF16)
    U = [P.sbuf(f"U{i}", [128, 512], BF16) for i in range(2)]
    hb = [P.sbuf(f"hbf{i}", [128, 8, 128], BF16) for i in range(2)]
    psX = [P.psum(f"psX{i}", [128, 512]) for i in range(2)]
    psA = [P.psum(f"psA{i}", [128, 512]) for i in range(2)]
    psB = [P.psum(f"psB{i}", [128, 512]) for i in range(2)]
    psC = [P.psum(f"psC{i}", [128, 512]) for i in range(2)]
    for ti in range(NT):
        h = hb[ti % 2]; hk = f"hbf{ti % 2}"
        P.dma(h[:], hTc.rearrange("(c p) t -> p c t", p=128)[:, :, ti * 128:(ti + 1) * 128], writes=[hk])
        ps = psX[ti % 2]
        for kc in range(8):
            P.op("pe", lambda e, ps=ps, kc=kc, h=h: e.matmul(ps[:, 0:256], lhsT=h[:, kc, :], rhs=wfb[:, kc, :], start=(kc == 0), stop=(kc == 7)),
                 reads=[hk, "wfb"], writes=[f"psX{ti % 2}"], inc=(kc == 7))
        P.op("act", lambda e, ps=ps, ti=ti: e.copy(out=Xf[:, ti, :], in_=ps[:, 0:256]), reads=[f"psX{ti % 2}"], writes=[f"Xf{ti}"])
        lat = ti < n2l
        j2 = ti if lat else ti - n2l
        n2 = n2l if lat else 2
        ma = MAl[:, j2, :] if lat else MAc[:, j2, :]
        Z = Zl if lat else Zc
        zk = "Zl" if lat else "Zc"
        for cc in range(2):
            pa = psA[cc]
            P.op("pe", lambda e, pa=pa, cc=cc, ti=ti, ma=ma: e.matmul(pa[:, 0:256], lhsT=Xf[:, ti, cc * 128:(cc + 1) * 128], rhs=ma, start=True, stop=True),
                 reads=[f"Xf{ti}", "MAl", "MAc"], writes=[f"psA{cc}"])
            dst = Z[:, cc, :, :].rearrange("p r (k j) -> p r k j", j=n2)[:, :, :, j2]
            eng = "dve" if cc == 0 else "act"
            if eng == "dve":
                P.op("dve", lambda e, pa=pa, dst=dst: e.tensor_copy(out=dst, in_=pa[:, 0:256].rearrange("p (r k) -> p r k", r=2)),
                     reads=[f"psA{cc}"], writes=[zk])
            else:
                P.op("act", lambda e, pa=pa, dst=dst: e.copy(out=dst, in_=pa[:, 0:256].rearrange("p (r k) -> p r k", r=2)),
                     reads=[f"psA{cc}"], writes=[zk])
    qi = 0
    for seg in range(2):
        lat = seg == 0
        n2 = n2l if lat else 2
        Z = Zl if lat else Zc
        zk = "Zl" if lat else "Zc"
        MC = MCl if lat else MCc
        kl = 128 // n2
        base = 0 if lat else n2l * 128
        for q in range(n2):
            pb = psB[qi % 2]; u = U[qi % 2]; uk = f"U{qi % 2}"
            i = 0
            for part in range(2):
                for cc in range(2):
                    P.op("pe", lambda e, pb=pb, part=part, cc=cc, q=q, i=i, Z=Z: e.matmul(
                        pb[:, :], lhsT=Z[:, cc, part, q * 128:(q + 1) * 128], rhs=FBb[:, part, cc, :], start=(i == 0), stop=(i == 3)),
                        reads=[zk, "FBb"], writes=[f"psB{qi % 2}"], inc=(i == 3))
                    i += 1
            P.op("act", lambda e, pb=pb, u=u: e.copy(out=u[:], in_=pb[:, :]), reads=[f"psB{qi % 2}"], writes=[uk])
            for mc in range(2):
                pc = psC[mc]
                P.op("pe", lambda e, pc=pc, u=u, mc=mc, MC=MC: e.matmul(pc[:, 0:128], lhsT=u[:, mc * 128:(mc + 1) * 128], rhs=MC[:, 0, :], start=True, stop=False),
                     reads=[uk, "MCl", "MCc"], writes=[f"psC{mc}"], inc=False)
                P.op("pe", lambda e, pc=pc, u=u, mc=mc, MC=MC: e.matmul(pc[:, 0:128], lhsT=u[:, 256 + mc * 128:256 + (mc + 1) * 128], rhs=MC[:, 1, :], start=False, stop=True),
                     reads=[uk, "MCl", "MCc"], writes=[f"psC{mc}"])
                dst = ybT[:, mc, base:base + 128 * n2].rearrange("p (k2 k1) -> p k1 k2", k1=128)[:, q * kl:(q + 1) * kl, :]
                P.op("dve", lambda e, pc=pc, dst=dst, n2=n2: e.tensor_copy(out=dst, in_=pc[:, 0:128].rearrange("p (a b) -> p a b", b=n2)),
                     reads=[f"psC{mc}"], writes=[f"ybT{seg}_{q}"])
            qi += 1
    allk = [f"ybT0_{q}" for q in range(n2l)] + [f"ybT1_{q}" for q in range(2)]
    toks = [P.dma(ybT_o[mc * 128:(mc + 1) * 128, :], ybT[:, mc, :], reads=allk, writes=[f"ybo{mc}"]) for mc in range(2)]
    return P.finish(toks)


def build_s3():
    P = Prog()
    lat_d = P.dram("lat", [TS_T, D], F32, "ExternalInput")
    hT_d = P.dram("hT", [D, TS_T], BF16, "ExternalInput")
    yT_d = P.dram("yT", [3, D, TS_T], BF16, "ExternalInput")
    ssq_d = P.dram("ssq4", [4, TS_T], F32, "ExternalInput")
    wg_d = P.dram("wg", [D, 3 * D], F32, "ExternalInput")
    wb_d = P.dram("wb", [3, D, D], F32, "ExternalInput")
    wo_d = P.dram("wo", [D, D], F32, "ExternalInput")
    g1_d = P.dram("g1b", [2, 128, D], F32, "ExternalInput")
    nwT = P.dram("nwT", [128, 8], F32, "ExternalInput")
    scT = P.dram("scT", [128, 8, 2], F32, "ExternalInput")
    shT = P.dram("shT", [128, 8, 2], F32, "ExternalInput")
    rw_d = P.dram("rw", [D, 32], F32, "ExternalInput")
    rb_d = P.dram("rbb", [128, 32], F32, "ExternalInput")
    id_d = P.dram("ident", [128, 128], F32, "ExternalInput")
    on_d = P.dram("ones4", [4, 128], F32, "ExternalInput")
    lat_o = P.dram("lat_o", [TS_T, D], F32, "ExternalOutput")
    h2T_o = P.dram("h2T_o", [D, TS_T], BF16, "ExternalOutput")
    wgt_o = P.dram("wgt_o", [TS_T, 32], F32, "ExternalOutput")

    stg = [P.sbuf(f"stg{i}", [128, 2048], F32) for i in range(2)]
    nst = [0]

    def cast_in(bf_view_fn, src_fn, n_a, rest, name):
        grp = max(1, 2048 // rest)
        for a0 in range(0, n_a, grp):
            g_ = min(grp, n_a - a0)
            s = stg[nst[0] % 2]; sk = f"stg{nst[0] % 2}"
            sv = s[:, 0:g_ * rest].rearrange("p (a b) -> p a b", a=g_)
            P.dma(sv, src_fn(a0, g_), writes=[sk])
            eng = "dve" if nst[0] % 2 == 0 else "act"
            if eng == "dve":
                P.op("dve", lambda e, sv=sv, a0=a0, g_=g_: e.tensor_copy(out=bf_view_fn(a0, g_), in_=sv), reads=[sk], writes=[name])
            else:
                P.op("act", lambda e, sv=sv, a0=a0, g_=g_: e.copy(out=bf_view_fn(a0, g_), in_=sv), reads=[sk], writes=[name])
            nst[0] += 1
    wgb = P.sbuf("wgb", [128, 8, 3 * D], BF16)
    wbb = P.sbuf("wbb", [128, 3, 8, D], BF16)
    wob = P.sbuf("wob", [128, 8, D], BF16)
    wgv = wg_d.rearrange("(c p) n -> p c n", p=128)
    for k in range(3):
        cast_in(lambda a0, g, k=k: wgb[:, a0:a0 + g, k * D:(k + 1) * D], lambda a0, g, k=k: wgv[:, a0:a0 + g, k * D:(k + 1) * D], 8, D, "wgb")
    for k in range(3):
        wbv = wb_d[k].rearrange("(c p) n -> p c n", p=128)
        cast_in(lambda a0, g, k=k: wbb[:, k, a0:a0 + g, :], lambda a0, g, wbv=wbv: wbv[:, a0:a0 + g, :], 8, D, "wbb")
    wov = wo_d.rearrange("(c p) n -> p c n", p=128)
    cast_in(lambda a0, g: wob[:, a0:a0 + g, :], lambda a0, g: wov[:, a0:a0 + g, :], 8, D, "wob")
    ident = P.sbuf("ident_s", [128, 128], F32)
    P.dma(ident[:], id_d, writes=["ident"])
    ones4 = P.sbuf("ones4s", [4, 128], F32)
    P.dma(ones4[:], on_d, writes=["ones4"])
    modA, modB = load_modAB(P, nwT, scT, shT)
    g1 = P.sbuf("g1", [128, 2, D], F32)
    P.dma(g1[:], g1_d.rearrange("w p d -> p w d"), writes=["g1"])
    rw = P.sbuf("rws", [128, 8, 32], F32)
    P.dma(rw[:], rw_d.rearrange("(c p) n -> p c n", p=128), writes=["rw"])
    rb = P.sbuf("rbs", [128, 32], F32)
    P.dma(rb[:], rb_d, writes=["rb"])

    hTt = [P.sbuf(f"hTt{i}", [128, 8, 128], BF16) for i in range(2)]
    yTt = [P.sbuf(f"yTt{i}", [128, 3, 8, 128], BF16) for i in range(2)]
    latt = [P.sbuf(f"latt{i}", [128, D], F32) for i in range(2)]
    ssqt = P.sbuf("ssqt", [4, 128], F32)
    rstdb = P.sbuf("rstdb", [128, 128], F32)
    sg = [P.sbuf(f"sg{i}", [128, 128], F32) for i in range(2)]
    term = [P.sbuf(f"term{i}", [128, 128], F32) for i in range(3)]
    mT = P.sbuf("mT", [128, 8, 128], BF16)
    tmpo = P.sbuf("tmpo", [128, 512], F32)
    h2f = P.sbuf("h2f", [128, 8, 128], F32)
    h2b = [P.sbuf(f"h2b{i}", [128, 8, 128], BF16) for i in range(2)]
    wgt = P.sbuf("wgt", [128, TS_NT, 32], F32)
    lg = P.sbuf("lg", [128, 32], F32)
    m8 = P.sbuf("m8", [128, 8], F32)
    nmax = P.sbuf("nmax", [128, 1], F32)
    msk = P.sbuf("msk", [128, 32], F32)
    ex = P.sbuf("ex", [128, 32], F32)
    ssum = P.sbuf("ssum", [128, 1], F32)
    scr = (P.sbuf("junk", [128, D], BF16), P.sbuf("ssq", [128, 1], F32), P.sbuf("rstd", [128, 1], F32),
           P.sbuf("xn", [128, D], F32))
    psG = [P.psum(f"psG{i}", [128, 512]) for i in range(2)]
    psPj = [P.psum(f"psPj{i}", [128, 512]) for i in range(2)]
    psO = P.psum("psO", [128, 512])
    psR = P.psum("psR", [128, 512])
    ps_pair = [P.psum(f"psT{i}", [128, 512]) for i in range(2)]
    toks = []
    hv = hT_d.rearrange("(c p) t -> p c t", p=128)
    yv = yT_d.rearrange("k (c p) t -> p k c t", p=128)
    n = 0
    for t in range(TS_NT):
        which = 1 if t == TS_NT - 1 else 0
        ts_ = slice(t * 128, (t + 1) * 128)
        ht = hTt[t % 2]; hk = f"hTt{t % 2}"
        yt = yTt[t % 2]; yk = f"yTt{t % 2}"
        lt = latt[t % 2]; lk = f"latt{t % 2}"
        P.dma(ht[:], hv[:, :, ts_], writes=[hk])
        for k in range(3):
            P.dma(yt[:, k], yv[:, k, :, ts_], writes=[yk])
        P.dma(lt[:], lat_d[ts_, :], writes=[lk])
        P.dma(ssqt[:], ssq_d[:, ts_], writes=["ssqt"])
        P.op("pe", lambda e: e.matmul(psR[:, 0:128], lhsT=ones4[:], rhs=ssqt[:], start=True, stop=True),
             reads=["ones4", "ssqt"], writes=["psR"])
        P.op("act", lambda e: e.activation(out=rstdb[:], in_=psR[:, 0:128], func=AF.Sqrt, scale=1.0 / D, bias=EPS),
             reads=["psR"], writes=["rstdb"])
        P.op("dve", lambda e: e.reciprocal(out=rstdb[:], in_=rstdb[:]), reads=["rstdb"], writes=["rstdb"])
        for dc in range(8):
            for k in range(3):
                pg = psG[n % 2]; pp = psPj[n % 2]; s_ = sg[n % 2]
                for kc in range(8):
                    P.op("pe", lambda e, pg=pg, kc=kc, k=k, dc=dc, ht=ht: e.matmul(
                        pg[:, 0:128], lhsT=wgb[:, kc, k * D + dc * 128:k * D + (dc + 1) * 128], rhs=ht[:, kc, :], start=(kc == 0), stop=(kc == 7)),
                        reads=[hk, "wgb"], writes=[f"psG{n % 2}"], inc=(kc == 7))
                P.op("act", lambda e, pg=pg, s_=s_: e.activation(out=s_[:], in_=pg[:, 0:128], func=AF.Sigmoid),
                     reads=[f"psG{n % 2}"], writes=[f"sg{n % 2}"])
                for wc in range(8):
                    P.op("pe", lambda e, pp=pp, wc=wc, k=k, dc=dc, yt=yt: e.matmul(
                        pp[:, 0:128], lhsT=wbb[:, k, wc, dc * 128:(dc + 1) * 128], rhs=yt[:, k, wc, :], start=(wc == 0), stop=(wc == 7)),
                        reads=[yk, "wbb"], writes=[f"psPj{n % 2}"], inc=(wc == 7))
                P.op("dve", lambda e, pp=pp, s_=s_, k=k: e.tensor_tensor(out=term[k][:], in0=pp[:, 0:128], in1=s_[:], op=ALU.mult),
                     reads=[f"psPj{n % 2}", f"sg{n % 2}"], writes=[f"term{k}"])
                n += 1
            P.op("pool", lambda e: e.tensor_tensor(out=term[2][:], in0=term[2][:], in1=rstdb[:], op=ALU.mult),
                 reads=["term2", "rstdb"], writes=["term2"])
            P.op("pool", lambda e: e.tensor_tensor(out=term[0][:], in0=term[0][:], in1=term[1][:], op=ALU.add),
                 reads=["term0", "term1"], writes=["term0"])
            P.op("pool", lambda e, dc=dc: e.tensor_tensor(out=mT[:, dc, :], in0=term[0][:], in1=term[2][:], op=ALU.add),
                 reads=["term0", "term2"], writes=[f"mT{dc}"])
        for half in range(2):
            for dc in range(8):
                P.op("pe", lambda e, dc=dc, half=half: e.matmul(psO[:, :], lhsT=mT[:, dc, :], rhs=wob[:, dc, half * 512:(half + 1) * 512],
                                                               start=(dc == 0), stop=(dc == 7)),
                     reads=[f"mT{dc}", "wob"], writes=["psO"], inc=(dc == 7))
            P.op("dve", lambda e, half=half, which=which: e.tensor_tensor(out=tmpo[:], in0=psO[:, :], in1=g1[:, which, half * 512:(half + 1) * 512], op=ALU.mult),
                 reads=["psO", "g1"], writes=["tmpo"])
            P.op("pool", lambda e, half=half, lt=lt: e.tensor_tensor(out=lt[:, half * 512:(half + 1) * 512], in0=lt[:, half * 512:(half + 1) * 512], in1=tmpo[:], op=ALU.add),
                 reads=["tmpo", lk], writes=[lk])
        toks.append(P.dma(lat_o[ts_, :], lt[:], reads=[lk], writes=[f"lato{t}"]))
        emit_norm_T(P, lt[:], lk, modA, modB, which, h2f, "h2f", ident, ps_pair, t, scr)
        hb_ = h2b[t % 2]
        P.op("pool", lambda e, hb_=hb_: e.tensor_copy(out=hb_[:], in_=h2f[:]), reads=[f"h2f_{c}" for c in range(8)], writes=[f"h2b{t % 2}"])
        toks.append(P.dma(h2T_o.rearrange("(c p) t -> p c t", p=128)[:, :, ts_], hb_[:], reads=[f"h2b{t % 2}"], writes=[f"h2o{t}"]))
        for kc in range(8):
            P.op("pe", lambda e, kc=kc: e.matmul(psR[:, 128:160], lhsT=h2f[:, kc, :], rhs=rw[:, kc, :], start=(kc == 0), stop=(kc == 7)),
                 reads=[f"h2f_{kc}", "rw"], writes=["psR"], inc=(kc == 7))
        P.op("dve", lambda e: e.tensor_tensor(out=lg[:], in0=psR[:, 128:160], in1=rb[:], op=ALU.add), reads=["psR", "rb"], writes=["lg"])
        P.op("dve", lambda e: e.max(out=m8[:], in_=lg[:]), reads=["lg"], writes=["m8"])
        P.op("dve", lambda e: e.tensor_scalar(out=msk[:], in0=lg[:], scalar1=m8[:, 3:4], scalar2=None, op0=ALU.is_ge), reads=["lg", "m8"], writes=["msk"])
        P.op("dve", lambda e: e.tensor_scalar(out=nmax[:], in0=m8[:, 0:1], scalar1=-1.0, scalar2=None, op0=ALU.mult), reads=["m8"], writes=["nmax"])
        P.op("act", lambda e: e.activation(out=ex[:], in_=lg[:], func=AF.Exp, bias=nmax[:, 0:1]), reads=["lg", "nmax"], writes=["ex"])
        P.op("dve", lambda e: e.tensor_tensor(out=ex[:], in0=ex[:], in1=msk[:], op=ALU.mult), reads=["ex", "msk"], writes=["ex"])
        P.op("dve", lambda e: e.reduce_sum(out=ssum[:], in_=ex[:], axis=AX.X), reads=["ex"], writes=["ssum"])
        P.op("dve", lambda e: e.reciprocal(out=ssum[:], in_=ssum[:]), reads=["ssum"], writes=["ssum"])
        P.op("dve", lambda e, t=t: e.tensor_scalar(out=wgt[:, t, :], in0=ex[:], scalar1=ssum[:, 0:1], scalar2=None, op0=ALU.mult),
             reads=["ex", "ssum"], writes=[f"wgt{t}"])
    toks.append(P.dma(wgt_o.rearrange("(t p) e -> p t e", p=128), wgt[:], reads=[f"wgt{t}" for t in range(TS_NT)], writes=["wgto"]))
    return P.finish(toks)


SW_ALPHA = 1.702
SW_LIMIT = 7.0
NTOK_ALL = NCORE * TS_T


def build_s4(ntok):
    P = Prog()
    NBLK = ntok // 1024
    h2T_d = P.dram("h2T", [D, ntok], BF16, "ExternalInput")
    ws_d = P.dram("wsel", [128, ntok // 128, 4], F32, "ExternalInput")
    w1_d = P.dram("w1", [4, D, 2 * D], F32, "ExternalInput")
    b1_d = P.dram("b1T", [128, 4, 16], F32, "ExternalInput")
    w2_d = P.dram("w2", [4, D, D], F32, "ExternalInput")
    b2_d = P.dram("b2", [1, 4, D], F32, "ExternalInput")
    part_o = P.dram("part_o", [ntok, D], BF16, "ExternalOutput")
    w1s = P.nc.dram_tensor("w1s", [4, D, 2 * D], BF16).ap()
    w2s = P.nc.dram_tensor("w2s", [4, D, D], BF16).ap()

    w1b = [P.sbuf(f"w1b{i}", [128, 8, 2 * D], BF16) for i in range(2)]
    w2b = [P.sbuf(f"w2b{i}", [128, 8, D], BF16) for i in range(2)]
    for e in range(4):
        P.dma(w1b[e % 2][:], w1_d[e].rearrange("(c p) n -> p c n", p=128), writes=[f"w1b{e % 2}"], eng="pool")
        P.dma(w1s[e].rearrange("(c p) n -> p c n", p=128), w1b[e % 2][:], reads=[f"w1b{e % 2}"], writes=[f"w1s{e}"])
        P.dma(w2b[e % 2][:], w2_d[e].rearrange("(c p) n -> p c n", p=128), writes=[f"w2b{e % 2}"], eng="pool")
        P.dma(w2s[e].rearrange("(c p) n -> p c n", p=128), w2b[e % 2][:], reads=[f"w2b{e % 2}"], writes=[f"w2s{e}"])
    b1 = P.sbuf("b1s", [128, 4, 16], F32)
    P.dma(b1[:], b1_d, writes=["b1"])
    b2b = P.sbuf("b2b", [1, 4, D], BF16)
    P.dma(b2b[:], b2_d, writes=["b2b"], eng="pool")
    onesb = P.sbuf("onesb", [1, 128], BF16)
    P.op("dve", lambda e: e.memset(onesb[:], 1.0), writes=["onesb"])
    ws = P.sbuf("wss", [128, ntok // 128, 4], F32)
    P.dma(ws[:], ws_d, writes=["ws"])

    hblk = [P.sbuf(f"hblk{i}", [128, 8, 1024], BF16) for i in range(2)]
    acc = P.sbuf("acc", [128, 8, D], F32)
    accb = P.sbuf("accb", [128, 2, D], BF16)
    actT = P.sbuf("actT", [128, 8, 1024], BF16)
    gsb = [P.sbuf(f"gsb{i}", [128, 512], F32) for i in range(2)]
    sgb = [P.sbuf(f"sgb{i}", [128, 512], F32) for i in range(2)]
    lsb = [P.sbuf(f"lsb{i}", [128, 512], F32) for i in range(2)]
    psGt = [P.psum(f"psGt{i}", [128, 512]) for i in range(2)]
    psLn = [P.psum(f"psLn{i}", [128, 512]) for i in range(2)]
    psDn = [P.psum(f"psDn{i}", [128, 512]) for i in range(2)]
    toks = []
    n = 0
    nd = 0
    wi = 0
    for blk in range(NBLK):
        hb = hblk[blk % 2]; hk = f"hblk{blk % 2}"
        P.dma(hb[:], h2T_d.rearrange("(c p) t -> p c t", p=128)[:, :, blk * 1024:(blk + 1) * 1024], writes=[hk])
        for e in range(4):
            wa = w1b[wi % 2]; wb_ = w2b[wi % 2]; k1 = f"w1b{wi % 2}"; k2 = f"w2b{wi % 2}"
            wi += 1
            P.dma(wa[:], w1s[e].rearrange("(c p) n -> p c n", p=128), reads=[f"w1s{e}"], writes=[k1])
            P.dma(wb_[:], w2s[e].rearrange("(c p) n -> p c n", p=128), reads=[f"w2s{e}"], writes=[k2])
            for half in range(2):
                hs = slice(half * 512, (half + 1) * 512)
                for fc in range(8):
                    pg = psGt[n % 2]; pl = psLn[n % 2]; g_ = gsb[n % 2]; s_ = sgb[n % 2]; l_ = lsb[n % 2]
                    i2 = n % 2
                    for kc in range(8):
                        P.op("pe", lambda e_, pg=pg, kc=kc, fc=fc, wa=wa, hb=hb, hs=hs: e_.matmul(
                            pg[:, :], lhsT=wa[:, kc, fc * 128:(fc + 1) * 128], rhs=hb[:, kc, hs], start=(kc == 0), stop=(kc == 7)),
                            reads=[hk, k1], writes=[f"psGt{i2}"], inc=(kc == 7))
                    for kc in range(8):
                        P.op("pe", lambda e_, pl=pl, kc=kc, fc=fc, wa=wa, hb=hb, hs=hs: e_.matmul(
                            pl[:, :], lhsT=wa[:, kc, D + fc * 128:D + (fc + 1) * 128], rhs=hb[:, kc, hs], start=(kc == 0), stop=(kc == 7)),
                            reads=[hk, k1], writes=[f"psLn{i2}"], inc=(kc == 7))
                    P.op("dve", lambda e_, pg=pg, g_=g_, e=e, fc=fc: e_.tensor_scalar(out=g_[:], in0=pg[:, :], scalar1=b1[:, e, fc:fc + 1], scalar2=SW_LIMIT,
                                                                                    op0=ALU.add, op1=ALU.min),
                         reads=[f"psGt{i2}", "b1"], writes=[f"gsb{i2}"])
                    P.op("act", lambda e_, g_=g_, s_=s_: e_.activation(out=s_[:], in_=g_[:], func=AF.Sigmoid, scale=SW_ALPHA),
                         reads=[f"gsb{i2}"], writes=[f"sgb{i2}"])
                    P.op("dve", lambda e_, pl=pl, l_=l_, e=e, fc=fc: e_.tensor_scalar(out=l_[:], in0=pl[:, :], scalar1=b1[:, e, 8 + fc:9 + fc], scalar2=SW_LIMIT,
                                                                                    op0=ALU.add, op1=ALU.min),
                         reads=[f"psLn{i2}", "b1"], writes=[f"lsb{i2}"])
                    P.op("dve", lambda e_, l_=l_: e_.tensor_scalar(out=l_[:], in0=l_[:], scalar1=-SW_LIMIT, scalar2=1.0, op0=ALU.max, op1=ALU.add),
                         reads=[f"lsb{i2}"], writes=[f"lsb{i2}"])
                    P.op("pool", lambda e_, g_=g_, s_=s_: e_.tensor_tensor(out=g_[:], in0=g_[:], in1=s_[:], op=ALU.mult),
                         reads=[f"gsb{i2}", f"sgb{i2}"], writes=[f"gsb{i2}"])
                    P.op("dve", lambda e_, g_=g_, l_=l_, fc=fc, hs=hs: e_.tensor_tensor(out=actT[:, fc, hs], in0=g_[:], in1=l_[:], op=ALU.mult),
                         reads=[f"gsb{i2}", f"lsb{i2}"], writes=[f"actT{fc}_{half}"])
                    n += 1
            for tl in range(8):
                half = tl // 4
                for dh in range(2):
                    pd = psDn[nd % 2]; i3 = nd % 2
                    for fc in range(8):
                        P.op("pe", lambda e_, pd=pd, fc=fc, tl=tl, dh=dh, wb_=wb_: e_.matmul(
                            pd[:, :], lhsT=actT[:, fc, tl * 128:(tl + 1) * 128], rhs=wb_[:, fc, dh * 512:(dh + 1) * 512], start=(fc == 0), stop=False),
                            reads=[f"actT{fc}_{half}", k2], writes=[f"psDn{i3}"], inc=False)
                    P.op("pe", lambda e_, pd=pd, e=e, dh=dh: e_.matmul(pd[:, :], lhsT=onesb[:], rhs=b2b[:, e, dh * 512:(dh + 1) * 512], start=False, stop=True),
                         reads=["onesb", "b2b"], writes=[f"psDn{i3}"])
                    wcol = ws[:, blk * 8 + tl, e:e + 1]
                    av = acc[:, tl, dh * 512:(dh + 1) * 512]
                    if e == 0:
                        P.op("dve", lambda e_, pd=pd, wcol=wcol, av=av: e_.tensor_scalar(out=av, in0=pd[:, :], scalar1=wcol, scalar2=None, op0=ALU.mult),
                             reads=[f"psDn{i3}", "ws"], writes=[f"acc{tl}"])
                    else:
                        P.op("dve", lambda e_, pd=pd, wcol=wcol, av=av: e_.scalar_tensor_tensor(out=av, in0=pd[:, :], scalar=wcol, in1=av, op0=ALU.mult, op1=ALU.add),
                             reads=[f"psDn{i3}", "ws", f"acc{tl}"], writes=[f"acc{tl}"])
                    nd += 1
        for tl in range(8):
            ab = accb[:, tl % 2, :]
            if tl % 2 == 0:
                P.op("act", lambda e_, tl=tl, ab=ab: e_.copy(out=ab, in_=acc[:, tl, :]), reads=[f"acc{tl}"], writes=[f"accb{tl % 2}"])
            else:
                P.op("pool", lambda e_, tl=tl, ab=ab: e_.tensor_copy(out=ab, in_=acc[:, tl, :]), reads=[f"acc{tl}"], writes=[f"accb{tl % 2}"])
            toks.append(P.dma(part_o[blk * 1024 + tl * 128:blk * 1024 + (tl + 1) * 128, :], ab,
                              reads=[f"accb{tl % 2}"], writes=[f"parto{blk}_{tl}"]))
    return P.finish(toks)


OFF_FOURIER = 1024
OFF_Z = 2048
OFF_XBC = 3072
OFF_DT = OFF_XBC + 2048
OFF_GATE = OFF_DT + 32
DEBUG = {}
_PROGS = {}


def _prog(name, fn):
    return fn()


def fT(v):
    return np.ascontiguousarray(np.asarray(v, np.float32).reshape(8, 128).T)


def pack_ts(lat, cx):
    out = np.zeros((NCORE, TS_T) + lat.shape[2:], lat.dtype)
    for b in range(B):
        for q in range(4):
            out[b * 4 + q, :2048] = lat[b, q * 2048:(q + 1) * 2048]
            out[b * 4 + q, 2048:2112] = cx[b, q * 64:(q + 1) * 64]
    return out


def unpack_ts(ts):
    lat = np.zeros((B, SEQ) + ts.shape[2:], ts.dtype)
    cx = np.zeros((B, CTX) + ts.shape[2:], ts.dtype)
    for b in range(B):
        for q in range(4):
            lat[b, q * 2048:(q + 1) * 2048] = ts[b * 4 + q, :2048]
            cx[b, q * 64:(q + 1) * 64] = ts[b * 4 + q, 2048:2112]
    return lat, cx


def pad_blocks(hT, segs, nb):
    out = np.zeros((hT.shape[0], nb, 260), hT.dtype)
    for bk in range(nb):
        s0 = bk * 256
        seg = [s_ for s_ in segs if s_[0] <= s0 < s_[1]][0]
        lo, hi = max(seg[0], s0 - 2), min(seg[1], s0 + 258)
        out[:, bk, (lo - (s0 - 2)):(hi - (s0 - 2))] = hT[:, lo:hi]
    return out


def mod_pair(mod, l, b, j):
    return np.stack([fT(mod[l, b, j]), fT(mod[l, 2, j])], -1)


def run_s1(lat_ts, mod, l, norm_w, parts=None, g2_layer=None, final_w=None):
    ident = np.eye(128, dtype=np.float32)
    fw = np.broadcast_to(np.asarray(final_w if final_w is not None else np.ones(D), np.float32), (128, D)).copy()
    maps = []
    for core in range(NCORE):
        b = core // 4
        m = {"xin": lat_ts[core], "nwT": fT(norm_w), "scT": mod_pair(mod, l, b, 1), "shT": mod_pair(mod, l, b, 0),
             "ident": ident, "fwb": fw}
        if parts is not None:
            m["part"] = parts[core]
            m["g2b"] = np.stack([np.broadcast_to(mod[g2_layer, b, 5], (128, D)), np.broadcast_to(mod[g2_layer, 2, 5], (128, D))]).astype(np.float32)
        maps.append(m)
    res = run_spmd(build_s1(parts is not None), maps)
    hT = np.stack([r["hT_o"] for r in res])
    if parts is not None:
        return hT, np.stack([r["lat_o"] for r in res]), np.stack([r["fin_o"] for r in res])
    return hT, lat_ts, None


def build_s2(gw, nlat):
    nc = build_s2a(gw, None, "a_")
    nc = build_s2b(gw, nc, "b_")
    nc = build_s2c(nlat, nc, "c_")
    return nc


def run_s2(hT_ts, l, inp):
    h_lat, h_ctx = unpack_ts(np.ascontiguousarray(hT_ts.transpose(0, 2, 1)))
    w_in = inp["w_in"][l]
    c6, m4 = ssd_consts()
    MAl, MCl = fourier_consts(64)
    MAc, MCc = fourier_consts(2)
    FB = fourier_fb()
    mapsa, mapsb, mapsc = [], [], []
    for core in range(NCORE):
        b, g = core // 4, core % 4
        lat_c = h_lat[b].reshape(128, 64, D).transpose(1, 0, 2).reshape(SEQ, D)
        ctx_c = h_ctx[b].reshape(128, 2, D).transpose(1, 0, 2).reshape(CTX, D)
        hTc = np.ascontiguousarray(np.concatenate([lat_c, ctx_c], 0).T)
        ma = {"hTc": hTc, "wp": np.ascontiguousarray(w_in[:, g * 256:(g + 1) * 256]),
              "pw": np.ascontiguousarray(inp["pool_w"][l, g]), "pscT": np.ascontiguousarray(inp["pool_scale"][l, g * 256:(g + 1) * 256].reshape(2, 128).T)}
        ma.update(pool_consts(POOL_WINDOWS[g], 64))
        mapsa.append(ma)
        mapsb.append({"hTc": hTc, "wf": np.ascontiguousarray(w_in[:, OFF_FOURIER + g * 256:OFF_FOURIER + (g + 1) * 256]),
                      "MA_l": MAl, "MA_c": MAc, "MC_l": MCl, "MC_c": MCc, "FB": FB})
        hT_cl = np.ascontiguousarray(np.concatenate([h_ctx[b], h_lat[b]], 0).T)
        xcols = np.r_[np.arange(g * 256, (g + 1) * 256), 1024 + np.arange(g * 128, (g + 1) * 128), 1536 + np.arange(g * 128, (g + 1) * 128)]
        dcols = np.r_[4 * g + np.arange(4), 16 + 4 * g + np.arange(4)]
        wdt = w_in[:, OFF_DT + dcols]
        wdt40 = np.zeros((D, 40), np.float32); wdt40[:, 0:8] = wdt; wdt40[:, 32:40] = wdt
        dtb = inp["dt_bias"][l].reshape(32)[dcols]
        dtb40 = np.zeros((40, 1), np.float32); dtb40[0:8, 0] = dtb; dtb40[32:40, 0] = dtb
        alog40 = np.zeros((40, 1), np.float32); alog40[32:40, 0] = inp["a_log"][l].reshape(32)[dcols]
        mapsc.append({"hTp": pad_blocks(hT_cl, [(0, CTX), (CTX, CTX + SEQ)], (CTX + SEQ) // 256),
                      "wxbc": np.ascontiguousarray(w_in[:, OFF_XBC + xcols]), "wz": np.ascontiguousarray(w_in[:, OFF_Z + g * 256:OFF_Z + (g + 1) * 256]),
                      "wdt40": wdt40, "convwT": np.ascontiguousarray(inp["conv_w"][l][:, xcols].reshape(5, 4, 128).transpose(2, 1, 0)),
                      "convbT": np.ascontiguousarray(inp["conv_b"][l][xcols].reshape(4, 128).T), "dtb40": dtb40, "alog40": alog40,
                      "dskb": np.broadcast_to(inp["d_skip"][l][None, 4 * g:4 * g + 4], (128, 4)).copy(),
                      "snwT": np.ascontiguousarray(inp["ssm_norm_w"][l][g * 256:(g + 1) * 256].reshape(2, 128).T), "c6": c6, "m4": m4})
    ra = rb = rc = run_spmd(build_s2(64, SEQ), [dict(list(a.items()) + list(b_.items()) + list(c_.items())) for a, b_, c_ in zip(mapsa, mapsb, mapsc)])
    y_lat = np.zeros((B, 3, SEQ, D), NBF); y_ctx = np.zeros((B, 3, CTX, D), NBF)
    ssq_lat = np.zeros((B, 4, SEQ), np.float32); ssq_ctx = np.zeros((B, 4, CTX), np.float32)
    for core in range(NCORE):
        b, g = core // 4, core % 4
        cs = slice(g * 256, (g + 1) * 256)
        ya = ra[core]["yaT_o"].T
        y_lat[b, 0, :, cs] = ya[:SEQ].reshape(64, 128, 256).transpose(1, 0, 2).reshape(SEQ, 256)
        y_ctx[b, 0, :, cs] = ya[SEQ:].reshape(2, 128, 256).transpose(1, 0, 2).reshape(CTX, 256)
        yb = rb[core]["ybT_o"].T
        y_lat[b, 1, :, cs] = yb[:SEQ]; y_ctx[b, 1, :, cs] = yb[SEQ:]
        yc = rc[core]["ycT_o"].T
        y_ctx[b, 2, :, cs] = yc[:CTX]; y_lat[b, 2, :, cs] = yc[CTX:]
        sq = rc[core]["ssq_o"][0]
        ssq_ctx[b, g] = sq[:CTX]; ssq_lat[b, g] = sq[CTX:]
    DEBUG[f"L{l}_y_lat"] = y_lat; DEBUG[f"L{l}_y_ctx"] = y_ctx; DEBUG[f"L{l}_ssq_lat"] = ssq_lat
    yT_ts = np.zeros((NCORE, 3, D, TS_T), NBF)
    ssq_ts = np.zeros((NCORE, 4, TS_T), np.float32)
    for core in range(NCORE):
        b, q = core // 4, core % 4
        for k in range(3):
            yT_ts[core, k, :, :2048] = y_lat[b, k, q * 2048:(q + 1) * 2048].T
            yT_ts[core, k, :, 2048:2112] = y_ctx[b, k, q * 64:(q + 1) * 64].T
        ssq_ts[core, :, :2048] = ssq_lat[b, :, q * 2048:(q + 1) * 2048]
        ssq_ts[core, :, 2048:2112] = ssq_ctx[b, :, q * 64:(q + 1) * 64]
    return yT_ts, ssq_ts


def run_s3(lat_ts, hT_ts, yT_ts, ssq_ts, mod, l, inp):
    ident = np.eye(128, dtype=np.float32)
    ones4 = np.ones((4, 128), np.float32)
    wg = np.ascontiguousarray(inp["w_in"][l][:, OFF_GATE:])
    rbb = np.broadcast_to(inp["router_b"][l], (128, 32)).astype(np.float32).copy()
    maps = []
    for core in range(NCORE):
        b = core // 4
        g1b = np.stack([np.broadcast_to(mod[l, b, 2], (128, D)), np.broadcast_to(mod[l, 2, 2], (128, D))]).astype(np.float32)
        maps.append({"lat": lat_ts[core], "hT": hT_ts[core], "yT": yT_ts[core], "ssq4": ssq_ts[core], "wg": wg, "wb": inp["w_branch"][l],
                     "wo": inp["w_out"][l], "g1b": g1b, "nwT": fT(inp["norm2_w"][l]), "scT": mod_pair(mod, l, b, 4), "shT": mod_pair(mod, l, b, 3),
                     "rw": inp["router_w"][l], "rbb": rbb, "ident": ident, "ones4": ones4})
    res = run_spmd(build_s3(), maps)
    return (np.stack([r["lat_o"] for r in res]), np.stack([r["h2T_o"] for r in res]), np.stack([r["wgt_o"] for r in res]))


def run_s4(h2T_ts, wgt_ts, l, inp):
    h2T_all = np.ascontiguousarray(np.concatenate(list(h2T_ts), axis=1))
    wgt_all = wgt_ts.reshape(NTOK_ALL, 32)
    maps = []
    for c in range(NCORE):
        es = slice(4 * c, 4 * c + 4)
        maps.append({"h2T": h2T_all, "wsel": np.ascontiguousarray(wgt_all[:, es].reshape(NTOK_ALL // 128, 128, 4).transpose(1, 0, 2)),
                     "w1": np.ascontiguousarray(inp["moe_w1"][l, es]), "b1T": np.ascontiguousarray(inp["moe_b1"][l, es].reshape(4, 16, 128).transpose(2, 0, 1)),
                     "w2": np.ascontiguousarray(inp["moe_w2"][l, es]), "b2": np.ascontiguousarray(inp["moe_b2"][l, es][None])})
    res = run_spmd(build_s4(NTOK_ALL), maps)
    parts = np.zeros((NCORE, NCORE, TS_T, D), NBF)
    for c in range(NCORE):
        p = res[c]["part_o"]
        for t in range(NCORE):
            parts[t, c] = p[t * TS_T:(t + 1) * TS_T]
    return parts


def kernel(**inputs):
    inp = {k: np.asarray(v) for k, v in inputs.items()}
    mod = run_s0(inp["c"], inp["c_ctx"], inp["w_ada"], inp["b_ada"])
    DEBUG["mod"] = mod
    lat_ts = pack_ts(inp["x"].astype(np.float32), inp["ctx"].astype(np.float32))
    parts = None
    for l in range(2):
        hT_ts, lat_ts, _ = run_s1(lat_ts, mod, l, inp["norm1_w"][l], parts=parts, g2_layer=(l - 1 if parts is not None else None))
        DEBUG[f"L{l}_hT"] = hT_ts
        yT_ts, ssq_ts = run_s2(hT_ts, l, inp)
        lat_ts, h2T_ts, wgt_ts = run_s3(lat_ts, hT_ts, yT_ts, ssq_ts, mod, l, inp)
        DEBUG[f"L{l}_lat2"] = lat_ts; DEBUG[f"L{l}_h2T"] = h2T_ts; DEBUG[f"L{l}_wgt"] = wgt_ts
        parts = run_s4(h2T_ts, wgt_ts, l, inp)
    _, lat_f, fin = run_s1(lat_ts, mod, 1, inp["norm1_w"][1], parts=parts, g2_layer=1, final_w=inp["final_norm_w"])
    DEBUG["lat_final"] = lat_f
    out, _ = unpack_ts(fin)
    return out.astype(np.float32)
```

```python
import numpy as np
import ml_dtypes
import concourse.bass as bass
import concourse.mybir as mybir
from concourse.bass_utils import run_bass_kernel_spmd

F32 = mybir.dt.float32
BF16 = mybir.dt.bfloat16
ALU = mybir.AluOpType
AF = mybir.ActivationFunctionType
AX = mybir.AxisListType

D = 1024
B = 2
SEQ = 8192
CTX = 256
NCORE = 8
TS_T = 2176
TS_NT = 17
EPS = 1e-6
NBF = ml_dtypes.bfloat16


_DRAM_CACHE = {}


class Prog:
    ENGS = ("pe", "act", "dve", "pool", "sp")
    NDMA = 48
    NSW = 8
    EPOCH = 30000

    def __init__(self, nc=None, prefix=""):
        self.nc = nc if nc is not None else bass.Bass("TRN2", target_bir_lowering=False)
        self.prefix = prefix
        self.q = {e: [] for e in self.ENGS}
        self.cnt = {e: 0 for e in self.ENGS}
        self.pending = {e: False for e in self.ENGS}
        self.sems = {}
        self.cur = {}
        self._ctx = []
        self._sem_handles = []
        for e in self.ENGS:
            self._new_epoch(e)
        self.dma_sems = [self._sem(f"{self.prefix}dq{i}") for i in range(self.NDMA)]
        self.dma_use = [0] * self.NDMA
        self.dma_i = 0
        self.dma_sw_i = 0
        self.seen = {e: {} for e in self.ENGS}
        self.last_w = {}
        self.readers = {}
        self.n_ins = 0

    def _sem(self, name):
        h = self.nc.alloc_semaphore(name=name)
        self._sem_handles.append(h)
        return h

    def _enter(self, cm):
        v = cm.__enter__()
        self._ctx.append(cm)
        return v

    def _new_epoch(self, e):
        k = len([1 for n in self.sems if n.startswith(e + "_")])
        s = self._sem(f"{self.prefix}{e}_{k}")
        self.sems[f"{e}_{k}"] = s
        self.cur[e] = (f"{e}_{k}", s)
        self.cnt[e] = 0

    def sbuf(self, name, shape, dt):
        return self._enter(self.nc.sbuf_tensor(self.prefix + name, list(shape), dt))

    def psum(self, name, shape, dt=F32):
        return self._enter(self.nc.psum_tensor(self.prefix + name, list(shape), dt))

    def dram(self, name, shape, dt, kind):
        cache = _DRAM_CACHE.setdefault(id(self.nc), {})
        if name not in cache:
            cache[name] = self.nc.dram_tensor(name, list(shape), dt, kind=kind).ap()
        return cache[name]

    def _deps(self, reads, writes):
        toks = []
        for k in reads:
            if k in self.last_w:
                toks.append(self.last_w[k])
        for k in writes:
            if k in self.last_w:
                toks.append(self.last_w[k])
            toks.extend(self.readers.get(k, []))
        return toks

    def _waits(self, eng, toks):
        best = {}
        for (name, sem, val) in toks:
            if val > best.get(name, (None, 0))[1]:
                best[name] = (sem, val)
        out = []
        for name, (sem, val) in best.items():
            if name.startswith("pe_") and eng == "pe":
                continue
            if self.seen[eng].get(name, 0) >= val:
                continue
            self.seen[eng][name] = val
            out.append((sem, val))
        return out

    def _record(self, tok, reads, writes):
        for k in reads:
            self.readers.setdefault(k, []).append(tok)
        for k in writes:
            self.last_w[k] = tok
            self.readers[k] = []

    def op(self, eng, fn, reads=(), writes=(), inc=True):
        writes = list(writes) + [k for k in reads if k.startswith("ps")]
        reads = [k for k in reads if not k.startswith("ps")]
        toks = self._deps(reads, writes)
        waits = self._waits(eng, toks)
        name, sem = self.cur[eng]
        tok = (name, sem, self.cnt[eng] + 1)
        self.q[eng].append((waits, fn, (sem if inc else None)))
        self._record(tok, reads, writes)
        self.n_ins += 1
        if inc:
            self.cnt[eng] += 1
            self.pending[eng] = False
            if self.cnt[eng] >= self.EPOCH:
                self._new_epoch(eng)
        else:
            self.pending[eng] = True

    def dma(self, out, in_, reads=(), writes=(), eng="sp"):
        if eng == "pool":
            slot = self.NDMA - self.NSW + (self.dma_sw_i % self.NSW)
            self.dma_sw_i += 1
        else:
            slot = self.dma_i % (self.NDMA - self.NSW)
            self.dma_i += 1
        sem = self.dma_sems[slot]
        toks = self._deps(reads, writes)
        if self.dma_use[slot] > 0:
            toks.append((f"dq{slot}", sem, 16 * self.dma_use[slot]))
        waits = self._waits(eng, toks)
        self.dma_use[slot] += 1
        tok = (f"dq{slot}", sem, 16 * self.dma_use[slot])
        self.q[eng].append((waits, lambda e: e.dma_start(out=out, in_=in_), ("dma", sem)))
        self._record(tok, reads, writes)
        self.n_ins += 1
        return tok

    def finish(self, final_toks):
        nc = self.nc
        fw = self._waits("sp", list(final_toks))
        self.q["sp"].append((fw, None, None))
        for e in self.ENGS:
            assert not self.pending[e], f"engine {e} has trailing un-inc'ed instructions"
        qs = self.q
        with nc.Block() as block:
            def run(engobj, lst):
                for waits, fn, inc in lst:
                    for sem, val in waits:
                        engobj.wait_ge(sem, val)
                    if fn is None:
                        continue
                    ins = fn(engobj)
                    if inc is None:
                        continue
                    if isinstance(inc, tuple):
                        ins.then_inc(inc[1], 16)
                    else:
                        ins.then_inc(inc, 1)

            @block.sync
            def _(e):
                run(e, qs["sp"])

            @block.tensor
            def _(e):
                run(e, qs["pe"])

            @block.scalar
            def _(e):
                run(e, qs["act"])

            @block.vector
            def _(e):
                run(e, qs["dve"])

            @block.gpsimd
            def _(e):
                run(e, qs["pool"])
        for cm in reversed(self._ctx):
            cm.__exit__(None, None, None)
        if self.prefix:
            nc.clear_and_free_semaphores(self._sem_handles)
            nc.all_engine_barrier()
        return nc


def run_spmd(prog_nc, in_maps):
    res = run_bass_kernel_spmd(prog_nc, in_maps, core_ids=list(range(NCORE)))
    return res.results


def build_s0():
    P = Prog()
    nc = P.nc
    NCOL = 768
    vT = P.dram("vT", [128, 8, 3], F32, "ExternalInput")
    w = P.dram("w", [2, 1024, NCOL], F32, "ExternalInput")
    bb = P.dram("bb", [2, 3, NCOL], F32, "ExternalInput")
    out = P.dram("mod", [2, 3, NCOL], F32, "ExternalOutput")
    vt = P.sbuf("vt", [128, 8, 3], F32)
    sv = P.sbuf("sv", [128, 8, 3], F32)
    wt = P.sbuf("wt", [128, 2, 8, NCOL], F32)
    bt = P.sbuf("bt", [3, 2, NCOL], F32)
    ot = P.sbuf("ot", [3, 2, NCOL], F32)
    ps = [P.psum(f"ps{i}", [3, 384]) for i in range(4)]
    P.dma(vt[:], vT, writes=["vt"])
    for l in range(2):
        P.dma(wt[:, l], w[l].rearrange("(c p) n -> p c n", p=128), writes=[f"wt{l}"])
        P.dma(bt[:, l], bb[l], writes=[f"bt{l}"])
    P.op("act", lambda e: e.activation(out=sv[:], in_=vt[:], func=AF.Silu), reads=["vt"], writes=["sv"])
    toks = []
    for l in range(2):
        for h in range(2):
            pst = ps[l * 2 + h]
            for kc in range(8):
                P.op("pe", lambda e, kc=kc, l=l, h=h, pst=pst: e.matmul(
                    pst[:], lhsT=sv[:, kc, :], rhs=wt[:, l, kc, h * 384:(h + 1) * 384],
                    start=(kc == 0), stop=(kc == 7)),
                    reads=["sv", f"wt{l}"], writes=[f"ps{l}{h}"], inc=(kc == 7))
            P.op("dve", lambda e, l=l, h=h, pst=pst: e.tensor_tensor(
                out=ot[:, l, h * 384:(h + 1) * 384], in0=pst[:], in1=bt[:, l, h * 384:(h + 1) * 384], op=ALU.add),
                reads=[f"ps{l}{h}", f"bt{l}"], writes=[f"ot{l}{h}"])
        toks.append(P.dma(out[l], ot[:, l], reads=[f"ot{l}0", f"ot{l}1"], writes=[f"out{l}"]))
    return P.finish(toks)


def run_s0(c, c_ctx, w_ada, b_ada):
    v = np.concatenate([c, c_ctx[None]], axis=0)
    vT = np.ascontiguousarray(v.T.reshape(8, 128, 3).transpose(1, 0, 2))
    nc = build_s0()
    maps = []
    for core in range(NCORE):
        sl = slice(core * 768, (core + 1) * 768)
        maps.append({
            "vT": vT,
            "w": np.ascontiguousarray(w_ada[:, :, sl]),
            "bb": np.ascontiguousarray(np.broadcast_to(b_ada[:, None, sl], (2, 3, 768))),
        })
    res = run_spmd(nc, maps)
    mod = np.concatenate([r["mod"] for r in res], axis=2)
    return mod.reshape(2, 3, 6, D)


def emit_norm_T(P, lat_tile, key_lat, modA, modB, which, hT_dst, hT_key, ident, ps_pair, tagi, scr):
    junk, ssq, rstd, xn = scr
    t = tagi
    P.op("act", lambda e: e.activation(out=junk[:], in_=lat_tile, func=AF.Square, accum_out=ssq[:]),
         reads=[key_lat], writes=["junk", "ssq"])
    P.op("act", lambda e: e.activation(out=rstd[:], in_=ssq[:], func=AF.Sqrt, bias=float(D * EPS)),
         reads=["ssq"], writes=["rstd"])
    P.op("dve", lambda e: e.reciprocal(out=rstd[:], in_=rstd[:]), reads=["rstd"], writes=["rstd"])
    P.op("dve", lambda e: e.tensor_scalar(out=xn[:], in0=lat_tile, scalar1=rstd[:, 0:1], scalar2=None, op0=ALU.mult),
         reads=[key_lat, "rstd"], writes=["xn"])
    for half in range(2):
        ps = ps_pair[half]
        for j in range(4):
            c = half * 4 + j
            P.op("pe", lambda e, c=c, j=j, ps=ps: e.transpose(ps[:, j * 128:(j + 1) * 128], xn[:, c * 128:(c + 1) * 128], ident[:]),
                 reads=["xn", "ident"], writes=[f"psT{half}"], inc=(j == 3))
        for j in range(4):
            c = half * 4 + j
            eng = "act" if j % 2 == 0 else "dve"
            if eng == "act":
                P.op("act", lambda e, c=c, j=j, ps=ps: e.activation(
                    out=hT_dst[:, c, :], in_=ps[:, j * 128:(j + 1) * 128], func=AF.Identity,
                    scale=modA[:, c, which:which + 1], bias=modB[:, c, which:which + 1]),
                    reads=[f"psT{half}", "modAB"], writes=[hT_key + f"_{c}"])
            else:
                P.op("dve", lambda e, c=c, j=j, ps=ps: e.tensor_scalar(
                    out=hT_dst[:, c, :], in0=ps[:, j * 128:(j + 1) * 128],
                    scalar1=modA[:, c, which:which + 1], scalar2=modB[:, c, which:which + 1],
                    op0=ALU.mult, op1=ALU.add),
                    reads=[f"psT{half}", "modAB"], writes=[hT_key + f"_{c}"])


def load_modAB(P, nwT_d, scT_d, shT_d, name=""):
    nw = P.sbuf(name + "nw", [128, 8], F32)
    modA = P.sbuf(name + "modA", [128, 8, 2], F32)
    modB = P.sbuf(name + "modB", [128, 8, 2], F32)
    P.dma(nw[:], nwT_d, writes=[name + "nw"])
    P.dma(modA[:], scT_d, writes=[name + "modA0"])
    P.dma(modB[:], shT_d, writes=["modAB_B" + name])
    for wch in range(2):
        P.op("dve", lambda e, wch=wch: e.scalar_tensor_tensor(
            out=modA[:, :, wch], in0=modA[:, :, wch], scalar=1.0, in1=nw[:], op0=ALU.add, op1=ALU.mult),
            reads=[name + "nw", name + "modA0"], writes=[name + "modA0"])
    P.op("dve", lambda e: e.tensor_scalar(out=modA[:], in0=modA[:], scalar1=32.0, scalar2=None, op0=ALU.mult),
         reads=[name + "modA0", "modAB_B" + name], writes=["modAB"])
    return modA, modB


def build_s1(with_partials):
    P = Prog()
    xin = P.dram("xin", [TS_T, D], F32, "ExternalInput")
    nwT = P.dram("nwT", [128, 8], F32, "ExternalInput")
    scT = P.dram("scT", [128, 8, 2], F32, "ExternalInput")
    shT = P.dram("shT", [128, 8, 2], F32, "ExternalInput")
    identd = P.dram("ident", [128, 128], F32, "ExternalInput")
    fwb = P.dram("fwb", [128, D], F32, "ExternalInput")
    if with_partials:
        part = P.dram("part", [NCORE, TS_T, D], BF16, "ExternalInput")
        g2b = P.dram("g2b", [2, 128, D], F32, "ExternalInput")
        lat_o = P.dram("lat_o", [TS_T, D], F32, "ExternalOutput")
        fin_o = P.dram("fin_o", [TS_T, D], F32, "ExternalOutput")
    hT_o = P.dram("hT_o", [D, TS_T], BF16, "ExternalOutput")

    ident = P.sbuf("ident_s", [128, 128], F32)
    P.dma(ident[:], identd, writes=["ident"])
    modA, modB = load_modAB(P, nwT, scT, shT)
    fw = P.sbuf("fw", [128, D], F32)
    P.dma(fw[:], fwb, writes=["fw"])
    if with_partials:
        g2 = P.sbuf("g2", [128, 2, D], F32)
        P.dma(g2[:], g2b.rearrange("w p d -> p w d"), writes=["g2"])
    hT = P.sbuf("hT", [128, 8, TS_T], BF16)
    lat = [P.sbuf(f"lat{i}", [128, D], F32) for i in range(2)]
    pt = ([P.sbuf("pt0", [128, D], F32)] + [P.sbuf(f"pt{i}", [128, D], BF16) for i in range(1, 4)]) if with_partials else None
    fin = [P.sbuf(f"fin{i}", [128, D], F32) for i in range(2)] if with_partials else None
    scr = (P.sbuf("junk", [128, D], BF16), P.sbuf("ssq", [128, 1], F32), P.sbuf("rstd", [128, 1], F32),
           P.sbuf("xn", [128, D], F32))
    ps_pair = [P.psum(f"psT{i}", [128, 512]) for i in range(2)]
    toks = []
    for t in range(TS_NT):
        which = 1 if t == TS_NT - 1 else 0
        lt = lat[t % 2]
        kl = f"lat{t % 2}"
        P.dma(lt[:], xin[t * 128:(t + 1) * 128, :], writes=[kl])
        if with_partials:
            for c in range(NCORE):
                bi = 1 + (c % 3)
                pb = pt[bi]
                P.dma(pb[:], part[c, t * 128:(t + 1) * 128, :], writes=[f"pt{bi}"])
                if c == 0:
                    P.op("dve", lambda e, pb=pb: e.tensor_copy(out=pt[0][:], in_=pb[:]), reads=[f"pt{bi}"], writes=["pt0"])
                    continue
                P.op("dve", lambda e, pb=pb: e.tensor_tensor(out=pt[0][:], in0=pt[0][:], in1=pb[:], op=ALU.add),
                     reads=[f"pt{bi}", "pt0"], writes=["pt0"])
            P.op("dve", lambda e, which=which: e.tensor_tensor(out=pt[0][:], in0=pt[0][:], in1=g2[:, which, :], op=ALU.mult),
                 reads=["pt0", "g2"], writes=["pt0"])
            P.op("dve", lambda e, lt=lt: e.tensor_tensor(out=lt[:], in0=lt[:], in1=pt[0][:], op=ALU.add),
                 reads=["pt0", kl], writes=[kl])
            toks.append(P.dma(lat_o[t * 128:(t + 1) * 128, :], lt[:], reads=[kl], writes=[f"lato{t}"]))
        emit_norm_T(P, lt[:], kl, modA, modB, which, hT[:, :, t * 128:(t + 1) * 128], f"hT{t}", ident, ps_pair, t, scr)
        if with_partials:
            fb = fin[t % 2]
            P.op("dve", lambda e, fb=fb: e.scalar_tensor_tensor(out=fb[:], in0=scr[3][:], scalar=32.0, in1=fw[:],
                                                                  op0=ALU.mult, op1=ALU.mult),
                 reads=["xn", "fw"], writes=[f"fin{t % 2}"])
            toks.append(P.dma(fin_o[t * 128:(t + 1) * 128, :], fb[:], reads=[f"fin{t % 2}"], writes=[f"fino{t}"]))
    for c in range(8):
        toks.append(P.dma(hT_o[c * 128:(c + 1) * 128, :], hT[:, c, :],
                          reads=[f"hT{t}_{c}" for t in range(TS_NT)], writes=[f"hTo{c}"]))
    return P.finish(toks)


def ssd_consts():
    tri_f = np.triu(np.ones((128, 128), np.float32))
    tri_b = np.tril(np.ones((128, 128), np.float32))
    NEG = -30000.0
    mf = np.where(np.arange(128)[None, :] >= np.arange(128)[:, None], 0.0, NEG).astype(np.float32)
    mb = np.where(np.arange(128)[None, :] <= np.arange(128)[:, None], 0.0, NEG).astype(np.float32)
    c = np.zeros((128, 6, 128), np.float32)
    c[:, 0] = tri_f; c[:, 1] = tri_b; c[:, 2] = -tri_f; c[:, 3] = -tri_b
    c[:, 4] = np.eye(128); c[:, 5] = 1.0
    m4 = np.stack([np.tile(mf, (1, 4)), np.tile(mb, (1, 4))], 1)
    return c, m4.astype(np.float32)


def build_s2c(nlat, nc=None, pfx=""):
    P = Prog(nc, pfx)
    TT = CTX + nlat
    NB = TT // 256
    NCH = TT // 128
    hTp = P.dram("hTp", [D, NB, 260], BF16, "ExternalInput")
    wxbc_d = P.dram("wxbc", [D, 512], F32, "ExternalInput")
    wz_d = P.dram("wz", [D, 256], F32, "ExternalInput")
    wdt_d = P.dram("wdt40", [D, 40], F32, "ExternalInput")
    cw_d = P.dram("convwT", [128, 4, 5], F32, "ExternalInput")
    cb_d = P.dram("convbT", [128, 4], F32, "ExternalInput")
    dtb_d = P.dram("dtb40", [40, 1], F32, "ExternalInput")
    alog_d = P.dram("alog40", [40, 1], F32, "ExternalInput")
    dsk_d = P.dram("dskb", [128, 4], F32, "ExternalInput")
    nw_d = P.dram("snwT", [128, 2], F32, "ExternalInput")
    c6_d = P.dram("c6", [128, 6, 128], F32, "ExternalInput")
    m4_d = P.dram("m4", [128, 2, 512], F32, "ExternalInput")
    ycT_o = P.dram("ycT_o", [256, TT], BF16, "ExternalOutput")
    ssq_o = P.dram("ssq_o", [1, TT], F32, "ExternalOutput")

    c6 = P.sbuf("c6s", [128, 6, 128], F32)
    m4 = P.sbuf("m4s", [128, 2, 512], F32)
    P.dma(c6[:], c6_d, writes=["c6"])
    P.dma(m4[:], m4_d, writes=["m4"])
    identb = P.sbuf("identb", [128, 128], BF16)
    P.op("dve", lambda e: e.tensor_copy(out=identb[:], in_=c6[:, 4, :]), reads=["c6"], writes=["identb"])
    TRI = [c6[:, 0, :], c6[:, 1, :]]
    NTRI = [c6[:, 2, :], c6[:, 3, :]]
    IDF = c6[:, 4, :]
    ONES = c6[:, 5, :]
    wst = P.sbuf("wst", [128, 8, 512], F32)
    wsc = P.sbuf("wsc", [128, 4, 512], F32)
    wxb = P.sbuf("wxb", [128, 8, 512], BF16)
    wzb = P.sbuf("wzb", [128, 8, 256], BF16)
    wdtb = P.sbuf("wdtb", [128, 8, 40], BF16)
    P.dma(wst[:], wxbc_d.rearrange("(c p) n -> p c n", p=128), writes=["wst"])
    P.op("dve", lambda e: e.tensor_copy(out=wxb[:], in_=wst[:]), reads=["wst"], writes=["wxb"])
    P.dma(wst[:, :, 0:256], wz_d.rearrange("(c p) n -> p c n", p=128), writes=["wst"])
    P.op("dve", lambda e: e.tensor_copy(out=wzb[:], in_=wst[:, :, 0:256]), reads=["wst"], writes=["wzb"])
    P.dma(wst[:, :, 256:296], wdt_d.rearrange("(c p) n -> p c n", p=128), writes=["wst"])
    P.op("dve", lambda e: e.tensor_copy(out=wdtb[:], in_=wst[:, :, 256:296]), reads=["wst"], writes=["wdtb"])
    ALIAS = ["abc0", "E0", "t10", "t30", "ST0", "cbT0", "zs0", "zs1", "g0", "g1", "sq0", "sq1"]
    P.op("dve", lambda e: e.memset(wst[:], 0.0), writes=["wst"] + ALIAS)
    cw = P.sbuf("cw", [128, 4, 5], F32)
    cb = P.sbuf("cb", [128, 4], F32)
    dtb = P.sbuf("dtb", [40, 1], F32)
    A40 = P.sbuf("A40", [40, 1], F32)
    dsk = P.sbuf("dsk", [128, 4], F32)
    snw = P.sbuf("snw", [128, 2], F32)
    P.dma(cw[:], cw_d, writes=["cw"])
    P.dma(cb[:], cb_d, writes=["cb"])
    P.dma(dtb[:], dtb_d, writes=["dtb"])
    P.dma(A40[:], alog_d, writes=["A40"])
    P.dma(dsk[:], dsk_d, writes=["dsk"])
    P.dma(snw[:], nw_d, writes=["snw"])
    P.op("act", lambda e: e.activation(out=A40[:], in_=A40[:], func=AF.Exp), reads=["A40"], writes=["A40"])
    P.op("dve", lambda e: e.tensor_scalar(out=A40[:], in0=A40[:], scalar1=-1.0, scalar2=None, op0=ALU.mult),
         reads=["A40"], writes=["A40"])

    xbcT = P.sbuf("xbcT", [128, 4, TT], BF16)
    dt40 = P.sbuf("dt40", [40, 256], F32)
    dtmp = P.sbuf("dtmp", [40, 256], F32)
    dta = P.sbuf("dta", [128, NCH, 16], F32)
    yacc = P.sbuf("yacc", [128, NCH, 256], BF16)
    cacc = [P.sbuf(f"cacc{i}", [128, 256], F32) for i in range(2)]
    hb = [P.sbuf(f"hb{i}", [128, 8, 260], BF16) for i in range(2)]
    psA = [P.psum(f"psA{i}", [128, 512]) for i in range(2)]
    psD = P.psum("psD", [128, 512])
    psY = P.psum("psY", [128, 512])
    psS = [P.psum(f"psS{i}", [128, 512]) for i in range(2)]
    psTb = [P.psum(f"psTb{i}", [128, 1024], BF16) for i in range(2)]

    for blk in range(NB):
        h = hb[blk % 2]
        hk = f"hb{blk % 2}"
        P.dma(h[:], hTp.rearrange("(c p) b t -> p c b t", p=128)[:, :, blk, :], writes=[hk])
        for cc in range(4):
            ps = psA[cc % 2]
            ca = cacc[cc % 2]; ck = f"cacc{cc % 2}"
            for kc in range(8):
                P.op("pe", lambda e, ps=ps, kc=kc, cc=cc, h=h: e.matmul(
                    ps[:, 0:260], lhsT=wxb[:, kc, cc * 128:(cc + 1) * 128], rhs=h[:, kc, :], start=(kc == 0), stop=(kc == 7)),
                    reads=[hk, "wxb"], writes=[f"psA{cc % 2}"], inc=(kc == 7))
            P.op("dve", lambda e, ps=ps, ca=ca, cc=cc: e.tensor_scalar(out=ca[:], in0=ps[:, 0:256], scalar1=cw[:, cc, 0:1], scalar2=None, op0=ALU.mult),
                 reads=[f"psA{cc % 2}", "cw"], writes=[ck])
            for k in range(1, 5):
                P.op("dve", lambda e, ps=ps, ca=ca, cc=cc, k=k: e.scalar_tensor_tensor(out=ca[:], in0=ps[:, k:k + 256], scalar=cw[:, cc, k:k + 1], in1=ca[:],
                                                                                       op0=ALU.mult, op1=ALU.add),
                     reads=[f"psA{cc % 2}", "cw", ck], writes=[ck])
            P.op("act", lambda e, ca=ca, cc=cc, blk=blk: e.activation(
                out=xbcT[:, cc, blk * 256:(blk + 1) * 256], in_=ca[:], func=AF.Silu, bias=cb[:, cc:cc + 1]),
                reads=[ck, "cb"], writes=[f"xbcT{blk}"])
        ps = psA[0]
        for kc in range(8):
            P.op("pe", lambda e, ps=ps, kc=kc, h=h: e.matmul(ps[0:40, 256:512], lhsT=wdtb[:, kc, :], rhs=h[:, kc, 2:258],
                                                              start=(kc == 0), stop=(kc == 7)),
                 reads=[hk, "wdtb"], writes=["psA0"], inc=(kc == 7))
        P.op("act", lambda e, ps=ps: e.activation(out=dtmp[:], in_=ps[0:40, 256:512], func=AF.Exp, bias=dtb[:, 0:1]),
             reads=["psA0", "dtb"], writes=["dtmp"])
        P.op("act", lambda e: e.activation(out=dt40[:], in_=dtmp[:], func=AF.Ln, bias=1.0),
             reads=["dtmp"], writes=["dt40"])
        P.op("dve", lambda e: e.tensor_scalar(out=dt40[32:40, :], in0=dt40[32:40, :], scalar1=A40[32:40, 0:1], scalar2=None, op0=ALU.mult),
             reads=["dt40", "A40"], writes=["dt40"])
        for j in range(2):
            ch = blk * 2 + j
            P.op("pe", lambda e, j=j: e.transpose(psS[0][:, j * 64:j * 64 + 40], dt40[:, j * 128:(j + 1) * 128], IDF[0:40, 0:40]),
                 reads=["dt40", "c6"], writes=["psS0"])
            P.op("dve", lambda e, j=j, ch=ch: e.tensor_copy(out=dta[:, ch, :].rearrange("p (a b) -> p a b", a=2),
                                                            in_=psS[0][:, j * 64:j * 64 + 64].rearrange("p (a b) -> p a b", a=2)[:, :, 0:8]),
                 reads=["psS0"], writes=[f"dta{ch}"])

    scr = [wst, wsc]
    sfx = ["0", "1"]
    P.op("dve", lambda e: e.memset(wsc[:], 0.0), writes=["abc1", "E1", "t11", "t31", "ST1", "cbT1"])
    tiles = []
    for d in range(2):
        w_ = scr[d]
        tiles.append(dict(
            abc=w_[:, 0, :].rearrange("p (h l) -> p h l", h=4), E=w_[:, 1, :].rearrange("p (h l) -> p h l", h=4),
            t1=w_[:, 2, 0:256].rearrange("p (h d) -> p h d", h=4), t3=w_[:, 2, 256:512].rearrange("p (h d) -> p h d", h=4),
            ST=w_[:, 3, 0:256].rearrange("p (h d) -> p h d", h=4), cbT=w_[:, 3, 256:384],
            STb=P.sbuf(f"STb{d}", [128, 4, 64], BF16), cs=P.sbuf(f"cs{d}", [128, 12], F32), ecs=P.sbuf(f"ecs{d}", [128, 12], F32),
            MT=P.sbuf(f"MT{d}", [128, 4, 128], BF16), xtok=P.sbuf(f"xtok{d}", [128, 4, 64], BF16), btok=P.sbuf(f"btok{d}", [128, 128], BF16),
            xdt=P.sbuf(f"xdt{d}", [128, 4, 64], BF16), xdd=P.sbuf(f"xdd{d}", [128, 4, 64], BF16),
            pD=(psD if d == 0 else psA[0]), kD=("psD" if d == 0 else "psA0"),
            pY=(psY if d == 0 else psA[1]), kY=("psY" if d == 0 else "psA1"),
            pS=psS[d], kS=f"psS{d}", pT=psTb[d], kT=f"psTb{d}"))
    written = set()

    def ssd_iter(d, ch):
        T = tiles[d]; x = sfx[d]
        sl = slice(ch * 128, (ch + 1) * 128)
        blkkey = f"xbcT{ch // 2}"
        a4 = dta[:, ch, 8 + 4 * d:12 + 4 * d]
        dt4 = dta[:, ch, 4 * d:4 * d + 4]
        pD, pY, pS, pT = T["pD"], T["pY"], T["pS"], T["pT"]
        kD, kY, kS, kT = T["kD"], T["kY"], T["kS"], T["kT"]
        for j in range(3):
            P.op("pe", lambda e, j=j: e.transpose(pT[:, j * 128:(j + 1) * 128], xbcT[:, j, sl], identb[:]),
                 reads=[blkkey, "identb"], writes=[kT], inc=(j == 2))
        yield
        P.op("act", lambda e: e.copy(out=T["xtok"][:].rearrange("p h d -> p (h d)"), in_=pT[:, 0:256]), reads=[kT], writes=["xtok" + x])
        yield
        P.op("act", lambda e: e.copy(out=T["btok"][:], in_=pT[:, 256:384]), reads=[kT], writes=["btok" + x])
        yield
        P.op("pe", lambda e: e.matmul(pS[:, 384:512], lhsT=xbcT[:, 2, sl], rhs=xbcT[:, 3, sl], start=True, stop=True),
             reads=[blkkey], writes=[kS])
        yield
        P.op("act", lambda e: e.copy(out=T["cbT"], in_=pS[:, 384:512]), reads=[kS], writes=["cbT" + x])
        yield
        P.op("pe", lambda e: e.matmul(pS[:, 256:260], lhsT=TRI[d], rhs=a4, start=True, stop=True),
             reads=[f"dta{ch}", "c6"], writes=[kS], inc=False)
        P.op("pe", lambda e: e.matmul(pS[:, 260:264], lhsT=ONES, rhs=a4, start=True, stop=True),
             reads=[f"dta{ch}", "c6"], writes=[kS])
        yield
        P.op("dve", lambda e: e.tensor_copy(out=T["abc"], in_=a4.unsqueeze(2).broadcast_to([128, 4, 128])),
             reads=[f"dta{ch}"], writes=["abc" + x])
        yield
        for hh in range(4):
            P.op("pe", lambda e, hh=hh: e.matmul(pD[:, hh * 128:(hh + 1) * 128], lhsT=T["abc"][:, hh, :], rhs=TRI[d],
                                                 start=(hh == 0), stop=False),
                 reads=["abc" + x, "c6"], writes=[kD], inc=False)
        P.op("pe", lambda e: e.matmul(pD[:, :], lhsT=NTRI[d], rhs=T["abc"].rearrange("p h l -> p (h l)"), start=False, stop=False),
             reads=["abc" + x, "c6"], writes=[kD], inc=False)
        P.op("pe", lambda e: e.matmul(pD[:, :], lhsT=IDF, rhs=m4[:, d, :], start=False, stop=True),
             reads=["m4", "c6"], writes=[kD])
        yield
        P.op("act", lambda e: e.activation(out=T["E"].rearrange("p h l -> p (h l)"), in_=pD[:, :], func=AF.Exp),
             reads=[kD], writes=["E" + x])
        yield
        P.op("dve", lambda e: e.tensor_tensor(out=T["MT"][:], in0=T["E"], in1=T["cbT"].unsqueeze(1).broadcast_to([128, 4, 128]), op=ALU.mult),
             reads=["E" + x, "cbT" + x], writes=["MT" + x])
        yield
        cs, ecs = T["cs"], T["ecs"]
        P.op("dve", lambda e: e.tensor_copy(out=cs[:, 0:4], in_=pS[:, 256:260]), reads=[kS], writes=["cs" + x])
        P.op("dve", lambda e: e.tensor_copy(out=cs[:, 8:12], in_=pS[:, 260:264]), reads=[kS], writes=["cs" + x])
        P.op("dve", lambda e: e.tensor_tensor(out=cs[:, 4:8], in0=cs[:, 8:12], in1=cs[:, 0:4], op=ALU.subtract),
             reads=["cs" + x], writes=["cs" + x])
        yield
        P.op("act", lambda e: e.activation(out=ecs[:], in_=cs[:], func=AF.Exp), reads=["cs" + x], writes=["ecs" + x])
        yield
        P.op("dve", lambda e: e.tensor_tensor(out=T["xdt"][:], in0=T["xtok"][:], in1=dt4.unsqueeze(2).broadcast_to([128, 4, 64]), op=ALU.mult),
             reads=["xtok" + x, f"dta{ch}"], writes=["xdt" + x])
        yield
        P.op("pool", lambda e: e.tensor_tensor(out=T["xdd"][:], in0=T["xdt"][:], in1=ecs[:, 4:8].unsqueeze(2).broadcast_to([128, 4, 64]), op=ALU.mult),
             reads=["xdt" + x, "ecs" + x], writes=["xdd" + x])
        yield
        for hh in range(4):
            P.op("pe", lambda e, hh=hh: e.matmul(pY[:, hh * 64:(hh + 1) * 64], lhsT=T["MT"][:, hh, :], rhs=T["xdt"][:, hh, :],
                                                 start=(hh == 0), stop=(hh == 3)),
                 reads=["MT" + x, "xdt" + x], writes=[kY], inc=(hh == 3))
        P.op("pe", lambda e: e.matmul(pY[:, 256:512], lhsT=xbcT[:, 3, sl], rhs=T["STb"][:].rearrange("p h d -> p (h d)"),
                                      start=True, stop=True),
             reads=[blkkey, "STb" + x], writes=[kY])
        yield
        t1 = T["t1"]; t3 = T["t3"]
        P.op("dve", lambda e: e.tensor_tensor(out=t1, in0=pY[:, 256:512].rearrange("p (h d) -> p h d", h=4),
                                              in1=ecs[:, 0:4].unsqueeze(2).broadcast_to([128, 4, 64]), op=ALU.mult),
             reads=[kY, "ecs" + x], writes=["t1" + x])
        P.op("dve", lambda e: e.tensor_tensor(out=t1, in0=t1, in1=pY[:, 0:256].rearrange("p (h d) -> p h d", h=4), op=ALU.add),
             reads=[kY, "t1" + x], writes=["t1" + x])
        yield
        ya = yacc[:, ch, :].rearrange("p (h d) -> p h d", h=4)
        if d == 0:
            P.op("pool", lambda e: e.tensor_tensor(out=t3, in0=T["xtok"][:], in1=dsk[:].unsqueeze(2).broadcast_to([128, 4, 64]), op=ALU.mult),
                 reads=["xtok" + x, "dsk"], writes=["t3" + x])
            P.op("pool", lambda e: e.tensor_tensor(out=t1, in0=t1, in1=t3, op=ALU.add), reads=["t1" + x, "t3" + x], writes=["t1" + x])
        if ch not in written:
            written.add(ch)
            P.op("dve", lambda e: e.tensor_copy(out=ya, in_=t1), reads=["t1" + x], writes=[f"yacc{ch}"])
        else:
            P.op("dve", lambda e: e.tensor_tensor(out=ya, in0=ya, in1=t1, op=ALU.add), reads=["t1" + x, f"yacc{ch}"], writes=[f"yacc{ch}"])
        yield
        P.op("pe", lambda e: e.matmul(pS[:, 0:256], lhsT=T["btok"][:], rhs=T["xdd"][:].rearrange("p h d -> p (h d)"), start=True, stop=True),
             reads=["btok" + x, "xdd" + x], writes=[kS])
        yield
        ST = T["ST"]
        P.op("dve", lambda e: e.tensor_tensor(out=ST, in0=ST, in1=ecs[:, 8:12].unsqueeze(2).broadcast_to([128, 4, 64]), op=ALU.mult),
             reads=["ST" + x, "ecs" + x], writes=["ST" + x])
        P.op("dve", lambda e: e.tensor_tensor(out=ST, in0=ST, in1=pS[:, 0:256].rearrange("p (h d) -> p h d", h=4), op=ALU.add),
             reads=["ST" + x, kS], writes=["ST" + x])
        yield
        P.op("act", lambda e: e.copy(out=T["STb"][:], in_=ST), reads=["ST" + x], writes=["STb" + x])
        yield

    orders = [list(range(NCH)), [1, 0] + list(range(NCH - 1, 1, -1))]
    for d in range(2):
        P.op("dve", lambda e, d=d: e.memset(tiles[d]["STb"][:], 0.0), writes=["STb" + sfx[d]])
    for s_ in range(NCH):
        gens = [ssd_iter(0, orders[0][s_]), ssd_iter(1, orders[1][s_])]
        alive = [True, True]
        while any(alive):
            for gi in range(2):
                if alive[gi]:
                    try:
                        next(gens[gi])
                    except StopIteration:
                        alive[gi] = False

    ycTb = [P.sbuf(f"ycTb{i}", [128, 2, 256], BF16) for i in range(2)]
    ssqb = [P.sbuf(f"ssqb{i}", [1, 256], F32) for i in range(2)]
    toks = []
    zs = wst[:, 4, :].rearrange("p (c t) -> p c t", c=2)
    g = wst[:, 5, :].rearrange("p (c t) -> p c t", c=2)
    sq = wst[:, 6, :].rearrange("p (c t) -> p c t", c=2)
    for blk in range(NB):
        h = hb[blk % 2]
        hk = f"hb{blk % 2}"
        P.dma(h[:], hTp.rearrange("(c p) b t -> p c b t", p=128)[:, :, blk, :], writes=[hk])
        for cc in range(2):
            ps = psA[cc]
            for kc in range(8):
                P.op("pe", lambda e, ps=ps, kc=kc, cc=cc, h=h: e.matmul(ps[:, 0:256], lhsT=wzb[:, kc, cc * 128:(cc + 1) * 128], rhs=h[:, kc, 2:258],
                                                                       start=(kc == 0), stop=(kc == 7)),
                     reads=[hk, "wzb"], writes=[f"psA{cc}"], inc=(kc == 7))
            P.op("act", lambda e, ps=ps, cc=cc: e.activation(out=zs[:, cc, :], in_=ps[:, 0:256], func=AF.Silu),
                 reads=[f"psA{cc}"], writes=[f"zs{cc}"])
        for j in range(2):
            ch = blk * 2 + j
            for cc in range(2):
                P.op("pe", lambda e, j=j, cc=cc, ch=ch: e.transpose(psTb[0][:, (cc * 2 + j) * 128:(cc * 2 + j + 1) * 128],
                                                                   yacc[:, ch, cc * 128:(cc + 1) * 128], identb[:]),
                     reads=[f"yacc{ch}", "identb"], writes=["psTb0"], inc=(j == 1 and cc == 1))
        for cc in range(2):
            P.op("dve", lambda e, cc=cc: e.tensor_tensor(out=g[:, cc, :], in0=psTb[0][:, cc * 256:(cc + 1) * 256], in1=zs[:, cc, :], op=ALU.mult),
                 reads=["psTb0", f"zs{cc}"], writes=[f"g{cc}"])
            P.op("act", lambda e, cc=cc: e.activation(out=sq[:, cc, :], in_=g[:, cc, :], func=AF.Square),
                 reads=[f"g{cc}"], writes=[f"sq{cc}"])
            P.op("dve", lambda e, cc=cc, blk=blk: e.tensor_scalar(out=ycTb[blk % 2][:, cc, :], in0=g[:, cc, :],
                                                                   scalar1=snw[:, cc:cc + 1], scalar2=None, op0=ALU.mult),
                 reads=[f"g{cc}", "snw"], writes=[f"ycTb{blk % 2}"])
        for cc in range(2):
            P.op("pe", lambda e, cc=cc: e.matmul(psD[0:1, 0:256], lhsT=ONES[:, 0:1], rhs=sq[:, cc, :], start=(cc == 0), stop=(cc == 1)),
                 reads=[f"sq{cc}", "c6"], writes=["psD"], inc=(cc == 1))
        P.op("act", lambda e, blk=blk: e.copy(out=ssqb[blk % 2][:], in_=psD[0:1, 0:256]),
             reads=["psD"], writes=[f"ssqb{blk % 2}"])
        toks.append(P.dma(ycT_o.rearrange("(c p) t -> p c t", p=128)[:, :, blk * 256:(blk + 1) * 256], ycTb[blk % 2][:],
                          reads=[f"ycTb{blk % 2}"], writes=[f"yo{blk}"]))
        toks.append(P.dma(ssq_o[:, blk * 256:(blk + 1) * 256], ssqb[blk % 2][:], reads=[f"ssqb{blk % 2}"], writes=[f"so{blk}"]))
    return P.finish(toks)


POOL_WINDOWS = (2, 4, 8, 16)


def _win(n, w):
    t = np.arange(n)
    return np.clip(t - w // 2, 0, n), np.clip(t + w - w // 2, 0, n)


def pool_consts(w, gw):
    lo, hi = _win(128, w)
    Ar = ((np.arange(128)[:, None] >= lo[None, :]) & (np.arange(128)[:, None] < hi[None, :])).astype(np.float32)
    BL = np.zeros((128, 16, 128), np.float32)
    for di, dl in enumerate(range(-8, 8)):
        if -(w // 2) <= dl <= w - w // 2 - 1:
            BL[:, di, :] = Ar
    normrow_l = np.broadcast_to((1.0 / (hi - lo))[None, :], (128, 128)).astype(np.float32)
    clo, chi = _win(gw, w)
    invc = np.broadcast_to((1.0 / (chi - clo))[None, :], (128, gw)).astype(np.float32)
    tlo, thi = _win(256, w)
    BC = np.zeros((128, 4, 128), np.float32)
    normrow_c = np.zeros((128, 2, 128), np.float32)
    for js in range(2):
        for jd in range(2):
            ts = 2 * np.arange(128)[:, None] + js
            td = 2 * np.arange(128)[None, :] + jd
            BC[:, js * 2 + jd, :] = ((ts >= tlo[td]) & (ts < thi[td])).astype(np.float32)
    for jd in range(2):
        td = 2 * np.arange(128) + jd
        normrow_c[:, jd, :] = (1.0 / (thi[td] - tlo[td]))[None, :]
    return {"BL": BL, "BC": BC, "normrow_l": np.ascontiguousarray(normrow_l), "invc": np.ascontiguousarray(invc),
            "normrow_c": normrow_c}


def build_s2a(gw, nc=None, pfx=""):
    P = Prog(nc, pfx)
    NT = gw + 2
    TT = NT * 128
    hTc = P.dram("hTc", [D, TT], BF16, "ExternalInput")
    wp_d = P.dram("wp", [D, 256], F32, "ExternalInput")
    pw_d = P.dram("pw", [256, 256], F32, "ExternalInput")
    ps_d = P.dram("pscT", [128, 2], F32, "ExternalInput")
    BL_d = P.dram("BL", [128, 16, 128], F32, "ExternalInput")
    BC_d = P.dram("BC", [128, 4, 128], F32, "ExternalInput")
    nl_d = P.dram("normrow_l", [128, 128], F32, "ExternalInput")
    ic_d = P.dram("invc", [128, gw], F32, "ExternalInput")
    ncx_d = P.dram("normrow_c", [128, 2, 128], F32, "ExternalInput")
    yaT_o = P.dram("yaT_o", [256, TT], BF16, "ExternalOutput")

    def load_cast(name, src_ap, shape):
        st = P.sbuf(name + "_st", shape, F32)
        bf = P.sbuf(name, shape, BF16)
        P.dma(st[:], src_ap, writes=[name + "_st"])
        P.op("dve", lambda e: e.tensor_copy(out=bf[:], in_=st[:]), reads=[name + "_st"], writes=[name])
        return bf
    wpb = load_cast("wpb", wp_d.rearrange("(c p) n -> p c n", p=128), [128, 8, 256])
    pwb = load_cast("pwb", pw_d.rearrange("(c p) n -> p c n", p=128), [128, 2, 256])
    BLb = load_cast("BLb", BL_d, [128, 16, 128])
    BCb = load_cast("BCb", BC_d, [128, 4, 128])
    psc = P.sbuf("psc", [128, 2], F32)
    nl = P.sbuf("nl", [128, 128], F32)
    ic = P.sbuf("ic", [128, gw], F32)
    ncx = P.sbuf("ncx", [128, 2, 128], F32)
    for t_, d_, k_ in ((psc, ps_d, "pscale"), (nl, nl_d, "nl"), (ic, ic_d, "ic"), (ncx, ncx_d, "ncx")):
        P.dma(t_[:], d_, writes=[k_])
    Xp = P.sbuf("Xp", [128, NT, 256], BF16)
    XpT = P.sbuf("XpT", [128, 2, TT], BF16)
    yaT = P.sbuf("yaT", [128, 2, TT], BF16)
    dT = [P.sbuf(f"dT{i}", [128, 2, 128], BF16) for i in range(2)]
    tmp = P.sbuf("ptmp", [128, 128], F32)
    hb = [P.sbuf(f"hbp{i}", [128, 8, 128], BF16) for i in range(2)]
    psX = [P.psum(f"psX{i}", [128, 512]) for i in range(2)]
    psXT = [P.psum(f"psXT{i}", [128, 512]) for i in range(2)]
    psP = [P.psum(f"psP{i}", [128, 512]) for i in range(2)]
    psO = [P.psum(f"psO{i}", [128, 512]) for i in range(2)]
    for ti in range(NT):
        h = hb[ti % 2]; hk = f"hbp{ti % 2}"
        P.dma(h[:], hTc.rearrange("(c p) t -> p c t", p=128)[:, :, ti * 128:(ti + 1) * 128], writes=[hk])
        ps = psX[ti % 2]
        for kc in range(8):
            P.op("pe", lambda e, ps=ps, kc=kc, h=h: e.matmul(ps[:, 0:256], lhsT=h[:, kc, :], rhs=wpb[:, kc, :], start=(kc == 0), stop=(kc == 7)),
                 reads=[hk, "wpb"], writes=[f"psX{ti % 2}"], inc=(kc == 7))
        P.op("act", lambda e, ps=ps, ti=ti: e.copy(out=Xp[:, ti, :], in_=ps[:, 0:256]), reads=[f"psX{ti % 2}"], writes=[f"Xp{ti}"])
        ps2 = psXT[ti % 2]
        for cc in range(2):
            for kc in range(8):
                P.op("pe", lambda e, ps2=ps2, kc=kc, cc=cc, h=h: e.matmul(ps2[:, cc * 128:(cc + 1) * 128], lhsT=wpb[:, kc, cc * 128:(cc + 1) * 128],
                                                                         rhs=h[:, kc, :], start=(kc == 0), stop=(kc == 7)),
                     reads=[hk, "wpb"], writes=[f"psXT{ti % 2}"], inc=(kc == 7 and cc == 1))
        P.op("dve", lambda e, ps2=ps2, ti=ti: e.tensor_copy(out=XpT[:, :, ti * 128:(ti + 1) * 128],
                                                            in_=ps2[:, 0:256].rearrange("p (c t) -> p c t", c=2)),
             reads=[f"psXT{ti % 2}"], writes=[f"XpT{ti}"])
    for ti in range(NT):
        if ti < gw:
            srcs = [(ti + dl, BLb[:, dl + 8, :]) for dl in range(-8, 8) if 0 <= ti + dl < gw]
            nrow = nl[:]
            sc = ic[:, ti:ti + 1]
        else:
            jd = ti - gw
            srcs = [(gw + js, BCb[:, js * 2 + jd, :]) for js in range(2)]
            nrow = ncx[:, jd, :]
            sc = None
        d = dT[ti % 2]; dk = f"dT{ti % 2}"
        for cc in range(2):
            ps = psP[cc]
            for i, (src, bm) in enumerate(srcs):
                P.op("pe", lambda e, ps=ps, src=src, bm=bm, cc=cc, i=i, n=len(srcs): e.matmul(
                    ps[:, 0:128], lhsT=Xp[:, src, cc * 128:(cc + 1) * 128], rhs=bm, start=(i == 0), stop=(i == n - 1)),
                    reads=[f"Xp{src}", "BLb", "BCb"], writes=[f"psP{cc}"], inc=(i == len(srcs) - 1))
            if sc is not None:
                P.op("dve", lambda e, ps=ps, sc=sc, nrow=nrow: e.scalar_tensor_tensor(out=tmp[:], in0=ps[:, 0:128], scalar=sc, in1=nrow,
                                                                                        op0=ALU.mult, op1=ALU.mult),
                     reads=[f"psP{cc}", "ic", "nl"], writes=["ptmp"])
            else:
                P.op("dve", lambda e, ps=ps, nrow=nrow: e.tensor_tensor(out=tmp[:], in0=ps[:, 0:128], in1=nrow, op=ALU.mult),
                     reads=[f"psP{cc}", "ncx"], writes=["ptmp"])
            P.op("dve", lambda e, d=d, cc=cc, ti=ti: e.tensor_tensor(out=d[:, cc, :], in0=tmp[:], in1=XpT[:, cc, ti * 128:(ti + 1) * 128], op=ALU.subtract),
                 reads=["ptmp", f"XpT{ti}"], writes=[dk])
        for dc in range(2):
            ps = psO[dc]
            for cc in range(2):
                P.op("pe", lambda e, ps=ps, cc=cc, dc=dc, d=d: e.matmul(ps[:, 0:128], lhsT=pwb[:, cc, dc * 128:(dc + 1) * 128], rhs=d[:, cc, :],
                                                                       start=(cc == 0), stop=(cc == 1)),
                     reads=[dk, "pwb"], writes=[f"psO{dc}"], inc=(cc == 1))
            P.op("act", lambda e, ps=ps, dc=dc, ti=ti: e.activation(out=yaT[:, dc, ti * 128:(ti + 1) * 128], in_=ps[:, 0:128], func=AF.Identity,
                                                                    scale=psc[:, dc:dc + 1]),
                 reads=[f"psO{dc}", "pscale"], writes=[f"yaT{ti}"])
    toks = [P.dma(yaT_o[dc * 128:(dc + 1) * 128, :], yaT[:, dc, :], reads=[f"yaT{t}" for t in range(NT)], writes=[f"yao{dc}"]) for dc in range(2)]
    return P.finish(toks)


def fourier_consts(n2):
    n = 128 * n2
    j1 = np.arange(128)[:, None]; k1 = np.arange(128)[None, :]
    MA = np.zeros((128, n2, 256), np.float32)
    for j2 in range(n2):
        ang = -2 * np.pi * ((j1 * k1) / 128.0 + (j2 * k1) / float(n))
        MA[:, j2, :128] = np.cos(ang); MA[:, j2, 128:] = np.sin(ang)
    kl = 128 // n2
    MC = np.zeros((128, 2, 128), np.float32)
    sc = 1.0 / np.sqrt(n * 256.0)
    for a in range(kl):
        for j2 in range(n2):
            for k2 in range(n2):
                ang = -2 * np.pi * j2 * k2 / n2
                MC[a * n2 + j2, 0, a * n2 + k2] = np.cos(ang) * sc
                MC[a * n2 + j2, 1, a * n2 + k2] = -np.sin(ang) * sc
    return MA, MC


def fourier_fb():
    c = np.arange(256)[:, None]; mm = np.arange(256)[None, :]
    ang = -2 * np.pi * c * mm / 256.0
    C_, S_ = np.cos(ang), np.sin(ang)
    FB = np.zeros((128, 2, 2, 512), np.float32)
    for cc in range(2):
        sl = slice(cc * 128, (cc + 1) * 128)
        FB[:, 0, cc, :256] = C_[sl]; FB[:, 0, cc, 256:] = S_[sl]
        FB[:, 1, cc, :256] = -S_[sl]; FB[:, 1, cc, 256:] = C_[sl]
    return FB


def build_s2b(n2l, nc=None, pfx=""):
    P = Prog(nc, pfx)
    NT = n2l + 2
    TT = NT * 128
    hTc = P.dram("hTc", [D, TT], BF16, "ExternalInput")
    wf_d = P.dram("wf", [D, 256], F32, "ExternalInput")
    MAl_d = P.dram("MA_l", [128, n2l, 256], F32, "ExternalInput")
    MAc_d = P.dram("MA_c", [128, 2, 256], F32, "ExternalInput")
    MCl_d = P.dram("MC_l", [128, 2, 128], F32, "ExternalInput")
    MCc_d = P.dram("MC_c", [128, 2, 128], F32, "ExternalInput")
    FB_d = P.dram("FB", [128, 2, 2, 512], F32, "ExternalInput")
    ybT_o = P.dram("ybT_o", [256, TT], BF16, "ExternalOutput")

    stg = [P.sbuf(f"stg{i}", [128, 2048], F32) for i in range(2)]
    nst = [0]

    def load_cast(name, src_ap, shape):
        bf = P.sbuf(name, shape, BF16)
        A = shape[1]
        rest = int(np.prod(shape[2:]))
        grp = max(1, 2048 // rest)
        for a0 in range(0, A, grp):
            g_ = min(grp, A - a0)
            s = stg[nst[0] % 2]; sk = f"stg{nst[0] % 2}"; nst[0] += 1
            if len(shape) == 3:
                sv = s[:, 0:g_ * rest].rearrange("p (a b) -> p a b", a=g_)
            else:
                sv = s[:, 0:g_ * rest].rearrange("p (a b c) -> p a b c", a=g_, b=shape[2])
            P.dma(sv, src_ap[:, a0:a0 + g_], writes=[sk])
            P.op("dve", lambda e, sv=sv, a0=a0, g_=g_: e.tensor_copy(out=bf[:, a0:a0 + g_], in_=sv), reads=[sk], writes=[name])
        return bf
    wfb = load_cast("wfb", wf_d.rearrange("(c p) n -> p c n", p=128), [128, 8, 256])
    MAl = load_cast("MAl", MAl_d, [128, n2l, 256])
    MAc = load_cast("MAc", MAc_d, [128, 2, 256])
    MCl = load_cast("MCl", MCl_d, [128, 2, 128])
    MCc = load_cast("MCc", MCc_d, [128, 2, 128])
    FBb = load_cast("FBb", FB_d, [128, 2, 2, 512])
    Xf = P.sbuf("Xf", [128, NT, 256], BF16)
    Zl = P.sbuf("Zl", [128, 2, 2, 128 * n2l], BF16)
    Zc = P.sbuf("Zc", [128, 2, 2, 256], BF16)
    ybT = P.sbuf("ybT", [128, 2, TT], BF16)
    U = [P.sbuf(f"U{i}", [128, 512], BF16) for i in range(2)]
    hb = [P.sbuf(f"hbf{i}", [128, 8, 128], BF16) for i in range(2)]
    psX = [P.psum(f"psX{i}", [128, 512]) for i in range(2)]
    psA = [P.psum(f"psA{i}", [128, 512]) for i in range(2)]
    psB = [P.psum(f"psB{i}", [128, 512]) for i in range(2)]
    psC = [P.psum(f"psC{i}", [128, 512]) for i in range(2)]
    for ti in range(NT):
        h = hb[ti % 2]; hk = f"hbf{ti % 2}"
        P.dma(h[:], hTc.rearrange("(c p) t -> p c t", p=128)[:, :, ti * 128:(ti + 1) * 128], writes=[hk])
        ps = psX[ti % 2]
        for kc in range(8):
            P.op("pe", lambda e, ps=ps, kc=kc, h=h: e.matmul(ps[:, 0:256], lhsT=h[:, kc, :], rhs=wfb[:, kc, :], start=(kc == 0), stop=(kc == 7)),
                 reads=[hk, "wfb"], writes=[f"psX{ti % 2}"], inc=(kc == 7))
        P.op("act", lambda e, ps=ps, ti=ti: e.copy(out=Xf[:, ti, :], in_=ps[:, 0:256]), reads=[f"psX{ti % 2}"], writes=[f"Xf{ti}"])
        lat = ti < n2l
        j2 = ti if lat else ti - n2l
        n2 = n2l if lat else 2
        ma = MAl[:, j2, :] if lat else MAc[:, j2, :]
        Z = Zl if lat else Zc
        zk = "Zl" if lat else "Zc"
        for cc in range(2):
            pa = psA[cc]
            P.op("pe", lambda e, pa=pa, cc=cc, ti=ti, ma=ma: e.matmul(pa[:, 0:256], lhsT=Xf[:, ti, cc * 128:(cc + 1) * 128], rhs=ma, start=True, stop=True),
                 reads=[f"Xf{ti}", "MAl", "MAc"], writes=[f"psA{cc}"])
            dst = Z[:, cc, :, :].rearrange("p r (k j) -> p r k j", j=n2)[:, :, :, j2]
            eng = "dve" if cc == 0 else "act"
            if eng == "dve":
                P.op("dve", lambda e, pa=pa, dst=dst: e.tensor_copy(out=dst, in_=pa[:, 0:256].rearrange("p (r k) -> p r k", r=2)),
                     reads=[f"psA{cc}"], writes=[zk])
            else:
                P.op("act", lambda e, pa=pa, dst=dst: e.copy(out=dst, in_=pa[:, 0:256].rearrange("p (r k) -> p r k", r=2)),
                     reads=[f"psA{cc}"], writes=[zk])
    qi = 0
    for seg in range(2):
        lat = seg == 0
        n2 = n2l if lat else 2
        Z = Zl if lat else Zc
        zk = "Zl" if lat else "Zc"
        MC = MCl if lat else MCc
        kl = 128 // n2
        base = 0 if lat else n2l * 128
        for q in range(n2):
            pb = psB[qi % 2]; u = U[qi % 2]; uk = f"U{qi % 2}"
            i = 0
            for part in range(2):
                for cc in range(2):
                    P.op("pe", lambda e, pb=pb, part=part, cc=cc, q=q, i=i, Z=Z: e.matmul(
                        pb[:, :], lhsT=Z[:, cc, part, q * 128:(q + 1) * 128], rhs=FBb[:, part, cc, :], start=(i == 0), stop=(i == 3)),
                        reads=[zk, "FBb"], writes=[f"psB{qi % 2}"], inc=(i == 3))
                    i += 1
            P.op("act", lambda e, pb=pb, u=u: e.copy(out=u[:], in_=pb[:, :]), reads=[f"psB{qi % 2}"], writes=[uk])
            for mc in range(2):
                pc = psC[mc]
                P.op("pe", lambda e, pc=pc, u=u, mc=mc, MC=MC: e.matmul(pc[:, 0:128], lhsT=u[:, mc * 128:(mc + 1) * 128], rhs=MC[:, 0, :], start=True, stop=False),
                     reads=[uk, "MCl", "MCc"], writes=[f"psC{mc}"], inc=False)
                P.op("pe", lambda e, pc=pc, u=u, mc=mc, MC=MC: e.matmul(pc[:, 0:128], lhsT=u[:, 256 + mc * 128:256 + (mc + 1) * 128], rhs=MC[:, 1, :], start=False, stop=True),
                     reads=[uk, "MCl", "MCc"], writes=[f"psC{mc}"])
                dst = ybT[:, mc, base:base + 128 * n2].rearrange("p (k2 k1) -> p k1 k2", k1=128)[:, q * kl:(q + 1) * kl, :]
                P.op("dve", lambda e, pc=pc, dst=dst, n2=n2: e.tensor_copy(out=dst, in_=pc[:, 0:128].rearrange("p (a b) -> p a b", b=n2)),
                     reads=[f"psC{mc}"], writes=[f"ybT{seg}_{q}"])
            qi += 1
    allk = [f"ybT0_{q}" for q in range(n2l)] + [f"ybT1_{q}" for q in range(2)]
    toks = [P.dma(ybT_o[mc * 128:(mc + 1) * 128, :], ybT[:, mc, :], reads=allk, writes=[f"ybo{mc}"]) for mc in range(2)]
    return P.finish(toks)


def build_s3():
    P = Prog()
    lat_d = P.dram("lat", [TS_T, D], F32, "ExternalInput")
    hT_d = P.dram("hT", [D, TS_T], BF16, "ExternalInput")
    yT_d = P.dram("yT", [3, D, TS_T], BF16, "ExternalInput")
    ssq_d = P.dram("ssq4", [4, TS_T], F32, "ExternalInput")
    wg_d = P.dram("wg", [D, 3 * D], F32, "ExternalInput")
    wb_d = P.dram("wb", [3, D, D], F32, "ExternalInput")
    wo_d = P.dram("wo", [D, D], F32, "ExternalInput")
    g1_d = P.dram("g1b", [2, 128, D], F32, "ExternalInput")
    nwT = P.dram("nwT", [128, 8], F32, "ExternalInput")
    scT = P.dram("scT", [128, 8, 2], F32, "ExternalInput")
    shT = P.dram("shT", [128, 8, 2], F32, "ExternalInput")
    rw_d = P.dram("rw", [D, 32], F32, "ExternalInput")
    rb_d = P.dram("rbb", [128, 32], F32, "ExternalInput")
    id_d = P.dram("ident", [128, 128], F32, "ExternalInput")
    on_d = P.dram("ones4", [4, 128], F32, "ExternalInput")
    lat_o = P.dram("lat_o", [TS_T, D], F32, "ExternalOutput")
    h2T_o = P.dram("h2T_o", [D, TS_T], BF16, "ExternalOutput")
    wgt_o = P.dram("wgt_o", [TS_T, 32], F32, "ExternalOutput")

    wgb = P.sbuf("wgb", [128, 8, 3 * D], BF16)
    wbb = P.sbuf("wbb", [128, 3, 8, D], BF16)
    wob = P.sbuf("wob", [128, 8, D], BF16)
    P.dma(wgb[:], wg_d.rearrange("(c p) n -> p c n", p=128), writes=["wgb"], eng="pool")
    for k in range(3):
        P.dma(wbb[:, k], wb_d[k].rearrange("(c p) n -> p c n", p=128), writes=["wbb"], eng="pool")
    P.dma(wob[:], wo_d.rearrange("(c p) n -> p c n", p=128), writes=["wob"], eng="pool")
    ident = P.sbuf("ident_s", [128, 128], F32)
    P.dma(ident[:], id_d, writes=["ident"])
    ones4 = P.sbuf("ones4s", [4, 128], F32)
    P.dma(ones4[:], on_d, writes=["ones4"])
    modA, modB = load_modAB(P, nwT, scT, shT)
    g1 = P.sbuf("g1", [128, 2, D], F32)
    P.dma(g1[:], g1_d.rearrange("w p d -> p w d"), writes=["g1"])
    rw = P.sbuf("rws", [128, 8, 32], F32)
    P.dma(rw[:], rw_d.rearrange("(c p) n -> p c n", p=128), writes=["rw"])
    rb = P.sbuf("rbs", [128, 32], F32)
    P.dma(rb[:], rb_d, writes=["rb"])

    hTt = [P.sbuf("hTt0", [128, 8, 512], BF16)]
    yTt = [P.sbuf("yTt0", [128, 3, 8, 512], BF16)]
    latt = [P.sbuf(f"latt{i}", [128, D], F32) for i in range(2)]
    ssqt = P.sbuf("ssqt", [4, 512], F32)
    rstdb = P.sbuf("rstdb", [128, 512], F32)
    sg = [P.sbuf(f"sg{i}", [128, 512], F32) for i in range(2)]
    term = [P.sbuf(f"term{i}", [128, 512], F32) for i in range(3)]
    mT = P.sbuf("mT", [128, 8, 512], BF16)
    tmpo = P.sbuf("tmpo", [128, 512], F32)
    h2f = P.sbuf("h2f", [128, 8, 128], F32)
    h2b = [P.sbuf(f"h2b{i}", [128, 8, 128], BF16) for i in range(2)]
    wgt = P.sbuf("wgt", [128, TS_NT, 32], F32)
    lg = P.sbuf("lg", [128, 32], F32)
    m8 = P.sbuf("m8", [128, 8], F32)
    nmax = P.sbuf("nmax", [128, 1], F32)
    msk = P.sbuf("msk", [128, 32], F32)
    ex = P.sbuf("ex", [128, 32], F32)
    ssum = P.sbuf("ssum", [128, 1], F32)
    scr = (P.sbuf("junk", [128, D], BF16), P.sbuf("ssq", [128, 1], F32), P.sbuf("rstd", [128, 1], F32),
           P.sbuf("xn", [128, D], F32))
    psG = [P.psum(f"psG{i}", [128, 512]) for i in range(2)]
    psPj = [P.psum(f"psPj{i}", [128, 512]) for i in range(2)]
    psO = P.psum("psO", [128, 512])
    psR = P.psum("psR", [128, 512])
    ps_pair = [P.psum(f"psT{i}", [128, 512]) for i in range(2)]
    toks = []
    hv = hT_d.rearrange("(c p) t -> p c t", p=128)
    yv = yT_d.rearrange("k (c p) t -> p k c t", p=128)
    n = 0
    for (t0, nt) in [(0, 512), (512, 512), (1024, 512), (1536, 512), (2048, 128)]:
      bs_ = slice(t0, t0 + nt)
      ht = hTt[0]; hk = "hTt0"
      yt = yTt[0]; yk = "yTt0"
      P.dma(ht[:, :, 0:nt], hv[:, :, bs_], writes=[hk])
      for k in range(3):
          P.dma(yt[:, k, :, 0:nt], yv[:, k, :, bs_], writes=[yk])
      P.dma(ssqt[:, 0:nt], ssq_d[:, bs_], writes=["ssqt"])
      P.op("pe", lambda e, nt=nt: e.matmul(psR[:, 0:nt], lhsT=ones4[:], rhs=ssqt[:, 0:nt], start=True, stop=True),
           reads=["ones4", "ssqt"], writes=["psR"])
      P.op("act", lambda e, nt=nt: e.activation(out=rstdb[:, 0:nt], in_=psR[:, 0:nt], func=AF.Sqrt, scale=1.0 / D, bias=EPS),
           reads=["psR"], writes=["rstdb"])
      P.op("dve", lambda e, nt=nt: e.reciprocal(out=rstdb[:, 0:nt], in_=rstdb[:, 0:nt]), reads=["rstdb"], writes=["rstdb"])
      for dc in range(8):
          for k in range(3):
              pg = psG[n % 2]; pp = psPj[n % 2]; s_ = sg[n % 2]
              for kc in range(8):
                  P.op("pe", lambda e, pg=pg, kc=kc, k=k, dc=dc, nt=nt: e.matmul(
                      pg[:, 0:nt], lhsT=wgb[:, kc, k * D + dc * 128:k * D + (dc + 1) * 128], rhs=ht[:, kc, 0:nt], start=(kc == 0), stop=(kc == 7)),
                      reads=[hk, "wgb"], writes=[f"psG{n % 2}"], inc=(kc == 7))
              P.op("act", lambda e, pg=pg, s_=s_, nt=nt: e.activation(out=s_[:, 0:nt], in_=pg[:, 0:nt], func=AF.Sigmoid),
                   reads=[f"psG{n % 2}"], writes=[f"sg{n % 2}"])
              for wc in range(8):
                  P.op("pe", lambda e, pp=pp, wc=wc, k=k, dc=dc, nt=nt: e.matmul(
                      pp[:, 0:nt], lhsT=wbb[:, k, wc, dc * 128:(dc + 1) * 128], rhs=yt[:, k, wc, 0:nt], start=(wc == 0), stop=(wc == 7)),
                      reads=[yk, "wbb"], writes=[f"psPj{n % 2}"], inc=(wc == 7))
              P.op("dve", lambda e, pp=pp, s_=s_, k=k, nt=nt: e.tensor_tensor(out=term[k][:, 0:nt], in0=pp[:, 0:nt], in1=s_[:, 0:nt], op=ALU.mult),
                   reads=[f"psPj{n % 2}", f"sg{n % 2}"], writes=[f"term{k}"])
              n += 1
          P.op("pool", lambda e, nt=nt: e.tensor_tensor(out=term[2][:, 0:nt], in0=term[2][:, 0:nt], in1=rstdb[:, 0:nt], op=ALU.mult),
               reads=["term2", "rstdb"], writes=["term2"])
          P.op("pool", lambda e, nt=nt: e.tensor_tensor(out=term[0][:, 0:nt], in0=term[0][:, 0:nt], in1=term[1][:, 0:nt], op=ALU.add),
               reads=["term0", "term1"], writes=["term0"])
          P.op("pool", lambda e, dc=dc, nt=nt: e.tensor_tensor(out=mT[:, dc, 0:nt], in0=term[0][:, 0:nt], in1=term[2][:, 0:nt], op=ALU.add),
               reads=["term0", "term2"], writes=[f"mT{dc}"])
      for j_ in range(nt // 128):
        t = t0 // 128 + j_
        which = 1 if t == TS_NT - 1 else 0
        ts_ = slice(t * 128, (t + 1) * 128)
        js_ = slice(j_ * 128, (j_ + 1) * 128)
        lt = latt[t % 2]; lk = f"latt{t % 2}"
        P.dma(lt[:], lat_d[ts_, :], writes=[lk])
        for half in range(2):
            for dc in range(8):
                P.op("pe", lambda e, dc=dc, half=half, js_=js_: e.matmul(psO[:, :], lhsT=mT[:, dc, js_], rhs=wob[:, dc, half * 512:(half + 1) * 512],
                                                               start=(dc == 0), stop=(dc == 7)),
                     reads=[f"mT{dc}", "wob"], writes=["psO"], inc=(dc == 7))
            P.op("dve", lambda e, half=half, which=which: e.tensor_tensor(out=tmpo[:], in0=psO[:, :], in1=g1[:, which, half * 512:(half + 1) * 512], op=ALU.mult),
                 reads=["psO", "g1"], writes=["tmpo"])
            P.op("pool", lambda e, half=half, lt=lt: e.tensor_tensor(out=lt[:, half * 512:(half + 1) * 512], in0=lt[:, half * 512:(half + 1) * 512], in1=tmpo[:], op=ALU.add),
                 reads=["tmpo", lk], writes=[lk])
        toks.append(P.dma(lat_o[ts_, :], lt[:], reads=[lk], writes=[f"lato{t}"]))
        emit_norm_T(P, lt[:], lk, modA, modB, which, h2f, "h2f", ident, ps_pair, t, scr)
        hb_ = h2b[t % 2]
        P.op("pool", lambda e, hb_=hb_: e.tensor_copy(out=hb_[:], in_=h2f[:]), reads=[f"h2f_{c}" for c in range(8)], writes=[f"h2b{t % 2}"])
        toks.append(P.dma(h2T_o.rearrange("(c p) t -> p c t", p=128)[:, :, ts_], hb_[:], reads=[f"h2b{t % 2}"], writes=[f"h2o{t}"]))
        for kc in range(8):
            P.op("pe", lambda e, kc=kc: e.matmul(psR[:, 128:160], lhsT=h2f[:, kc, :], rhs=rw[:, kc, :], start=(kc == 0), stop=(kc == 7)),
                 reads=[f"h2f_{kc}", "rw"], writes=["psR"], inc=(kc == 7))
        P.op("dve", lambda e: e.tensor_tensor(out=lg[:], in0=psR[:, 128:160], in1=rb[:], op=ALU.add), reads=["psR", "rb"], writes=["lg"])
        P.op("dve", lambda e: e.max(out=m8[:], in_=lg[:]), reads=["lg"], writes=["m8"])
        P.op("dve", lambda e: e.tensor_scalar(out=msk[:], in0=lg[:], scalar1=m8[:, 3:4], scalar2=None, op0=ALU.is_ge), reads=["lg", "m8"], writes=["msk"])
        P.op("dve", lambda e: e.tensor_scalar(out=nmax[:], in0=m8[:, 0:1], scalar1=-1.0, scalar2=None, op0=ALU.mult), reads=["m8"], writes=["nmax"])
        P.op("act", lambda e: e.activation(out=ex[:], in_=lg[:], func=AF.Exp, bias=nmax[:, 0:1]), reads=["lg", "nmax"], writes=["ex"])
        P.op("dve", lambda e: e.tensor_tensor(out=ex[:], in0=ex[:], in1=msk[:], op=ALU.mult), reads=["ex", "msk"], writes=["ex"])
        P.op("dve", lambda e: e.reduce_sum(out=ssum[:], in_=ex[:], axis=AX.X), reads=["ex"], writes=["ssum"])
        P.op("dve", lambda e: e.reciprocal(out=ssum[:], in_=ssum[:]), reads=["ssum"], writes=["ssum"])
        P.op("dve", lambda e, t=t: e.tensor_scalar(out=wgt[:, t, :], in0=ex[:], scalar1=ssum[:, 0:1], scalar2=None, op0=ALU.mult),
             reads=["ex", "ssum"], writes=[f"wgt{t}"])
    toks.append(P.dma(wgt_o.rearrange("(t p) e -> p t e", p=128), wgt[:], reads=[f"wgt{t}" for t in range(TS_NT)], writes=["wgto"]))
    return P.finish(toks)


SW_ALPHA = 1.702
SW_LIMIT = 7.0
NTOK_ALL = NCORE * TS_T


def build_s4(ntok):
    P = Prog()
    NBLK = ntok // 1024
    h2T_d = P.dram("h2T", [D, ntok], BF16, "ExternalInput")
    ws_d = P.dram("wsel", [128, ntok // 128, 4], F32, "ExternalInput")
    w1_d = P.dram("w1", [4, D, 2 * D], F32, "ExternalInput")
    b1_d = P.dram("b1T", [128, 4, 16], F32, "ExternalInput")
    w2_d = P.dram("w2", [4, D, D], F32, "ExternalInput")
    b2_d = P.dram("b2", [1, 4, D], F32, "ExternalInput")
    part_o = P.dram("part_o", [ntok, D], BF16, "ExternalOutput")
    w1s = P.nc.dram_tensor("w1s", [4, D, 2 * D], BF16).ap()
    w2s = P.nc.dram_tensor("w2s", [4, D, D], BF16).ap()

    w1b = [P.sbuf(f"w1b{i}", [128, 8, 2 * D], BF16) for i in range(2)]
    w2b = [P.sbuf(f"w2b{i}", [128, 8, D], BF16) for i in range(2)]
    for e in range(4):
        P.dma(w1b[e % 2][:], w1_d[e].rearrange("(c p) n -> p c n", p=128), writes=[f"w1b{e % 2}"], eng="pool")
        P.dma(w1s[e].rearrange("(c p) n -> p c n", p=128), w1b[e % 2][:], reads=[f"w1b{e % 2}"], writes=[f"w1s{e}"])
        P.dma(w2b[e % 2][:], w2_d[e].rearrange("(c p) n -> p c n", p=128), writes=[f"w2b{e % 2}"], eng="pool")
        P.dma(w2s[e].rearrange("(c p) n -> p c n", p=128), w2b[e % 2][:], reads=[f"w2b{e % 2}"], writes=[f"w2s{e}"])
    b1 = P.sbuf("b1s", [128, 4, 16], F32)
    P.dma(b1[:], b1_d, writes=["b1"])
    b2b = P.sbuf("b2b", [1, 4, D], BF16)
    P.dma(b2b[:], b2_d, writes=["b2b"], eng="pool")
    onesb = P.sbuf("onesb", [1, 128], BF16)
    P.op("dve", lambda e: e.memset(onesb[:], 1.0), writes=["onesb"])
    ws = P.sbuf("wss", [128, ntok // 128, 4], F32)
    P.dma(ws[:], ws_d, writes=["ws"])

    hblk = [P.sbuf(f"hblk{i}", [128, 8, 1024], BF16) for i in range(2)]
    acc = P.sbuf("acc", [128, 8, D], F32)
    accb = P.sbuf("accb", [128, 2, D], BF16)
    actT = P.sbuf("actT", [128, 8, 1024], BF16)
    gsb = [P.sbuf(f"gsb{i}", [128, 512], F32) for i in range(2)]
    sgb = [P.sbuf(f"sgb{i}", [128, 512], F32) for i in range(2)]
    lsb = [P.sbuf(f"lsb{i}", [128, 512], F32) for i in range(2)]
    psGt = [P.psum(f"psGt{i}", [128, 512]) for i in range(2)]
    psLn = [P.psum(f"psLn{i}", [128, 512]) for i in range(2)]
    psDn = [P.psum(f"psDn{i}", [128, 512]) for i in range(2)]
    toks = []
    n = 0
    nd = 0
    wi = 0
    for blk in range(NBLK):
        hb = hblk[blk % 2]; hk = f"hblk{blk % 2}"
        P.dma(hb[:], h2T_d.rearrange("(c p) t -> p c t", p=128)[:, :, blk * 1024:(blk + 1) * 1024], writes=[hk])
        for e in range(4):
            wa = w1b[wi % 2]; wb_ = w2b[wi % 2]; k1 = f"w1b{wi % 2}"; k2 = f"w2b{wi % 2}"
            wi += 1
            P.dma(wa[:], w1s[e].rearrange("(c p) n -> p c n", p=128), reads=[f"w1s{e}"], writes=[k1])
            P.dma(wb_[:], w2s[e].rearrange("(c p) n -> p c n", p=128), reads=[f"w2s{e}"], writes=[k2])
            for half in range(2):
                hs = slice(half * 512, (half + 1) * 512)
                for fc in range(8):
                    pg = psGt[n % 2]; pl = psLn[n % 2]; g_ = gsb[n % 2]; s_ = sgb[n % 2]; l_ = lsb[n % 2]
                    i2 = n % 2
                    for kc in range(8):
                        P.op("pe", lambda e_, pg=pg, kc=kc, fc=fc, wa=wa, hb=hb, hs=hs: e_.matmul(
                            pg[:, :], lhsT=wa[:, kc, fc * 128:(fc + 1) * 128], rhs=hb[:, kc, hs], start=(kc == 0), stop=(kc == 7)),
                            reads=[hk, k1], writes=[f"psGt{i2}"], inc=(kc == 7))
                    for kc in range(8):
                        P.op("pe", lambda e_, pl=pl, kc=kc, fc=fc, wa=wa, hb=hb, hs=hs: e_.matmul(
                            pl[:, :], lhsT=wa[:, kc, D + fc * 128:D + (fc + 1) * 128], rhs=hb[:, kc, hs], start=(kc == 0), stop=(kc == 7)),
                            reads=[hk, k1], writes=[f"psLn{i2}"], inc=(kc == 7))
                    P.op("dve", lambda e_, pg=pg, g_=g_, e=e, fc=fc: e_.tensor_scalar(out=g_[:], in0=pg[:, :], scalar1=b1[:, e, fc:fc + 1], scalar2=SW_LIMIT,
                                                                                    op0=ALU.add, op1=ALU.min),
                         reads=[f"psGt{i2}", "b1"], writes=[f"gsb{i2}"])
                    P.op("act", lambda e_, g_=g_, s_=s_: e_.activation(out=s_[:], in_=g_[:], func=AF.Sigmoid, scale=SW_ALPHA),
                         reads=[f"gsb{i2}"], writes=[f"sgb{i2}"])
                    P.op("dve", lambda e_, pl=pl, l_=l_, e=e, fc=fc: e_.tensor_scalar(out=l_[:], in0=pl[:, :], scalar1=b1[:, e, 8 + fc:9 + fc], scalar2=SW_LIMIT,
                                                                                    op0=ALU.add, op1=ALU.min),
                         reads=[f"psLn{i2}", "b1"], writes=[f"lsb{i2}"])
                    P.op("dve", lambda e_, l_=l_: e_.tensor_scalar(out=l_[:], in0=l_[:], scalar1=-SW_LIMIT, scalar2=1.0, op0=ALU.max, op1=ALU.add),
                         reads=[f"lsb{i2}"], writes=[f"lsb{i2}"])
                    P.op("pool", lambda e_, g_=g_, s_=s_: e_.tensor_tensor(out=g_[:], in0=g_[:], in1=s_[:], op=ALU.mult),
                         reads=[f"gsb{i2}", f"sgb{i2}"], writes=[f"gsb{i2}"])
                    P.op("dve", lambda e_, g_=g_, l_=l_, fc=fc, hs=hs: e_.tensor_tensor(out=actT[:, fc, hs], in0=g_[:], in1=l_[:], op=ALU.mult),
                         reads=[f"gsb{i2}", f"lsb{i2}"], writes=[f"actT{fc}_{half}"])
                    n += 1
            for tl in range(8):
                half = tl // 4
                for dh in range(2):
                    pd = psDn[nd % 2]; i3 = nd % 2
                    for fc in range(8):
                        P.op("pe", lambda e_, pd=pd, fc=fc, tl=tl, dh=dh, wb_=wb_: e_.matmul(
                            pd[:, :], lhsT=actT[:, fc, tl * 128:(tl + 1) * 128], rhs=wb_[:, fc, dh * 512:(dh + 1) * 512], start=(fc == 0), stop=False),
                            reads=[f"actT{fc}_{half}", k2], writes=[f"psDn{i3}"], inc=False)
                    P.op("pe", lambda e_, pd=pd, e=e, dh=dh: e_.matmul(pd[:, :], lhsT=onesb[:], rhs=b2b[:, e, dh * 512:(dh + 1) * 512], start=False, stop=True),
                         reads=["onesb", "b2b"], writes=[f"psDn{i3}"])
                    wcol = ws[:, blk * 8 + tl, e:e + 1]
                    av = acc[:, tl, dh * 512:(dh + 1) * 512]
                    if e == 0:
                        P.op("dve", lambda e_, pd=pd, wcol=wcol, av=av: e_.tensor_scalar(out=av, in0=pd[:, :], scalar1=wcol, scalar2=None, op0=ALU.mult),
                             reads=[f"psDn{i3}", "ws"], writes=[f"acc{tl}"])
                    else:
                        P.op("dve", lambda e_, pd=pd, wcol=wcol, av=av: e_.scalar_tensor_tensor(out=av, in0=pd[:, :], scalar=wcol, in1=av, op0=ALU.mult, op1=ALU.add),
                             reads=[f"psDn{i3}", "ws", f"acc{tl}"], writes=[f"acc{tl}"])
                    nd += 1
        for tl in range(8):
            ab = accb[:, tl % 2, :]
            if tl % 2 == 0:
                P.op("act", lambda e_, tl=tl, ab=ab: e_.copy(out=ab, in_=acc[:, tl, :]), reads=[f"acc{tl}"], writes=[f"accb{tl % 2}"])
            else:
                P.op("pool", lambda e_, tl=tl, ab=ab: e_.tensor_copy(out=ab, in_=acc[:, tl, :]), reads=[f"acc{tl}"], writes=[f"accb{tl % 2}"])
            toks.append(P.dma(part_o[blk * 1024 + tl * 128:blk * 1024 + (tl + 1) * 128, :], ab,
                              reads=[f"accb{tl % 2}"], writes=[f"parto{blk}_{tl}"]))
    return P.finish(toks)


OFF_FOURIER = 1024
OFF_Z = 2048
OFF_XBC = 3072
OFF_DT = OFF_XBC + 2048
OFF_GATE = OFF_DT + 32
DEBUG = {}
_PROGS = {}


def _prog(name, fn):
    return fn()


def fT(v):
    return np.ascontiguousarray(np.asarray(v, np.float32).reshape(8, 128).T)


def pack_ts(lat, cx):
    out = np.zeros((NCORE, TS_T) + lat.shape[2:], lat.dtype)
    for b in range(B):
        for q in range(4):
            out[b * 4 + q, :2048] = lat[b, q * 2048:(q + 1) * 2048]
            out[b * 4 + q, 2048:2112] = cx[b, q * 64:(q + 1) * 64]
    return out


def unpack_ts(ts):
    lat = np.zeros((B, SEQ) + ts.shape[2:], ts.dtype)
    cx = np.zeros((B, CTX) + ts.shape[2:], ts.dtype)
    for b in range(B):
        for q in range(4):
            lat[b, q * 2048:(q + 1) * 2048] = ts[b * 4 + q, :2048]
            cx[b, q * 64:(q + 1) * 64] = ts[b * 4 + q, 2048:2112]
    return lat, cx


def pad_blocks(hT, segs, nb):
    out = np.zeros((hT.shape[0], nb, 260), hT.dtype)
    for bk in range(nb):
        s0 = bk * 256
        seg = [s_ for s_ in segs if s_[0] <= s0 < s_[1]][0]
        lo, hi = max(seg[0], s0 - 2), min(seg[1], s0 + 258)
        out[:, bk, (lo - (s0 - 2)):(hi - (s0 - 2))] = hT[:, lo:hi]
    return out


def mod_pair(mod, l, b, j):
    return np.stack([fT(mod[l, b, j]), fT(mod[l, 2, j])], -1)


def run_s1(lat_ts, mod, l, norm_w, parts=None, g2_layer=None, final_w=None):
    ident = np.eye(128, dtype=np.float32)
    fw = np.broadcast_to(np.asarray(final_w if final_w is not None else np.ones(D), np.float32), (128, D)).copy()
    maps = []
    for core in range(NCORE):
        b = core // 4
        m = {"xin": lat_ts[core], "nwT": fT(norm_w), "scT": mod_pair(mod, l, b, 1), "shT": mod_pair(mod, l, b, 0),
             "ident": ident, "fwb": fw}
        if parts is not None:
            m["part"] = parts[core]
            m["g2b"] = np.stack([np.broadcast_to(mod[g2_layer, b, 5], (128, D)), np.broadcast_to(mod[g2_layer, 2, 5], (128, D))]).astype(np.float32)
        maps.append(m)
    res = run_spmd(build_s1(parts is not None), maps)
    hT = np.stack([r["hT_o"] for r in res])
    if parts is not None:
        return hT, np.stack([r["lat_o"] for r in res]), np.stack([r["fin_o"] for r in res])
    return hT, lat_ts, None


def build_s2(gw, nlat):
    nc = build_s2a(gw, None, "a_")
    nc = build_s2b(gw, nc, "b_")
    nc = build_s2c(nlat, nc, "c_")
    return nc


def run_s2(hT_ts, l, inp):
    h_lat, h_ctx = unpack_ts(np.ascontiguousarray(hT_ts.transpose(0, 2, 1)))
    w_in = inp["w_in"][l]
    c6, m4 = ssd_consts()
    MAl, MCl = fourier_consts(64)
    MAc, MCc = fourier_consts(2)
    FB = fourier_fb()
    mapsa, mapsb, mapsc = [], [], []
    for core in range(NCORE):
        b, g = core // 4, core % 4
        lat_c = h_lat[b].reshape(128, 64, D).transpose(1, 0, 2).reshape(SEQ, D)
        ctx_c = h_ctx[b].reshape(128, 2, D).transpose(1, 0, 2).reshape(CTX, D)
        hTc = np.ascontiguousarray(np.concatenate([lat_c, ctx_c], 0).T)
        ma = {"hTc": hTc, "wp": np.ascontiguousarray(w_in[:, g * 256:(g + 1) * 256]),
              "pw": np.ascontiguousarray(inp["pool_w"][l, g]), "pscT": np.ascontiguousarray(inp["pool_scale"][l, g * 256:(g + 1) * 256].reshape(2, 128).T)}
        ma.update(pool_consts(POOL_WINDOWS[g], 64))
        mapsa.append(ma)
        mapsb.append({"hTc": hTc, "wf": np.ascontiguousarray(w_in[:, OFF_FOURIER + g * 256:OFF_FOURIER + (g + 1) * 256]),
                      "MA_l": MAl, "MA_c": MAc, "MC_l": MCl, "MC_c": MCc, "FB": FB})
        hT_cl = np.ascontiguousarray(np.concatenate([h_ctx[b], h_lat[b]], 0).T)
        xcols = np.r_[np.arange(g * 256, (g + 1) * 256), 1024 + np.arange(g * 128, (g + 1) * 128), 1536 + np.arange(g * 128, (g + 1) * 128)]
        dcols = np.r_[4 * g + np.arange(4), 16 + 4 * g + np.arange(4)]
        wdt = w_in[:, OFF_DT + dcols]
        wdt40 = np.zeros((D, 40), np.float32); wdt40[:, 0:8] = wdt; wdt40[:, 32:40] = wdt
        dtb = inp["dt_bias"][l].reshape(32)[dcols]
        dtb40 = np.zeros((40, 1), np.float32); dtb40[0:8, 0] = dtb; dtb40[32:40, 0] = dtb
        alog40 = np.zeros((40, 1), np.float32); alog40[32:40, 0] = inp["a_log"][l].reshape(32)[dcols]
        mapsc.append({"hTp": pad_blocks(hT_cl, [(0, CTX), (CTX, CTX + SEQ)], (CTX + SEQ) // 256),
                      "wxbc": np.ascontiguousarray(w_in[:, OFF_XBC + xcols]), "wz": np.ascontiguousarray(w_in[:, OFF_Z + g * 256:OFF_Z + (g + 1) * 256]),
                      "wdt40": wdt40, "convwT": np.ascontiguousarray(inp["conv_w"][l][:, xcols].reshape(5, 4, 128).transpose(2, 1, 0)),
                      "convbT": np.ascontiguousarray(inp["conv_b"][l][xcols].reshape(4, 128).T), "dtb40": dtb40, "alog40": alog40,
                      "dskb": np.broadcast_to(inp["d_skip"][l][None, 4 * g:4 * g + 4], (128, 4)).copy(),
                      "snwT": np.ascontiguousarray(inp["ssm_norm_w"][l][g * 256:(g + 1) * 256].reshape(2, 128).T), "c6": c6, "m4": m4})
    ra = rb = rc = run_spmd(build_s2(64, SEQ), [dict(list(a.items()) + list(b_.items()) + list(c_.items())) for a, b_, c_ in zip(mapsa, mapsb, mapsc)])
    y_lat = np.zeros((B, 3, SEQ, D), NBF); y_ctx = np.zeros((B, 3, CTX, D), NBF)
    ssq_lat = np.zeros((B, 4, SEQ), np.float32); ssq_ctx = np.zeros((B, 4, CTX), np.float32)
    for core in range(NCORE):
        b, g = core // 4, core % 4
        cs = slice(g * 256, (g + 1) * 256)
        ya = ra[core]["yaT_o"].T
        y_lat[b, 0, :, cs] = ya[:SEQ].reshape(64, 128, 256).transpose(1, 0, 2).reshape(SEQ, 256)
        y_ctx[b, 0, :, cs] = ya[SEQ:].reshape(2, 128, 256).transpose(1, 0, 2).reshape(CTX, 256)
        yb = rb[core]["ybT_o"].T
        y_lat[b, 1, :, cs] = yb[:SEQ]; y_ctx[b, 1, :, cs] = yb[SEQ:]
        yc = rc[core]["ycT_o"].T
        y_ctx[b, 2, :, cs] = yc[:CTX]; y_lat[b, 2, :, cs] = yc[CTX:]
        sq = rc[core]["ssq_o"][0]
        ssq_ctx[b, g] = sq[:CTX]; ssq_lat[b, g] = sq[CTX:]
    DEBUG[f"L{l}_y_lat"] = y_lat; DEBUG[f"L{l}_y_ctx"] = y_ctx; DEBUG[f"L{l}_ssq_lat"] = ssq_lat
    yT_ts = np.zeros((NCORE, 3, D, TS_T), NBF)
    ssq_ts = np.zeros((NCORE, 4, TS_T), np.float32)
    for core in range(NCORE):
        b, q = core // 4, core % 4
        for k in range(3):
            yT_ts[core, k, :, :2048] = y_lat[b, k, q * 2048:(q + 1) * 2048].T
            yT_ts[core, k, :, 2048:2112] = y_ctx[b, k, q * 64:(q + 1) * 64].T
        ssq_ts[core, :, :2048] = ssq_lat[b, :, q * 2048:(q + 1) * 2048]
        ssq_ts[core, :, 2048:2112] = ssq_ctx[b, :, q * 64:(q + 1) * 64]
    return yT_ts, ssq_ts


def run_s3(lat_ts, hT_ts, yT_ts, ssq_ts, mod, l, inp):
    ident = np.eye(128, dtype=np.float32)
    ones4 = np.ones((4, 128), np.float32)
    wg = np.ascontiguousarray(inp["w_in"][l][:, OFF_GATE:])
    rbb = np.broadcast_to(inp["router_b"][l], (128, 32)).astype(np.float32).copy()
    maps = []
    for core in range(NCORE):
        b = core // 4
        g1b = np.stack([np.broadcast_to(mod[l, b, 2], (128, D)), np.broadcast_to(mod[l, 2, 2], (128, D))]).astype(np.float32)
        maps.append({"lat": lat_ts[core], "hT": hT_ts[core], "yT": yT_ts[core], "ssq4": ssq_ts[core], "wg": wg, "wb": inp["w_branch"][l],
                     "wo": inp["w_out"][l], "g1b": g1b, "nwT": fT(inp["norm2_w"][l]), "scT": mod_pair(mod, l, b, 4), "shT": mod_pair(mod, l, b, 3),
                     "rw": inp["router_w"][l], "rbb": rbb, "ident": ident, "ones4": ones4})
    res = run_spmd(build_s3(), maps)
    return (np.stack([r["lat_o"] for r in res]), np.stack([r["h2T_o"] for r in res]), np.stack([r["wgt_o"] for r in res]))


def run_s4(h2T_ts, wgt_ts, l, inp):
    h2T_all = np.ascontiguousarray(np.concatenate(list(h2T_ts), axis=1))
    wgt_all = wgt_ts.reshape(NTOK_ALL, 32)
    maps = []
    for c in range(NCORE):
        es = slice(4 * c, 4 * c + 4)
        maps.append({"h2T": h2T_all, "wsel": np.ascontiguousarray(wgt_all[:, es].reshape(NTOK_ALL // 128, 128, 4).transpose(1, 0, 2)),
                     "w1": np.ascontiguousarray(inp["moe_w1"][l, es]), "b1T": np.ascontiguousarray(inp["moe_b1"][l, es].reshape(4, 16, 128).transpose(2, 0, 1)),
                     "w2": np.ascontiguousarray(inp["moe_w2"][l, es]), "b2": np.ascontiguousarray(inp["moe_b2"][l, es][None])})
    res = run_spmd(build_s4(NTOK_ALL), maps)
    parts = np.zeros((NCORE, NCORE, TS_T, D), NBF)
    for c in range(NCORE):
        p = res[c]["part_o"]
        for t in range(NCORE):
            parts[t, c] = p[t * TS_T:(t + 1) * TS_T]
    return parts


def kernel(**inputs):
    inp = {k: np.asarray(v) for k, v in inputs.items()}
    mod = run_s0(inp["c"], inp["c_ctx"], inp["w_ada"], inp["b_ada"])
    DEBUG["mod"] = mod
    lat_ts = pack_ts(inp["x"].astype(np.float32), inp["ctx"].astype(np.float32))
    parts = None
    for l in range(2):
        hT_ts, lat_ts, _ = run_s1(lat_ts, mod, l, inp["norm1_w"][l], parts=parts, g2_layer=(l - 1 if parts is not None else None))
        DEBUG[f"L{l}_hT"] = hT_ts
        yT_ts, ssq_ts = run_s2(hT_ts, l, inp)
        lat_ts, h2T_ts, wgt_ts = run_s3(lat_ts, hT_ts, yT_ts, ssq_ts, mod, l, inp)
        DEBUG[f"L{l}_lat2"] = lat_ts; DEBUG[f"L{l}_h2T"] = h2T_ts; DEBUG[f"L{l}_wgt"] = wgt_ts
        parts = run_s4(h2T_ts, wgt_ts, l, inp)
    _, lat_f, fin = run_s1(lat_ts, mod, 1, inp["norm1_w"][1], parts=parts, g2_layer=1, final_w=inp["final_norm_w"])
    DEBUG["lat_final"] = lat_f
    out, _ = unpack_ts(fin)
    return out.astype(np.float32)
```

```python
import numpy as np
import ml_dtypes
import concourse.bass as bass
import concourse.mybir as mybir
from concourse.bass_utils import run_bass_kernel_spmd

F32 = mybir.dt.float32
BF16 = mybir.dt.bfloat16
ALU = mybir.AluOpType
AF = mybir.ActivationFunctionType
AX = mybir.AxisListType

D = 1024
B = 2
SEQ = 8192
CTX = 256
NCORE = 8
TS_T = 2176
TS_NT = 17
EPS = 1e-6
NBF = ml_dtypes.bfloat16


_DRAM_CACHE = {}


class Prog:
    ENGS = ("pe", "act", "dve", "pool", "sp")
    NDMA = 48
    NSW = 8
    EPOCH = 30000

    def __init__(self, nc=None, prefix=""):
        self.nc = nc if nc is not None else bass.Bass("TRN2", target_bir_lowering=False)
        self.prefix = prefix
        self.q = {e: [] for e in self.ENGS}
        self.cnt = {e: 0 for e in self.ENGS}
        self.pending = {e: False for e in self.ENGS}
        self.sems = {}
        self.cur = {}
        self._ctx = []
        self._sem_handles = []
        for e in self.ENGS:
            self._new_epoch(e)
        self.dma_sems = [self._sem(f"{self.prefix}dq{i}") for i in range(self.NDMA)]
        self.dma_use = [0] * self.NDMA
        self.dma_i = 0
        self.dma_sw_i = 0
        self.seen = {e: {} for e in self.ENGS}
        self.last_w = {}
        self.readers = {}
        self.n_ins = 0

    def _sem(self, name):
        h = self.nc.alloc_semaphore(name=name)
        self._sem_handles.append(h)
        return h

    def _enter(self, cm):
        v = cm.__enter__()
        self._ctx.append(cm)
        return v

    def _new_epoch(self, e):
        k = len([1 for n in self.sems if n.startswith(e + "_")])
        s = self._sem(f"{self.prefix}{e}_{k}")
        self.sems[f"{e}_{k}"] = s
        self.cur[e] = (f"{e}_{k}", s)
        self.cnt[e] = 0

    def sbuf(self, name, shape, dt):
        return self._enter(self.nc.sbuf_tensor(self.prefix + name, list(shape), dt))

    def psum(self, name, shape, dt=F32):
        return self._enter(self.nc.psum_tensor(self.prefix + name, list(shape), dt))

    def dram(self, name, shape, dt, kind):
        cache = _DRAM_CACHE.setdefault(id(self.nc), {})
        if name not in cache:
            cache[name] = self.nc.dram_tensor(name, list(shape), dt, kind=kind).ap()
        return cache[name]

    def _deps(self, reads, writes):
        toks = []
        for k in reads:
            if k in self.last_w:
                toks.append(self.last_w[k])
        for k in writes:
            if k in self.last_w:
                toks.append(self.last_w[k])
            toks.extend(self.readers.get(k, []))
        return toks

    def _waits(self, eng, toks):
        best = {}
        for (name, sem, val) in toks:
            if val > best.get(name, (None, 0))[1]:
                best[name] = (sem, val)
        out = []
        for name, (sem, val) in best.items():
            if name.startswith("pe_") and eng == "pe":
                continue
            if self.seen[eng].get(name, 0) >= val:
                continue
            self.seen[eng][name] = val
            out.append((sem, val))
        return out

    def _record(self, tok, reads, writes):
        for k in reads:
            self.readers.setdefault(k, []).append(tok)
        for k in writes:
            self.last_w[k] = tok
            self.readers[k] = []

    def op(self, eng, fn, reads=(), writes=(), inc=True):
        writes = list(writes) + [k for k in reads if k.startswith("ps")]
        reads = [k for k in reads if not k.startswith("ps")]
        toks = self._deps(reads, writes)
        waits = self._waits(eng, toks)
        name, sem = self.cur[eng]
        tok = (name, sem, self.cnt[eng] + 1)
        self.q[eng].append((waits, fn, (sem if inc else None)))
        self._record(tok, reads, writes)
        self.n_ins += 1
        if inc:
            self.cnt[eng] += 1
            self.pending[eng] = False
            if self.cnt[eng] >= self.EPOCH:
                self._new_epoch(eng)
        else:
            self.pending[eng] = True

    def dma(self, out, in_, reads=(), writes=(), eng="sp"):
        if eng == "pool":
            slot = self.NDMA - self.NSW + (self.dma_sw_i % self.NSW)
            self.dma_sw_i += 1
        else:
            slot = self.dma_i % (self.NDMA - self.NSW)
            self.dma_i += 1
        sem = self.dma_sems[slot]
        toks = self._deps(reads, writes)
        if self.dma_use[slot] > 0:
            toks.append((f"dq{slot}", sem, 16 * self.dma_use[slot]))
        waits = self._waits(eng, toks)
        self.dma_use[slot] += 1
        tok = (f"dq{slot}", sem, 16 * self.dma_use[slot])
        self.q[eng].append((waits, lambda e: e.dma_start(out=out, in_=in_), ("dma", sem)))
        self._record(tok, reads, writes)
        self.n_ins += 1
        return tok

    def finish(self, final_toks):
        nc = self.nc
        fw = self._waits("sp", list(final_toks))
        self.q["sp"].append((fw, None, None))
        for e in self.ENGS:
            assert not self.pending[e], f"engine {e} has trailing un-inc'ed instructions"
        qs = self.q
        with nc.Block() as block:
            def run(engobj, lst):
                for waits, fn, inc in lst:
                    for sem, val in waits:
                        engobj.wait_ge(sem, val)
                    if fn is None:
                        continue
                    ins = fn(engobj)
                    if inc is None:
                        continue
                    if isinstance(inc, tuple):
                        ins.then_inc(inc[1], 16)
                    else:
                        ins.then_inc(inc, 1)

            @block.sync
            def _(e):
                run(e, qs["sp"])

            @block.tensor
            def _(e):
                run(e, qs["pe"])

            @block.scalar
            def _(e):
                run(e, qs["act"])

            @block.vector
            def _(e):
                run(e, qs["dve"])

            @block.gpsimd
            def _(e):
                run(e, qs["pool"])
        for cm in reversed(self._ctx):
            cm.__exit__(None, None, None)
        if self.prefix:
            nc.clear_and_free_semaphores(self._sem_handles)
            nc.all_engine_barrier()
        return nc


def run_spmd(prog_nc, in_maps):
    res = run_bass_kernel_spmd(prog_nc, in_maps, core_ids=list(range(NCORE)))
    return res.results


def build_s0():
    P = Prog()
    nc = P.nc
    NCOL = 768
    vT = P.dram("vT", [128, 8, 3], F32, "ExternalInput")
    w = P.dram("w", [2, 1024, NCOL], F32, "ExternalInput")
    bb = P.dram("bb", [2, 3, NCOL], F32, "ExternalInput")
    out = P.dram("mod", [2, 3, NCOL], F32, "ExternalOutput")
    vt = P.sbuf("vt", [128, 8, 3], F32)
    sv = P.sbuf("sv", [128, 8, 3], F32)
    wt = P.sbuf("wt", [128, 2, 8, NCOL], F32)
    bt = P.sbuf("bt", [3, 2, NCOL], F32)
    ot = P.sbuf("ot", [3, 2, NCOL], F32)
    ps = [P.psum(f"ps{i}", [3, 384]) for i in range(4)]
    P.dma(vt[:], vT, writes=["vt"])
    for l in range(2):
        P.dma(wt[:, l], w[l].rearrange("(c p) n -> p c n", p=128), writes=[f"wt{l}"])
        P.dma(bt[:, l], bb[l], writes=[f"bt{l}"])
    P.op("act", lambda e: e.activation(out=sv[:], in_=vt[:], func=AF.Silu), reads=["vt"], writes=["sv"])
    toks = []
    for l in range(2):
        for h in range(2):
            pst = ps[l * 2 + h]
            for kc in range(8):
                P.op("pe", lambda e, kc=kc, l=l, h=h, pst=pst: e.matmul(
                    pst[:], lhsT=sv[:, kc, :], rhs=wt[:, l, kc, h * 384:(h + 1) * 384],
                    start=(kc == 0), stop=(kc == 7)),
                    reads=["sv", f"wt{l}"], writes=[f"ps{l}{h}"], inc=(kc == 7))
            P.op("dve", lambda e, l=l, h=h, pst=pst: e.tensor_tensor(
                out=ot[:, l, h * 384:(h + 1) * 384], in0=pst[:], in1=bt[:, l, h * 384:(h + 1) * 384], op=ALU.add),
                reads=[f"ps{l}{h}", f"bt{l}"], writes=[f"ot{l}{h}"])
        toks.append(P.dma(out[l], ot[:, l], reads=[f"ot{l}0", f"ot{l}1"], writes=[f"out{l}"]))
    return P.finish(toks)


def run_s0(c, c_ctx, w_ada, b_ada):
    v = np.concatenate([c, c_ctx[None]], axis=0)
    vT = np.ascontiguousarray(v.T.reshape(8, 128, 3).transpose(1, 0, 2))
    nc = build_s0()
    maps = []
    for core in range(NCORE):
        sl = slice(core * 768, (core + 1) * 768)
        maps.append({
            "vT": vT,
            "w": np.ascontiguousarray(w_ada[:, :, sl]),
            "bb": np.ascontiguousarray(np.broadcast_to(b_ada[:, None, sl], (2, 3, 768))),
        })
    res = run_spmd(nc, maps)
    mod = np.concatenate([r["mod"] for r in res], axis=2)
    return mod.reshape(2, 3, 6, D)


def emit_norm_T(P, lat_tile, key_lat, modA, modB, which, hT_dst, hT_key, ident, ps_pair, tagi, scr):
    junk, ssq, rstd, xn = scr
    t = tagi
    P.op("act", lambda e: e.activation(out=junk[:], in_=lat_tile, func=AF.Square, accum_out=ssq[:]),
         reads=[key_lat], writes=["junk", "ssq"])
    P.op("act", lambda e: e.activation(out=rstd[:], in_=ssq[:], func=AF.Sqrt, bias=float(D * EPS)),
         reads=["ssq"], writes=["rstd"])
    P.op("dve", lambda e: e.reciprocal(out=rstd[:], in_=rstd[:]), reads=["rstd"], writes=["rstd"])
    P.op("dve", lambda e: e.tensor_scalar(out=xn[:], in0=lat_tile, scalar1=rstd[:, 0:1], scalar2=None, op0=ALU.mult),
         reads=[key_lat, "rstd"], writes=["xn"])
    for half in range(2):
        ps = ps_pair[half]
        for j in range(4):
            c = half * 4 + j
            P.op("pe", lambda e, c=c, j=j, ps=ps: e.transpose(ps[:, j * 128:(j + 1) * 128], xn[:, c * 128:(c + 1) * 128], ident[:]),
                 reads=["xn", "ident"], writes=[f"psT{half}"], inc=(j == 3))
        for j in range(4):
            c = half * 4 + j
            eng = "act" if j % 2 == 0 else "dve"
            if eng == "act":
                P.op("act", lambda e, c=c, j=j, ps=ps: e.activation(
                    out=hT_dst[:, c, :], in_=ps[:, j * 128:(j + 1) * 128], func=AF.Identity,
                    scale=modA[:, c, which:which + 1], bias=modB[:, c, which:which + 1]),
                    reads=[f"psT{half}", "modAB"], writes=[hT_key + f"_{c}"])
            else:
                P.op("dve", lambda e, c=c, j=j, ps=ps: e.tensor_scalar(
                    out=hT_dst[:, c, :], in0=ps[:, j * 128:(j + 1) * 128],
                    scalar1=modA[:, c, which:which + 1], scalar2=modB[:, c, which:which + 1],
                    op0=ALU.mult, op1=ALU.add),
                    reads=[f"psT{half}", "modAB"], writes=[hT_key + f"_{c}"])


def load_modAB(P, nwT_d, scT_d, shT_d, name=""):
    nw = P.sbuf(name + "nw", [128, 8], F32)
    modA = P.sbuf(name + "modA", [128, 8, 2], F32)
    modB = P.sbuf(name + "modB", [128, 8, 2], F32)
    P.dma(nw[:], nwT_d, writes=[name + "nw"])
    P.dma(modA[:], scT_d, writes=[name + "modA0"])
    P.dma(modB[:], shT_d, writes=["modAB_B" + name])
    for wch in range(2):
        P.op("dve", lambda e, wch=wch: e.scalar_tensor_tensor(
            out=modA[:, :, wch], in0=modA[:, :, wch], scalar=1.0, in1=nw[:], op0=ALU.add, op1=ALU.mult),
            reads=[name + "nw", name + "modA0"], writes=[name + "modA0"])
    P.op("dve", lambda e: e.tensor_scalar(out=modA[:], in0=modA[:], scalar1=32.0, scalar2=None, op0=ALU.mult),
         reads=[name + "modA0", "modAB_B" + name], writes=["modAB"])
    return modA, modB


def build_s1(with_partials):
    P = Prog()
    xin = P.dram("xin", [TS_T, D], F32, "ExternalInput")
    nwT = P.dram("nwT", [128, 8], F32, "ExternalInput")
    scT = P.dram("scT", [128, 8, 2], F32, "ExternalInput")
    shT = P.dram("shT", [128, 8, 2], F32, "ExternalInput")
    identd = P.dram("ident", [128, 128], F32, "ExternalInput")
    fwb = P.dram("fwb", [128, D], F32, "ExternalInput")
    if with_partials:
        part = P.dram("part", [NCORE, TS_T, D], BF16, "ExternalInput")
        g2b = P.dram("g2b", [2, 128, D], F32, "ExternalInput")
        lat_o = P.dram("lat_o", [TS_T, D], F32, "ExternalOutput")
        fin_o = P.dram("fin_o", [TS_T, D], F32, "ExternalOutput")
    hT_o = P.dram("hT_o", [D, TS_T], BF16, "ExternalOutput")

    ident = P.sbuf("ident_s", [128, 128], F32)
    P.dma(ident[:], identd, writes=["ident"])
    modA, modB = load_modAB(P, nwT, scT, shT)
    fw = P.sbuf("fw", [128, D], F32)
    P.dma(fw[:], fwb, writes=["fw"])
    if with_partials:
        g2 = P.sbuf("g2", [128, 2, D], F32)
        P.dma(g2[:], g2b.rearrange("w p d -> p w d"), writes=["g2"])
    hT = P.sbuf("hT", [128, 8, TS_T], BF16)
    lat = [P.sbuf(f"lat{i}", [128, D], F32) for i in range(2)]
    pt = ([P.sbuf("pt0", [128, D], F32)] + [P.sbuf(f"pt{i}", [128, D], BF16) for i in range(1, 4)]) if with_partials else None
    fin = [P.sbuf(f"fin{i}", [128, D], F32) for i in range(2)] if with_partials else None
    scr = (P.sbuf("junk", [128, D], BF16), P.sbuf("ssq", [128, 1], F32), P.sbuf("rstd", [128, 1], F32),
           P.sbuf("xn", [128, D], F32))
    ps_pair = [P.psum(f"psT{i}", [128, 512]) for i in range(2)]
    toks = []
    for t in range(TS_NT):
        which = 1 if t == TS_NT - 1 else 0
        lt = lat[t % 2]
        kl = f"lat{t % 2}"
        P.dma(lt[:], xin[t * 128:(t + 1) * 128, :], writes=[kl])
        if with_partials:
            for c in range(NCORE):
                bi = 1 + (c % 3)
                pb = pt[bi]
                P.dma(pb[:], part[c, t * 128:(t + 1) * 128, :], writes=[f"pt{bi}"])
                if c == 0:
                    P.op("dve", lambda e, pb=pb: e.tensor_copy(out=pt[0][:], in_=pb[:]), reads=[f"pt{bi}"], writes=["pt0"])
                    continue
                P.op("dve", lambda e, pb=pb: e.tensor_tensor(out=pt[0][:], in0=pt[0][:], in1=pb[:], op=ALU.add),
                     reads=[f"pt{bi}", "pt0"], writes=["pt0"])
            P.op("dve", lambda e, which=which: e.tensor_tensor(out=pt[0][:], in0=pt[0][:], in1=g2[:, which, :], op=ALU.mult),
                 reads=["pt0", "g2"], writes=["pt0"])
            P.op("dve", lambda e, lt=lt: e.tensor_tensor(out=lt[:], in0=lt[:], in1=pt[0][:], op=ALU.add),
                 reads=["pt0", kl], writes=[kl])
            toks.append(P.dma(lat_o[t * 128:(t + 1) * 128, :], lt[:], reads=[kl], writes=[f"lato{t}"]))
        emit_norm_T(P, lt[:], kl, modA, modB, which, hT[:, :, t * 128:(t + 1) * 128], f"hT{t}", ident, ps_pair, t, scr)
        if with_partials:
            fb = fin[t % 2]
            P.op("dve", lambda e, fb=fb: e.scalar_tensor_tensor(out=fb[:], in0=scr[3][:], scalar=32.0, in1=fw[:],
                                                                  op0=ALU.mult, op1=ALU.mult),
                 reads=["xn", "fw"], writes=[f"fin{t % 2}"])
            toks.append(P.dma(fin_o[t * 128:(t + 1) * 128, :], fb[:], reads=[f"fin{t % 2}"], writes=[f"fino{t}"]))
    for c in range(8):
        toks.append(P.dma(hT_o[c * 128:(c + 1) * 128, :], hT[:, c, :],
                          reads=[f"hT{t}_{c}" for t in range(TS_NT)], writes=[f"hTo{c}"]))
    return P.finish(toks)


def ssd_consts():
    tri_f = np.triu(np.ones((128, 128), np.float32))
    tri_b = np.tril(np.ones((128, 128), np.float32))
    NEG = -30000.0
    mf = np.where(np.arange(128)[None, :] >= np.arange(128)[:, None], 0.0, NEG).astype(np.float32)
    mb = np.where(np.arange(128)[None, :] <= np.arange(128)[:, None], 0.0, NEG).astype(np.float32)
    c = np.zeros((128, 6, 128), np.float32)
    c[:, 0] = tri_f; c[:, 1] = tri_b; c[:, 2] = -tri_f; c[:, 3] = -tri_b
    c[:, 4] = np.eye(128); c[:, 5] = 1.0
    m4 = np.stack([np.tile(mf, (1, 4)), np.tile(mb, (1, 4))], 1)
    return c, m4.astype(np.float32)


def build_s2c(nlat, nc=None, pfx=""):
    P = Prog(nc, pfx)
    TT = CTX + nlat
    NB = TT // 256
    NCH = TT // 128
    hTp = P.dram("hTp", [D, NB, 260], BF16, "ExternalInput")
    wxbc_d = P.dram("wxbc", [D, 512], F32, "ExternalInput")
    wz_d = P.dram("wz", [D, 256], F32, "ExternalInput")
    wdt_d = P.dram("wdt40", [D, 40], F32, "ExternalInput")
    cw_d = P.dram("convwT", [128, 4, 5], F32, "ExternalInput")
    cb_d = P.dram("convbT", [128, 4], F32, "ExternalInput")
    dtb_d = P.dram("dtb40", [40, 1], F32, "ExternalInput")
    alog_d = P.dram("alog40", [40, 1], F32, "ExternalInput")
    dsk_d = P.dram("dskb", [128, 4], F32, "ExternalInput")
    nw_d = P.dram("snwT", [128, 2], F32, "ExternalInput")
    c6_d = P.dram("c6", [128, 6, 128], F32, "ExternalInput")
    m4_d = P.dram("m4", [128, 2, 512], F32, "ExternalInput")
    ycT_o = P.dram("ycT_o", [256, TT], BF16, "ExternalOutput")
    ssq_o = P.dram("ssq_o", [1, TT], F32, "ExternalOutput")

    c6 = P.sbuf("c6s", [128, 6, 128], F32)
    m4 = P.sbuf("m4s", [128, 2, 512], F32)
    P.dma(c6[:], c6_d, writes=["c6"])
    P.dma(m4[:], m4_d, writes=["m4"])
    identb = P.sbuf("identb", [128, 128], BF16)
    P.op("dve", lambda e: e.tensor_copy(out=identb[:], in_=c6[:, 4, :]), reads=["c6"], writes=["identb"])
    TRI = [c6[:, 0, :], c6[:, 1, :]]
    NTRI = [c6[:, 2, :], c6[:, 3, :]]
    IDF = c6[:, 4, :]
    ONES = c6[:, 5, :]
    wst = P.sbuf("wst", [128, 8, 512], F32)
    wsc = P.sbuf("wsc", [128, 4, 512], F32)
    wxb = P.sbuf("wxb", [128, 8, 512], BF16)
    wzb = P.sbuf("wzb", [128, 8, 256], BF16)
    wdtb = P.sbuf("wdtb", [128, 8, 40], BF16)
    P.dma(wst[:], wxbc_d.rearrange("(c p) n -> p c n", p=128), writes=["wst"])
    P.op("dve", lambda e: e.tensor_copy(out=wxb[:], in_=wst[:]), reads=["wst"], writes=["wxb"])
    P.dma(wst[:, :, 0:256], wz_d.rearrange("(c p) n -> p c n", p=128), writes=["wst"])
    P.op("dve", lambda e: e.tensor_copy(out=wzb[:], in_=wst[:, :, 0:256]), reads=["wst"], writes=["wzb"])
    P.dma(wst[:, :, 256:296], wdt_d.rearrange("(c p) n -> p c n", p=128), writes=["wst"])
    P.op("dve", lambda e: e.tensor_copy(out=wdtb[:], in_=wst[:, :, 256:296]), reads=["wst"], writes=["wdtb"])
    ALIAS = ["abc0", "E0", "t10", "t30", "ST0", "cbT0", "zs0", "zs1", "g0", "g1", "sq0", "sq1"]
    P.op("dve", lambda e: e.memset(wst[:], 0.0), writes=["wst"] + ALIAS)
    cw = P.sbuf("cw", [128, 4, 5], F32)
    cb = P.sbuf("cb", [128, 4], F32)
    dtb = P.sbuf("dtb", [40, 1], F32)
    A40 = P.sbuf("A40", [40, 1], F32)
    dsk = P.sbuf("dsk", [128, 4], F32)
    snw = P.sbuf("snw", [128, 2], F32)
    P.dma(cw[:], cw_d, writes=["cw"])
    P.dma(cb[:], cb_d, writes=["cb"])
    P.dma(dtb[:], dtb_d, writes=["dtb"])
    P.dma(A40[:], alog_d, writes=["A40"])
    P.dma(dsk[:], dsk_d, writes=["dsk"])
    P.dma(snw[:], nw_d, writes=["snw"])
    P.op("act", lambda e: e.activation(out=A40[:], in_=A40[:], func=AF.Exp), reads=["A40"], writes=["A40"])
    P.op("dve", lambda e: e.tensor_scalar(out=A40[:], in0=A40[:], scalar1=-1.0, scalar2=None, op0=ALU.mult),
         reads=["A40"], writes=["A40"])

    xbcT = P.sbuf("xbcT", [128, 4, TT], BF16)
    dt40 = P.sbuf("dt40", [40, 256], F32)
    dtmp = P.sbuf("dtmp", [40, 256], F32)
    dta = P.sbuf("dta", [128, NCH, 16], F32)
    yacc = P.sbuf("yacc", [128, NCH, 256], BF16)
    cacc = [P.sbuf(f"cacc{i}", [128, 256], F32) for i in range(2)]
    hb = [P.sbuf(f"hb{i}", [128, 8, 260], BF16) for i in range(2)]
    psA = [P.psum(f"psA{i}", [128, 512]) for i in range(2)]
    psD = P.psum("psD", [128, 512])
    psY = P.psum("psY", [128, 512])
    psS = [P.psum(f"psS{i}", [128, 512]) for i in range(2)]
    psTb = [P.psum(f"psTb{i}", [128, 1024], BF16) for i in range(2)]

    for blk in range(NB):
        h = hb[blk % 2]
        hk = f"hb{blk % 2}"
        P.dma(h[:], hTp.rearrange("(c p) b t -> p c b t", p=128)[:, :, blk, :], writes=[hk])
        for cc in range(4):
            ps = psA[cc % 2]
            ca = cacc[cc % 2]; ck = f"cacc{cc % 2}"
            for kc in range(8):
                P.op("pe", lambda e, ps=ps, kc=kc, cc=cc, h=h: e.matmul(
                    ps[:, 0:260], lhsT=wxb[:, kc, cc * 128:(cc + 1) * 128], rhs=h[:, kc, :], start=(kc == 0), stop=(kc == 7)),
                    reads=[hk, "wxb"], writes=[f"psA{cc % 2}"], inc=(kc == 7))
            P.op("dve", lambda e, ps=ps, ca=ca, cc=cc: e.tensor_scalar(out=ca[:], in0=ps[:, 0:256], scalar1=cw[:, cc, 0:1], scalar2=None, op0=ALU.mult),
                 reads=[f"psA{cc % 2}", "cw"], writes=[ck])
            for k in range(1, 5):
                P.op("dve", lambda e, ps=ps, ca=ca, cc=cc, k=k: e.scalar_tensor_tensor(out=ca[:], in0=ps[:, k:k + 256], scalar=cw[:, cc, k:k + 1], in1=ca[:],
                                                                                       op0=ALU.mult, op1=ALU.add),
                     reads=[f"psA{cc % 2}", "cw", ck], writes=[ck])
            P.op("act", lambda e, ca=ca, cc=cc, blk=blk: e.activation(
                out=xbcT[:, cc, blk * 256:(blk + 1) * 256], in_=ca[:], func=AF.Silu, bias=cb[:, cc:cc + 1]),
                reads=[ck, "cb"], writes=[f"xbcT{blk}"])
        ps = psA[0]
        for kc in range(8):
            P.op("pe", lambda e, ps=ps, kc=kc, h=h: e.matmul(ps[0:40, 256:512], lhsT=wdtb[:, kc, :], rhs=h[:, kc, 2:258],
                                                              start=(kc == 0), stop=(kc == 7)),
                 reads=[hk, "wdtb"], writes=["psA0"], inc=(kc == 7))
        P.op("act", lambda e, ps=ps: e.activation(out=dtmp[:], in_=ps[0:40, 256:512], func=AF.Exp, bias=dtb[:, 0:1]),
             reads=["psA0", "dtb"], writes=["dtmp"])
        P.op("act", lambda e: e.activation(out=dt40[:], in_=dtmp[:], func=AF.Ln, bias=1.0),
             reads=["dtmp"], writes=["dt40"])
        P.op("dve", lambda e: e.tensor_scalar(out=dt40[32:40, :], in0=dt40[32:40, :], scalar1=A40[32:40, 0:1], scalar2=None, op0=ALU.mult),
             reads=["dt40", "A40"], writes=["dt40"])
        for j in range(2):
            ch = blk * 2 + j
            P.op("pe", lambda e, j=j: e.transpose(psS[0][:, j * 64:j * 64 + 40], dt40[:, j * 128:(j + 1) * 128], IDF[0:40, 0:40]),
                 reads=["dt40", "c6"], writes=["psS0"])
            P.op("dve", lambda e, j=j, ch=ch: e.tensor_copy(out=dta[:, ch, :].rearrange("p (a b) -> p a b", a=2),
                                                            in_=psS[0][:, j * 64:j * 64 + 64].rearrange("p (a b) -> p a b", a=2)[:, :, 0:8]),
                 reads=["psS0"], writes=[f"dta{ch}"])

    scr = [wst, wsc]
    sfx = ["0", "1"]
    P.op("dve", lambda e: e.memset(wsc[:], 0.0), writes=["abc1", "E1", "t11", "t31", "ST1", "cbT1"])
    tiles = []
    for d in range(2):
        w_ = scr[d]
        tiles.append(dict(
            abc=w_[:, 0, :].rearrange("p (h l) -> p h l", h=4), E=w_[:, 1, :].rearrange("p (h l) -> p h l", h=4),
            t1=w_[:, 2, 0:256].rearrange("p (h d) -> p h d", h=4), t3=w_[:, 2, 256:512].rearrange("p (h d) -> p h d", h=4),
            ST=w_[:, 3, 0:256].rearrange("p (h d) -> p h d", h=4), cbT=w_[:, 3, 256:384],
            STb=P.sbuf(f"STb{d}", [128, 4, 64], BF16), cs=P.sbuf(f"cs{d}", [128, 12], F32), ecs=P.sbuf(f"ecs{d}", [128, 12], F32),
            MT=P.sbuf(f"MT{d}", [128, 4, 128], BF16), xtok=P.sbuf(f"xtok{d}", [128, 4, 64], BF16), btok=P.sbuf(f"btok{d}", [128, 128], BF16),
            xdt=P.sbuf(f"xdt{d}", [128, 4, 64], BF16), xdd=P.sbuf(f"xdd{d}", [128, 4, 64], BF16),
            pD=(psD if d == 0 else psA[0]), kD=("psD" if d == 0 else "psA0"),
            pY=(psY if d == 0 else psA[1]), kY=("psY" if d == 0 else "psA1"),
            pS=psS[d], kS=f"psS{d}", pT=psTb[d], kT=f"psTb{d}"))
    written = set()

    def ssd_iter(d, ch):
        T = tiles[d]; x = sfx[d]
        sl = slice(ch * 128, (ch + 1) * 128)
        blkkey = f"xbcT{ch // 2}"
        a4 = dta[:, ch, 8 + 4 * d:12 + 4 * d]
        dt4 = dta[:, ch, 4 * d:4 * d + 4]
        pD, pY, pS, pT = T["pD"], T["pY"], T["pS"], T["pT"]
        kD, kY, kS, kT = T["kD"], T["kY"], T["kS"], T["kT"]
        for j in range(3):
            P.op("pe", lambda e, j=j: e.transpose(pT[:, j * 128:(j + 1) * 128], xbcT[:, j, sl], identb[:]),
                 reads=[blkkey, "identb"], writes=[kT], inc=(j == 2))
        yield
        P.op("act", lambda e: e.copy(out=T["xtok"][:].rearrange("p h d -> p (h d)"), in_=pT[:, 0:256]), reads=[kT], writes=["xtok" + x])
        yield
        P.op("act", lambda e: e.copy(out=T["btok"][:], in_=pT[:, 256:384]), reads=[kT], writes=["btok" + x])
        yield
        P.op("pe", lambda e: e.matmul(pS[:, 384:512], lhsT=xbcT[:, 2, sl], rhs=xbcT[:, 3, sl], start=True, stop=True),
             reads=[blkkey], writes=[kS])
        yield
        P.op("act", lambda e: e.copy(out=T["cbT"], in_=pS[:, 384:512]), reads=[kS], writes=["cbT" + x])
        yield
        P.op("pe", lambda e: e.matmul(pS[:, 256:260], lhsT=TRI[d], rhs=a4, start=True, stop=True),
             reads=[f"dta{ch}", "c6"], writes=[kS], inc=False)
        P.op("pe", lambda e: e.matmul(pS[:, 260:264], lhsT=ONES, rhs=a4, start=True, stop=True),
             reads=[f"dta{ch}", "c6"], writes=[kS])
        yield
        P.op("dve", lambda e: e.tensor_copy(out=T["abc"], in_=a4.unsqueeze(2).broadcast_to([128, 4, 128])),
             reads=[f"dta{ch}"], writes=["abc" + x])
        yield
        for hh in range(4):
            P.op("pe", lambda e, hh=hh: e.matmul(pD[:, hh * 128:(hh + 1) * 128], lhsT=T["abc"][:, hh, :], rhs=TRI[d],
                                                 start=(hh == 0), stop=False),
                 reads=["abc" + x, "c6"], writes=[kD], inc=False)
        P.op("pe", lambda e: e.matmul(pD[:, :], lhsT=NTRI[d], rhs=T["abc"].rearrange("p h l -> p (h l)"), start=False, stop=False),
             reads=["abc" + x, "c6"], writes=[kD], inc=False)
        P.op("pe", lambda e: e.matmul(pD[:, :], lhsT=IDF, rhs=m4[:, d, :], start=False, stop=True),
             reads=["m4", "c6"], writes=[kD])
        yield
        P.op("act", lambda e: e.activation(out=T["E"].rearrange("p h l -> p (h l)"), in_=pD[:, :], func=AF.Exp),
             reads=[kD], writes=["E" + x])
        yield
        P.op("dve", lambda e: e.tensor_tensor(out=T["MT"][:], in0=T["E"], in1=T["cbT"].unsqueeze(1).broadcast_to([128, 4, 128]), op=ALU.mult),
             reads=["E" + x, "cbT" + x], writes=["MT" + x])
        yield
        cs, ecs = T["cs"], T["ecs"]
        P.op("dve", lambda e: e.tensor_copy(out=cs[:, 0:4], in_=pS[:, 256:260]), reads=[kS], writes=["cs" + x])
        P.op("dve", lambda e: e.tensor_copy(out=cs[:, 8:12], in_=pS[:, 260:264]), reads=[kS], writes=["cs" + x])
        P.op("dve", lambda e: e.tensor_tensor(out=cs[:, 4:8], in0=cs[:, 8:12], in1=cs[:, 0:4], op=ALU.subtract),
             reads=["cs" + x], writes=["cs" + x])
        yield
        P.op("act", lambda e: e.activation(out=ecs[:], in_=cs[:], func=AF.Exp), reads=["cs" + x], writes=["ecs" + x])
        yield
        P.op("dve", lambda e: e.tensor_tensor(out=T["xdt"][:], in0=T["xtok"][:], in1=dt4.unsqueeze(2).broadcast_to([128, 4, 64]), op=ALU.mult),
             reads=["xtok" + x, f"dta{ch}"], writes=["xdt" + x])
        yield
        P.op("pool", lambda e: e.tensor_tensor(out=T["xdd"][:], in0=T["xdt"][:], in1=ecs[:, 4:8].unsqueeze(2).broadcast_to([128, 4, 64]), op=ALU.mult),
             reads=["xdt" + x, "ecs" + x], writes=["xdd" + x])
        yield
        for hh in range(4):
            P.op("pe", lambda e, hh=hh: e.matmul(pY[:, hh * 64:(hh + 1) * 64], lhsT=T["MT"][:, hh, :], rhs=T["xdt"][:, hh, :],
                                                 start=(hh == 0), stop=(hh == 3)),
                 reads=["MT" + x, "xdt" + x], writes=[kY], inc=(hh == 3))
        P.op("pe", lambda e: e.matmul(pY[:, 256:512], lhsT=xbcT[:, 3, sl], rhs=T["STb"][:].rearrange("p h d -> p (h d)"),
                                      start=True, stop=True),
             reads=[blkkey, "STb" + x], writes=[kY])
        yield
        t1 = T["t1"]; t3 = T["t3"]
        P.op("dve", lambda e: e.tensor_tensor(out=t1, in0=pY[:, 256:512].rearrange("p (h d) -> p h d", h=4),
                                              in1=ecs[:, 0:4].unsqueeze(2).broadcast_to([128, 4, 64]), op=ALU.mult),
             reads=[kY, "ecs" + x], writes=["t1" + x])
        P.op("dve", lambda e: e.tensor_tensor(out=t1, in0=t1, in1=pY[:, 0:256].rearrange("p (h d) -> p h d", h=4), op=ALU.add),
             reads=[kY, "t1" + x], writes=["t1" + x])
        yield
        ya = yacc[:, ch, :].rearrange("p (h d) -> p h d", h=4)
        if d == 0:
            P.op("pool", lambda e: e.tensor_tensor(out=t3, in0=T["xtok"][:], in1=dsk[:].unsqueeze(2).broadcast_to([128, 4, 64]), op=ALU.mult),
                 reads=["xtok" + x, "dsk"], writes=["t3" + x])
            P.op("pool", lambda e: e.tensor_tensor(out=t1, in0=t1, in1=t3, op=ALU.add), reads=["t1" + x, "t3" + x], writes=["t1" + x])
        if ch not in written:
            written.add(ch)
            P.op("dve", lambda e: e.tensor_copy(out=ya, in_=t1), reads=["t1" + x], writes=[f"yacc{ch}"])
        else:
            P.op("dve", lambda e: e.tensor_tensor(out=ya, in0=ya, in1=t1, op=ALU.add), reads=["t1" + x, f"yacc{ch}"], writes=[f"yacc{ch}"])
        yield
        P.op("pe", lambda e: e.matmul(pS[:, 0:256], lhsT=T["btok"][:], rhs=T["xdd"][:].rearrange("p h d -> p (h d)"), start=True, stop=True),
             reads=["btok" + x, "xdd" + x], writes=[kS])
        yield
        ST = T["ST"]
        P.op("dve", lambda e: e.tensor_tensor(out=ST, in0=ST, in1=ecs[:, 8:12].unsqueeze(2).broadcast_to([128, 4, 64]), op=ALU.mult),
             reads=["ST" + x, "ecs" + x], writes=["ST" + x])
        P.op("dve", lambda e: e.tensor_tensor(out=ST, in0=ST, in1=pS[:, 0:256].rearrange("p (h d) -> p h d", h=4), op=ALU.add),
             reads=["ST" + x, kS], writes=["ST" + x])
        yield
        P.op("act", lambda e: e.copy(out=T["STb"][:], in_=ST), reads=["ST" + x], writes=["STb" + x])
        yield

    orders = [list(range(NCH)), [1, 0] + list(range(NCH - 1, 1, -1))]
    for d in range(2):
        P.op("dve", lambda e, d=d: e.memset(tiles[d]["STb"][:], 0.0), writes=["STb" + sfx[d]])
    for s_ in range(NCH):
        gens = [ssd_iter(0, orders[0][s_]), ssd_iter(1, orders[1][s_])]
        alive = [True, True]
        while any(alive):
            for gi in range(2):
                if alive[gi]:
                    try:
                        next(gens[gi])
                    except StopIteration:
                        alive[gi] = False

    ycTb = [P.sbuf(f"ycTb{i}", [128, 2, 256], BF16) for i in range(2)]
    ssqb = [P.sbuf(f"ssqb{i}", [1, 256], F32) for i in range(2)]
    toks = []
    zs = wst[:, 4, :].rearrange("p (c t) -> p c t", c=2)
    g = wst[:, 5, :].rearrange("p (c t) -> p c t", c=2)
    sq = wst[:, 6, :].rearrange("p (c t) -> p c t", c=2)
    for blk in range(NB):
        h = hb[blk % 2]
        hk = f"hb{blk % 2}"
        P.dma(h[:], hTp.rearrange("(c p) b t -> p c b t", p=128)[:, :, blk, :], writes=[hk])
        for cc in range(2):
            ps = psA[cc]
            for kc in range(8):
                P.op("pe", lambda e, ps=ps, kc=kc, cc=cc, h=h: e.matmul(ps[:, 0:256], lhsT=wzb[:, kc, cc * 128:(cc + 1) * 128], rhs=h[:, kc, 2:258],
                                                                       start=(kc == 0), stop=(kc == 7)),
                     reads=[hk, "wzb"], writes=[f"psA{cc}"], inc=(kc == 7))
            P.op("act", lambda e, ps=ps, cc=cc: e.activation(out=zs[:, cc, :], in_=ps[:, 0:256], func=AF.Silu),
                 reads=[f"psA{cc}"], writes=[f"zs{cc}"])
        for j in range(2):
            ch = blk * 2 + j
            for cc in range(2):
                P.op("pe", lambda e, j=j, cc=cc, ch=ch: e.transpose(psTb[0][:, (cc * 2 + j) * 128:(cc * 2 + j + 1) * 128],
                                                                   yacc[:, ch, cc * 128:(cc + 1) * 128], identb[:]),
                     reads=[f"yacc{ch}", "identb"], writes=["psTb0"], inc=(j == 1 and cc == 1))
        for cc in range(2):
            P.op("dve", lambda e, cc=cc: e.tensor_tensor(out=g[:, cc, :], in0=psTb[0][:, cc * 256:(cc + 1) * 256], in1=zs[:, cc, :], op=ALU.mult),
                 reads=["psTb0", f"zs{cc}"], writes=[f"g{cc}"])
            P.op("act", lambda e, cc=cc: e.activation(out=sq[:, cc, :], in_=g[:, cc, :], func=AF.Square),
                 reads=[f"g{cc}"], writes=[f"sq{cc}"])
            P.op("dve", lambda e, cc=cc, blk=blk: e.tensor_scalar(out=ycTb[blk % 2][:, cc, :], in0=g[:, cc, :],
                                                                   scalar1=snw[:, cc:cc + 1], scalar2=None, op0=ALU.mult),
                 reads=[f"g{cc}", "snw"], writes=[f"ycTb{blk % 2}"])
        for cc in range(2):
            P.op("pe", lambda e, cc=cc: e.matmul(psD[0:1, 0:256], lhsT=ONES[:, 0:1], rhs=sq[:, cc, :], start=(cc == 0), stop=(cc == 1)),
                 reads=[f"sq{cc}", "c6"], writes=["psD"], inc=(cc == 1))
        P.op("act", lambda e, blk=blk: e.copy(out=ssqb[blk % 2][:], in_=psD[0:1, 0:256]),
             reads=["psD"], writes=[f"ssqb{blk % 2}"])
        toks.append(P.dma(ycT_o.rearrange("(c p) t -> p c t", p=128)[:, :, blk * 256:(blk + 1) * 256], ycTb[blk % 2][:],
                          reads=[f"ycTb{blk % 2}"], writes=[f"yo{blk}"]))
        toks.append(P.dma(ssq_o[:, blk * 256:(blk + 1) * 256], ssqb[blk % 2][:], reads=[f"ssqb{blk % 2}"], writes=[f"so{blk}"]))
    return P.finish(toks)


POOL_WINDOWS = (2, 4, 8, 16)


def _win(n, w):
    t = np.arange(n)
    return np.clip(t - w // 2, 0, n), np.clip(t + w - w // 2, 0, n)


def pool_consts(w, gw):
    lo, hi = _win(128, w)
    Ar = ((np.arange(128)[:, None] >= lo[None, :]) & (np.arange(128)[:, None] < hi[None, :])).astype(np.float32)
    BL = np.zeros((128, 16, 128), np.float32)
    for di, dl in enumerate(range(-8, 8)):
        if -(w // 2) <= dl <= w - w // 2 - 1:
            BL[:, di, :] = Ar
    normrow_l = np.broadcast_to((1.0 / (hi - lo))[None, :], (128, 128)).astype(np.float32)
    clo, chi = _win(gw, w)
    invc = np.broadcast_to((1.0 / (chi - clo))[None, :], (128, gw)).astype(np.float32)
    tlo, thi = _win(256, w)
    BC = np.zeros((128, 4, 128), np.float32)
    normrow_c = np.zeros((128, 2, 128), np.float32)
    for js in range(2):
        for jd in range(2):
            ts = 2 * np.arange(128)[:, None] + js
            td = 2 * np.arange(128)[None, :] + jd
            BC[:, js * 2 + jd, :] = ((ts >= tlo[td]) & (ts < thi[td])).astype(np.float32)
    for jd in range(2):
        td = 2 * np.arange(128) + jd
        normrow_c[:, jd, :] = (1.0 / (thi[td] - tlo[td]))[None, :]
    return {"BL": BL, "BC": BC, "normrow_l": np.ascontiguousarray(normrow_l), "invc": np.ascontiguousarray(invc),
            "normrow_c": normrow_c}


def build_s2a(gw, nc=None, pfx=""):
    P = Prog(nc, pfx)
    NT = gw + 2
    TT = NT * 128
    hTc = P.dram("hTc", [D, TT], BF16, "ExternalInput")
    wp_d = P.dram("wp", [D, 256], F32, "ExternalInput")
    pw_d = P.dram("pw", [256, 256], F32, "ExternalInput")
    ps_d = P.dram("pscT", [128, 2], F32, "ExternalInput")
    BL_d = P.dram("BL", [128, 16, 128], F32, "ExternalInput")
    BC_d = P.dram("BC", [128, 4, 128], F32, "ExternalInput")
    nl_d = P.dram("normrow_l", [128, 128], F32, "ExternalInput")
    ic_d = P.dram("invc", [128, gw], F32, "ExternalInput")
    ncx_d = P.dram("normrow_c", [128, 2, 128], F32, "ExternalInput")
    yaT_o = P.dram("yaT_o", [256, TT], BF16, "ExternalOutput")

    def load_cast(name, src_ap, shape):
        st = P.sbuf(name + "_st", shape, F32)
        bf = P.sbuf(name, shape, BF16)
        P.dma(st[:], src_ap, writes=[name + "_st"])
        P.op("dve", lambda e: e.tensor_copy(out=bf[:], in_=st[:]), reads=[name + "_st"], writes=[name])
        return bf
    wpb = load_cast("wpb", wp_d.rearrange("(c p) n -> p c n", p=128), [128, 8, 256])
    pwb = load_cast("pwb", pw_d.rearrange("(c p) n -> p c n", p=128), [128, 2, 256])
    BLb = load_cast("BLb", BL_d, [128, 16, 128])
    BCb = load_cast("BCb", BC_d, [128, 4, 128])
    psc = P.sbuf("psc", [128, 2], F32)
    nl = P.sbuf("nl", [128, 128], F32)
    ic = P.sbuf("ic", [128, gw], F32)
    ncx = P.sbuf("ncx", [128, 2, 128], F32)
    for t_, d_, k_ in ((psc, ps_d, "pscale"), (nl, nl_d, "nl"), (ic, ic_d, "ic"), (ncx, ncx_d, "ncx")):
        P.dma(t_[:], d_, writes=[k_])
    Xp = P.sbuf("Xp", [128, NT, 256], BF16)
    XpT = P.sbuf("XpT", [128, 2, TT], BF16)
    yaT = P.sbuf("yaT", [128, 2, TT], BF16)
    dT = [P.sbuf(f"dT{i}", [128, 2, 128], BF16) for i in range(2)]
    tmp = P.sbuf("ptmp", [128, 128], F32)
    hb = [P.sbuf(f"hbp{i}", [128, 8, 128], BF16) for i in range(2)]
    psX = [P.psum(f"psX{i}", [128, 512]) for i in range(2)]
    psXT = [P.psum(f"psXT{i}", [128, 512]) for i in range(2)]
    psP = [P.psum(f"psP{i}", [128, 512]) for i in range(2)]
    psO = [P.psum(f"psO{i}", [128, 512]) for i in range(2)]
    for ti in range(NT):
        h = hb[ti % 2]; hk = f"hbp{ti % 2}"
        P.dma(h[:], hTc.rearrange("(c p) t -> p c t", p=128)[:, :, ti * 128:(ti + 1) * 128], writes=[hk])
        ps = psX[ti % 2]
        for kc in range(8):
            P.op("pe", lambda e, ps=ps, kc=kc, h=h: e.matmul(ps[:, 0:256], lhsT=h[:, kc, :], rhs=wpb[:, kc, :], start=(kc == 0), stop=(kc == 7)),
                 reads=[hk, "wpb"], writes=[f"psX{ti % 2}"], inc=(kc == 7))
        P.op("act", lambda e, ps=ps, ti=ti: e.copy(out=Xp[:, ti, :], in_=ps[:, 0:256]), reads=[f"psX{ti % 2}"], writes=[f"Xp{ti}"])
        ps2 = psXT[ti % 2]
        for cc in range(2):
            for kc in range(8):
                P.op("pe", lambda e, ps2=ps2, kc=kc, cc=cc, h=h: e.matmul(ps2[:, cc * 128:(cc + 1) * 128], lhsT=wpb[:, kc, cc * 128:(cc + 1) * 128],
                                                                         rhs=h[:, kc, :], start=(kc == 0), stop=(kc == 7)),
                     reads=[hk, "wpb"], writes=[f"psXT{ti % 2}"], inc=(kc == 7 and cc == 1))
        P.op("dve", lambda e, ps2=ps2, ti=ti: e.tensor_copy(out=XpT[:, :, ti * 128:(ti + 1) * 128],
                                                            in_=ps2[:, 0:256].rearrange("p (c t) -> p c t", c=2)),
             reads=[f"psXT{ti % 2}"], writes=[f"XpT{ti}"])
    for ti in range(NT):
        if ti < gw:
            srcs = [(ti + dl, BLb[:, dl + 8, :]) for dl in range(-8, 8) if 0 <= ti + dl < gw]
            nrow = nl[:]
            sc = ic[:, ti:ti + 1]
        else:
            jd = ti - gw
            srcs = [(gw + js, BCb[:, js * 2 + jd, :]) for js in range(2)]
            nrow = ncx[:, jd, :]
            sc = None
        d = dT[ti % 2]; dk = f"dT{ti % 2}"
        for cc in range(2):
            ps = psP[cc]
            for i, (src, bm) in enumerate(srcs):
                P.op("pe", lambda e, ps=ps, src=src, bm=bm, cc=cc, i=i, n=len(srcs): e.matmul(
                    ps[:, 0:128], lhsT=Xp[:, src, cc * 128:(cc + 1) * 128], rhs=bm, start=(i == 0), stop=(i == n - 1)),
                    reads=[f"Xp{src}", "BLb", "BCb"], writes=[f"psP{cc}"], inc=(i == len(srcs) - 1))
            if sc is not None:
                P.op("dve", lambda e, ps=ps, sc=sc, nrow=nrow: e.scalar_tensor_tensor(out=tmp[:], in0=ps[:, 0:128], scalar=sc, in1=nrow,
                                                                                        op0=ALU.mult, op1=ALU.mult),
                     reads=[f"psP{cc}", "ic", "nl"], writes=["ptmp"])
            else:
                P.op("dve", lambda e, ps=ps, nrow=nrow: e.tensor_tensor(out=tmp[:], in0=ps[:, 0:128], in1=nrow, op=ALU.mult),
                     reads=[f"psP{cc}", "ncx"], writes=["ptmp"])
            P.op("dve", lambda e, d=d, cc=cc, ti=ti: e.tensor_tensor(out=d[:, cc, :], in0=tmp[:], in1=XpT[:, cc, ti * 128:(ti + 1) * 128], op=ALU.subtract),
                 reads=["ptmp", f"XpT{ti}"], writes=[dk])
        for dc in range(2):
            ps = psO[dc]
            for cc in range(2):
                P.op("pe", lambda e, ps=ps, cc=cc, dc=dc, d=d: e.matmul(ps[:, 0:128], lhsT=pwb[:, cc, dc * 128:(dc + 1) * 128], rhs=d[:, cc, :],
                                                                       start=(cc == 0), stop=(cc == 1)),
                     reads=[dk, "pwb"], writes=[f"psO{dc}"], inc=(cc == 1))
            P.op("act", lambda e, ps=ps, dc=dc, ti=ti: e.activation(out=yaT[:, dc, ti * 128:(ti + 1) * 128], in_=ps[:, 0:128], func=AF.Identity,
                                                                    scale=psc[:, dc:dc + 1]),
                 reads=[f"psO{dc}", "pscale"], writes=[f"yaT{ti}"])
    toks = [P.dma(yaT_o[dc * 128:(dc + 1) * 128, :], yaT[:, dc, :], reads=[f"yaT{t}" for t in range(NT)], writes=[f"yao{dc}"]) for dc in range(2)]
    return P.finish(toks)


def fourier_consts(n2):
    n = 128 * n2
    j1 = np.arange(128)[:, None]; k1 = np.arange(128)[None, :]
    MA = np.zeros((128, n2, 256), np.float32)
    for j2 in range(n2):
        ang = -2 * np.pi * ((j1 * k1) / 128.0 + (j2 * k1) / float(n))
        MA[:, j2, :128] = np.cos(ang); MA[:, j2, 128:] = np.sin(ang)
    kl = 128 // n2
    MC = np.zeros((128, 2, 128), np.float32)
    sc = 1.0 / np.sqrt(n * 256.0)
    for a in range(kl):
        for j2 in range(n2):
            for k2 in range(n2):
                ang = -2 * np.pi * j2 * k2 / n2
                MC[a * n2 + j2, 0, a * n2 + k2] = np.cos(ang) * sc
                MC[a * n2 + j2, 1, a * n2 + k2] = -np.sin(ang) * sc
    return MA, MC


def fourier_fb():
    c = np.arange(256)[:, None]; mm = np.arange(256)[None, :]
    ang = -2 * np.pi * c * mm / 256.0
    C_, S_ = np.cos(ang), np.sin(ang)
    FB = np.zeros((128, 2, 2, 512), np.float32)
    for cc in range(2):
        sl = slice(cc * 128, (cc + 1) * 128)
        FB[:, 0, cc, :256] = C_[sl]; FB[:, 0, cc, 256:] = S_[sl]
        FB[:, 1, cc, :256] = -S_[sl]; FB[:, 1, cc, 256:] = C_[sl]
    return FB


def build_s2b(n2l, nc=None, pfx=""):
    P = Prog(nc, pfx)
    NT = n2l + 2
    TT = NT * 128
    hTc = P.dram("hTc", [D, TT], BF16, "ExternalInput")
    wf_d = P.dram("wf", [D, 256], F32, "ExternalInput")
    MAl_d = P.dram("MA_l", [128, n2l, 256], F32, "ExternalInput")
    MAc_d = P.dram("MA_c", [128, 2, 256], F32, "ExternalInput")
    MCl_d = P.dram("MC_l", [128, 2, 128], F32, "ExternalInput")
    MCc_d = P.dram("MC_c", [128, 2, 128], F32, "ExternalInput")
    FB_d = P.dram("FB", [128, 2, 2, 512], F32, "ExternalInput")
    ybT_o = P.dram("ybT_o", [256, TT], BF16, "ExternalOutput")

    stg = [P.sbuf(f"stg{i}", [128, 2048], F32) for i in range(2)]
    nst = [0]

    def load_cast(name, src_ap, shape):
        bf = P.sbuf(name, shape, BF16)
        A = shape[1]
        rest = int(np.prod(shape[2:]))
        grp = max(1, 2048 // rest)
        for a0 in range(0, A, grp):
            g_ = min(grp, A - a0)
            s = stg[nst[0] % 2]; sk = f"stg{nst[0] % 2}"; nst[0] += 1
            if len(shape) == 3:
                sv = s[:, 0:g_ * rest].rearrange("p (a b) -> p a b", a=g_)
            else:
                sv = s[:, 0:g_ * rest].rearrange("p (a b c) -> p a b c", a=g_, b=shape[2])
            P.dma(sv, src_ap[:, a0:a0 + g_], writes=[sk])
            P.op("dve", lambda e, sv=sv, a0=a0, g_=g_: e.tensor_copy(out=bf[:, a0:a0 + g_], in_=sv), reads=[sk], writes=[name])
        return bf
    wfb = load_cast("wfb", wf_d.rearrange("(c p) n -> p c n", p=128), [128, 8, 256])
    MAl = load_cast("MAl", MAl_d, [128, n2l, 256])
    MAc = load_cast("MAc", MAc_d, [128, 2, 256])
    MCl = load_cast("MCl", MCl_d, [128, 2, 128])
    MCc = load_cast("MCc", MCc_d, [128, 2, 128])
    FBb = load_cast("FBb", FB_d, [128, 2, 2, 512])
    Xf = P.sbuf("Xf", [128, NT, 256], BF16)
    Zl = P.sbuf("Zl", [128, 2, 2, 128 * n2l], BF16)
    Zc = P.sbuf("Zc", [128, 2, 2, 256], BF16)
    ybT = P.sbuf("ybT", [128, 2, TT], BF16)
    U = [P.sbuf(f"U{i}", [128, 512], BF16) for i in range(2)]
    hb = [P.sbuf(f"hbf{i}", [128, 8, 128], BF16) for i in range(2)]
    psX = [P.psum(f"psX{i}", [128, 512]) for i in range(2)]
    psA = [P.psum(f"psA{i}", [128, 512]) for i in range(2)]
    psB = [P.psum(f"psB{i}", [128, 512]) for i in range(2)]
    psC = [P.psum(f"psC{i}", [128, 512]) for i in range(2)]
    for ti in range(NT):
        h = hb[ti % 2]; hk = f"hbf{ti % 2}"
        P.dma(h[:], hTc.rearrange("(c p) t -> p c t", p=128)[:, :, ti * 128:(ti + 1) * 128], writes=[hk])
        ps = psX[ti % 2]
        for kc in range(8):
            P.op("pe", lambda e, ps=ps, kc=kc, h=h: e.matmul(ps[:, 0:256], lhsT=h[:, kc, :], rhs=wfb[:, kc, :], start=(kc == 0), stop=(kc == 7)),
                 reads=[hk, "wfb"], writes=[f"psX{ti % 2}"], inc=(kc == 7))
        P.op("act", lambda e, ps=ps, ti=ti: e.copy(out=Xf[:, ti, :], in_=ps[:, 0:256]), reads=[f"psX{ti % 2}"], writes=[f"Xf{ti}"])
        lat = ti < n2l
        j2 = ti if lat else ti - n2l
        n2 = n2l if lat else 2
        ma = MAl[:, j2, :] if lat else MAc[:, j2, :]
        Z = Zl if lat else Zc
        zk = "Zl" if lat else "Zc"
        for cc in range(2):
            pa = psA[cc]
            P.op("pe", lambda e, pa=pa, cc=cc, ti=ti, ma=ma: e.matmul(pa[:, 0:256], lhsT=Xf[:, ti, cc * 128:(cc + 1) * 128], rhs=ma, start=True, stop=True),
                 reads=[f"Xf{ti}", "MAl", "MAc"], writes=[f"psA{cc}"])
            dst = Z[:, cc, :, :].rearrange("p r (k j) -> p r k j", j=n2)[:, :, :, j2]
            eng = "dve" if cc == 0 else "act"
            if eng == "dve":
                P.op("dve", lambda e, pa=pa, dst=dst: e.tensor_copy(out=dst, in_=pa[:, 0:256].rearrange("p (r k) -> p r k", r=2)),
                     reads=[f"psA{cc}"], writes=[zk])
            else:
                P.op("act", lambda e, pa=pa, dst=dst: e.copy(out=dst, in_=pa[:, 0:256].rearrange("p (r k) -> p r k", r=2)),
                     reads=[f"psA{cc}"], writes=[zk])
    qi = 0
    for seg in range(2):
        lat = seg == 0
        n2 = n2l if lat else 2
        Z = Zl if lat else Zc
        zk = "Zl" if lat else "Zc"
        MC = MCl if lat else MCc
        kl = 128 // n2
        base = 0 if lat else n2l * 128
        for q in range(n2):
            pb = psB[qi % 2]; u = U[qi % 2]; uk = f"U{qi % 2}"
            i = 0
            for part in range(2):
                for cc in range(2):
                    P.op("pe", lambda e, pb=pb, part=part, cc=cc, q=q, i=i, Z=Z: e.matmul(
                        pb[:, :], lhsT=Z[:, cc, part, q * 128:(q + 1) * 128], rhs=FBb[:, part, cc, :], start=(i == 0), stop=(i == 3)),
                        reads=[zk, "FBb"], writes=[f"psB{qi % 2}"], inc=(i == 3))
                    i += 1
            P.op("act", lambda e, pb=pb, u=u: e.copy(out=u[:], in_=pb[:, :]), reads=[f"psB{qi % 2}"], writes=[uk])
            for mc in range(2):
                pc = psC[mc]
                P.op("pe", lambda e, pc=pc, u=u, mc=mc, MC=MC: e.matmul(pc[:, 0:128], lhsT=u[:, mc * 128:(mc + 1) * 128], rhs=MC[:, 0, :], start=True, stop=False),
                     reads=[uk, "MCl", "MCc"], writes=[f"psC{mc}"], inc=False)
                P.op("pe", lambda e, pc=pc, u=u, mc=mc, MC=MC: e.matmul(pc[:, 0:128], lhsT=u[:, 256 + mc * 128:256 + (mc + 1) * 128], rhs=MC[:, 1, :], start=False, stop=True),
                     reads=[uk, "MCl", "MCc"], writes=[f"psC{mc}"])
                dst = ybT[:, mc, base:base + 128 * n2].rearrange("p (k2 k1) -> p k1 k2", k1=128)[:, q * kl:(q + 1) * kl, :]
                P.op("dve", lambda e, pc=pc, dst=dst, n2=n2: e.tensor_copy(out=dst, in_=pc[:, 0:128].rearrange("p (a b) -> p a b", b=n2)),
                     reads=[f"psC{mc}"], writes=[f"ybT{seg}_{q}"])
            qi += 1
    allk = [f"ybT0_{q}" for q in range(n2l)] + [f"ybT1_{q}" for q in range(2)]
    toks = [P.dma(ybT_o[mc * 128:(mc + 1) * 128, :], ybT[:, mc, :], reads=allk, writes=[f"ybo{mc}"]) for mc in range(2)]
    return P.finish(toks)


def build_s3():
    P = Prog()
    lat_d = P.dram("lat", [TS_T, D], F32, "ExternalInput")
    hT_d = P.dram("hT", [D, TS_T], BF16, "ExternalInput")
    yT_d = P.dram("yT", [3, D, TS_T], BF16, "ExternalInput")
    ssq_d = P.dram("ssq4", [4, TS_T], F32, "ExternalInput")
    wg_d = P.dram("wg", [D, 3 * D], F32, "ExternalInput")
    wb_d = P.dram("wb", [3, D, D], F32, "ExternalInput")
    wo_d = P.dram("wo", [D, D], F32, "ExternalInput")
    g1_d = P.dram("g1b", [2, 128, D], F32, "ExternalInput")
    nwT = P.dram("nwT", [128, 8], F32, "ExternalInput")
    scT = P.dram("scT", [128, 8, 2], F32, "ExternalInput")
    shT = P.dram("shT", [128, 8, 2], F32, "ExternalInput")
    rw_d = P.dram("rw", [D, 32], F32, "ExternalInput")
    rb_d = P.dram("rbb", [128, 32], F32, "ExternalInput")
    id_d = P.dram("ident", [128, 128], F32, "ExternalInput")
    on_d = P.dram("ones4", [4, 128], F32, "ExternalInput")
    lat_o = P.dram("lat_o", [TS_T, D], F32, "ExternalOutput")
    h2T_o = P.dram("h2T_o", [D, TS_T], BF16, "ExternalOutput")
    wgt_o = P.dram("wgt_o", [TS_T, 32], F32, "ExternalOutput")

    wgb = P.sbuf("wgb", [128, 8, 3 * D], BF16)
    wbb = P.sbuf("wbb", [128, 3, 8, D], BF16)
    wob = P.sbuf("wob", [128, 8, D], BF16)
    P.dma(wgb[:], wg_d.rearrange("(c p) n -> p c n", p=128), writes=["wgb"], eng="pool")
    for k in range(3):
        P.dma(wbb[:, k], wb_d[k].rearrange("(c p) n -> p c n", p=128), writes=["wbb"], eng="pool")
    P.dma(wob[:], wo_d.rearrange("(c p) n -> p c n", p=128), writes=["wob"], eng="pool")
    ident = P.sbuf("ident_s", [128, 128], F32)
    P.dma(ident[:], id_d, writes=["ident"])
    ones4 = P.sbuf("ones4s", [4, 128], F32)
    P.dma(ones4[:], on_d, writes=["ones4"])
    modA, modB = load_modAB(P, nwT, scT, shT)
    g1 = P.sbuf("g1", [128, 2, D], F32)
    P.dma(g1[:], g1_d.rearrange("w p d -> p w d"), writes=["g1"])
    rw = P.sbuf("rws", [128, 8, 32], F32)
    P.dma(rw[:], rw_d.rearrange("(c p) n -> p c n", p=128), writes=["rw"])
    rb = P.sbuf("rbs", [128, 32], F32)
    P.dma(rb[:], rb_d, writes=["rb"])

    hTt = [P.sbuf("hTt0", [128, 8, 512], BF16)]
    yTt = [P.sbuf("yTt0", [128, 3, 8, 512], BF16)]
    latt = [P.sbuf(f"latt{i}", [128, D], F32) for i in range(2)]
    ssqt = P.sbuf("ssqt", [4, 512], F32)
    rstdb = P.sbuf("rstdb", [128, 512], F32)
    sg = [P.sbuf(f"sg{i}", [128, 512], F32) for i in range(2)]
    term = [P.sbuf(f"term{i}", [128, 512], F32) for i in range(3)]
    mT = P.sbuf("mT", [128, 8, 512], BF16)
    tmpo = P.sbuf("tmpo", [128, 512], F32)
    h2f = P.sbuf("h2f", [128, 8, 128], F32)
    h2b = [P.sbuf(f"h2b{i}", [128, 8, 128], BF16) for i in range(2)]
    wgt = P.sbuf("wgt", [128, TS_NT, 32], F32)
    lg = P.sbuf("lg", [128, 32], F32)
    m8 = P.sbuf("m8", [128, 8], F32)
    nmax = P.sbuf("nmax", [128, 1], F32)
    msk = P.sbuf("msk", [128, 32], F32)
    ex = P.sbuf("ex", [128, 32], F32)
    ssum = P.sbuf("ssum", [128, 1], F32)
    scr = (P.sbuf("junk", [128, D], BF16), P.sbuf("ssq", [128, 1], F32), P.sbuf("rstd", [128, 1], F32),
           P.sbuf("xn", [128, D], F32))
    psG = [P.psum(f"psG{i}", [128, 512]) for i in range(2)]
    psPj = [P.psum(f"psPj{i}", [128, 512]) for i in range(2)]
    psO = P.psum("psO", [128, 512])
    psR = P.psum("psR", [128, 512])
    ps_pair = [P.psum(f"psT{i}", [128, 512]) for i in range(2)]
    toks = []
    hv = hT_d.rearrange("(c p) t -> p c t", p=128)
    yv = yT_d.rearrange("k (c p) t -> p k c t", p=128)
    n = 0
    for (t0, nt) in [(0, 512), (512, 512), (1024, 512), (1536, 512), (2048, 128)]:
      bs_ = slice(t0, t0 + nt)
      ht = hTt[0]; hk = "hTt0"
      yt = yTt[0]; yk = "yTt0"
      P.dma(ht[:, :, 0:nt], hv[:, :, bs_], writes=[hk])
      for k in range(3):
          P.dma(yt[:, k, :, 0:nt], yv[:, k, :, bs_], writes=[yk])
      P.dma(ssqt[:, 0:nt], ssq_d[:, bs_], writes=["ssqt"])
      P.op("pe", lambda e, nt=nt: e.matmul(psR[:, 0:nt], lhsT=ones4[:], rhs=ssqt[:, 0:nt], start=True, stop=True),
           reads=["ones4", "ssqt"], writes=["psR"])
      P.op("act", lambda e, nt=nt: e.activation(out=rstdb[:, 0:nt], in_=psR[:, 0:nt], func=AF.Sqrt, scale=1.0 / D, bias=EPS),
           reads=["psR"], writes=["rstdb"])
      P.op("dve", lambda e, nt=nt: e.reciprocal(out=rstdb[:, 0:nt], in_=rstdb[:, 0:nt]), reads=["rstdb"], writes=["rstdb"])
      for dc in range(8):
          for k in range(3):
              pg = psG[n % 2]; pp = psPj[n % 2]; s_ = sg[n % 2]
              for kc in range(8):
                  P.op("pe", lambda e, pg=pg, kc=kc, k=k, dc=dc, nt=nt: e.matmul(
                      pg[:, 0:nt], lhsT=wgb[:, kc, k * D + dc * 128:k * D + (dc + 1) * 128], rhs=ht[:, kc, 0:nt], start=(kc == 0), stop=(kc == 7)),
                      reads=[hk, "wgb"], writes=[f"psG{n % 2}"], inc=(kc == 7))
              P.op("act", lambda e, pg=pg, s_=s_, nt=nt: e.activation(out=s_[:, 0:nt], in_=pg[:, 0:nt], func=AF.Sigmoid),
                   reads=[f"psG{n % 2}"], writes=[f"sg{n % 2}"])
              for wc in range(8):
                  P.op("pe", lambda e, pp=pp, wc=wc, k=k, dc=dc, nt=nt: e.matmul(
                      pp[:, 0:nt], lhsT=wbb[:, k, wc, dc * 128:(dc + 1) * 128], rhs=yt[:, k, wc, 0:nt], start=(wc == 0), stop=(wc == 7)),
                      reads=[yk, "wbb"], writes=[f"psPj{n % 2}"], inc=(wc == 7))
              P.op("dve", lambda e, pp=pp, s_=s_, k=k, nt=nt: e.tensor_tensor(out=term[k][:, 0:nt], in0=pp[:, 0:nt], in1=s_[:, 0:nt], op=ALU.mult),
                   reads=[f"psPj{n % 2}", f"sg{n % 2}"], writes=[f"term{k}"])
              n += 1
          P.op("pool", lambda e, nt=nt: e.tensor_tensor(out=term[2][:, 0:nt], in0=term[2][:, 0:nt], in1=rstdb[:, 0:nt], op=ALU.mult),
               reads=["term2", "rstdb"], writes=["term2"])
          P.op("pool", lambda e, nt=nt: e.tensor_tensor(out=term[0][:, 0:nt], in0=term[0][:, 0:nt], in1=term[1][:, 0:nt], op=ALU.add),
               reads=["term0", "term1"], writes=["term0"])
          P.op("pool", lambda e, dc=dc, nt=nt: e.tensor_tensor(out=mT[:, dc, 0:nt], in0=term[0][:, 0:nt], in1=term[2][:, 0:nt], op=ALU.add),
               reads=["term0", "term2"], writes=[f"mT{dc}"])
      for j_ in range(nt // 128):
        t = t0 // 128 + j_
        which = 1 if t == TS_NT - 1 else 0
        ts_ = slice(t * 128, (t + 1) * 128)
        js_ = slice(j_ * 128, (j_ + 1) * 128)
        lt = latt[t % 2]; lk = f"latt{t % 2}"
        P.dma(lt[:], lat_d[ts_, :], writes=[lk])
        for half in range(2):
            for dc in range(8):
                P.op("pe", lambda e, dc=dc, half=half, js_=js_: e.matmul(psO[:, :], lhsT=mT[:, dc, js_], rhs=wob[:, dc, half * 512:(half + 1) * 512],
                                                               start=(dc == 0), stop=(dc == 7)),
                     reads=[f"mT{dc}", "wob"], writes=["psO"], inc=(dc == 7))
            P.op("dve", lambda e, half=half, which=which: e.tensor_tensor(out=tmpo[:], in0=psO[:, :], in1=g1[:, which, half * 512:(half + 1) * 512], op=ALU.mult),
                 reads=["psO", "g1"], writes=["tmpo"])
            P.op("pool", lambda e, half=half, lt=lt: e.tensor_tensor(out=lt[:, half * 512:(half + 1) * 512], in0=lt[:, half * 512:(half + 1) * 512], in1=tmpo[:], op=ALU.add),
                 reads=["tmpo", lk], writes=[lk])
        toks.append(P.dma(lat_o[ts_, :], lt[:], reads=[lk], writes=[f"lato{t}"]))
        emit_norm_T(P, lt[:], lk, modA, modB, which, h2f, "h2f", ident, ps_pair, t, scr)
        hb_ = h2b[t % 2]
        P.op("pool", lambda e, hb_=hb_: e.tensor_copy(out=hb_[:], in_=h2f[:]), reads=[f"h2f_{c}" for c in range(8)], writes=[f"h2b{t % 2}"])
        toks.append(P.dma(h2T_o.rearrange("(c p) t -> p c t", p=128)[:, :, ts_], hb_[:], reads=[f"h2b{t % 2}"], writes=[f"h2o{t}"]))
        for kc in range(8):
            P.op("pe", lambda e, kc=kc: e.matmul(psR[:, 128:160], lhsT=h2f[:, kc, :], rhs=rw[:, kc, :], start=(kc == 0), stop=(kc == 7)),
                 reads=[f"h2f_{kc}", "rw"], writes=["psR"], inc=(kc == 7))
        P.op("dve", lambda e: e.tensor_tensor(out=lg[:], in0=psR[:, 128:160], in1=rb[:], op=ALU.add), reads=["psR", "rb"], writes=["lg"])
        P.op("dve", lambda e: e.max(out=m8[:], in_=lg[:]), reads=["lg"], writes=["m8"])
        P.op("dve", lambda e: e.tensor_scalar(out=msk[:], in0=lg[:], scalar1=m8[:, 3:4], scalar2=None, op0=ALU.is_ge), reads=["lg", "m8"], writes=["msk"])
        P.op("dve", lambda e: e.tensor_scalar(out=nmax[:], in0=m8[:, 0:1], scalar1=-1.0, scalar2=None, op0=ALU.mult), reads=["m8"], writes=["nmax"])
        P.op("act", lambda e: e.activation(out=ex[:], in_=lg[:], func=AF.Exp, bias=nmax[:, 0:1]), reads=["lg", "nmax"], writes=["ex"])
        P.op("dve", lambda e: e.tensor_tensor(out=ex[:], in0=ex[:], in1=msk[:], op=ALU.mult), reads=["ex", "msk"], writes=["ex"])
        P.op("dve", lambda e: e.reduce_sum(out=ssum[:], in_=ex[:], axis=AX.X), reads=["ex"], writes=["ssum"])
        P.op("dve", lambda e: e.reciprocal(out=ssum[:], in_=ssum[:]), reads=["ssum"], writes=["ssum"])
        P.op("dve", lambda e, t=t: e.tensor_scalar(out=wgt[:, t, :], in0=ex[:], scalar1=ssum[:, 0:1], scalar2=None, op0=ALU.mult),
             reads=["ex", "ssum"], writes=[f"wgt{t}"])
    toks.append(P.dma(wgt_o.rearrange("(t p) e -> p t e", p=128), wgt[:], reads=[f"wgt{t}" for t in range(TS_NT)], writes=["wgto"]))
    return P.finish(toks)


SW_ALPHA = 1.702
SW_LIMIT = 7.0
NTOK_ALL = NCORE * TS_T


def build_s4(ntok):
    P = Prog()
    NBLK = ntok // 1024
    h2T_d = P.dram("h2T", [D, ntok], BF16, "ExternalInput")
    ws_d = P.dram("wsel", [128, ntok // 128, 4], F32, "ExternalInput")
    w1_d = P.dram("w1", [4, D, 2 * D], F32, "ExternalInput")
    b1_d = P.dram("b1T", [128, 4, 16], F32, "ExternalInput")
    w2_d = P.dram("w2", [4, D, D], F32, "ExternalInput")
    b2_d = P.dram("b2", [1, 4, D], F32, "ExternalInput")
    part_o = P.dram("part_o", [ntok, D], BF16, "ExternalOutput")
    w1s = P.nc.dram_tensor("w1s", [4, D, 2 * D], BF16).ap()
    w2s = P.nc.dram_tensor("w2s", [4, D, D], BF16).ap()

    w1b = [P.sbuf(f"w1b{i}", [128, 8, 2 * D], BF16) for i in range(2)]
    w2b = [P.sbuf(f"w2b{i}", [128, 8, D], BF16) for i in range(2)]
    for e in range(4):
        P.dma(w1b[e % 2][:], w1_d[e].rearrange("(c p) n -> p c n", p=128), writes=[f"w1b{e % 2}"], eng="pool")
        P.dma(w1s[e].rearrange("(c p) n -> p c n", p=128), w1b[e % 2][:], reads=[f"w1b{e % 2}"], writes=[f"w1s{e}"])
        P.dma(w2b[e % 2][:], w2_d[e].rearrange("(c p) n -> p c n", p=128), writes=[f"w2b{e % 2}"], eng="pool")
        P.dma(w2s[e].rearrange("(c p) n -> p c n", p=128), w2b[e % 2][:], reads=[f"w2b{e % 2}"], writes=[f"w2s{e}"])
    b1 = P.sbuf("b1s", [128, 4, 16], F32)
    P.dma(b1[:], b1_d, writes=["b1"])
    b2b = P.sbuf("b2b", [1, 4, D], BF16)
    P.dma(b2b[:], b2_d, writes=["b2b"], eng="pool")
    onesb = P.sbuf("onesb", [1, 128], BF16)
    P.op("dve", lambda e: e.memset(onesb[:], 1.0), writes=["onesb"])
    ws = P.sbuf("wss", [128, ntok // 128, 4], F32)
    P.dma(ws[:], ws_d, writes=["ws"])

    hblk = [P.sbuf(f"hblk{i}", [128, 8, 1024], BF16) for i in range(2)]
    acc = P.sbuf("acc", [128, 8, D], F32)
    accb = P.sbuf("accb", [128, 2, D], BF16)
    actT = P.sbuf("actT", [128, 8, 1024], BF16)
    gsb = [P.sbuf(f"gsb{i}", [128, 512], F32) for i in range(2)]
    sgb = [P.sbuf(f"sgb{i}", [128, 512], F32) for i in range(2)]
    lsb = [P.sbuf(f"lsb{i}", [128, 512], F32) for i in range(2)]
    psGt = [P.psum(f"psGt{i}", [128, 512]) for i in range(2)]
    psLn = [P.psum(f"psLn{i}", [128, 512]) for i in range(2)]
    psDn = [P.psum(f"psDn{i}", [128, 512]) for i in range(2)]
    toks = []
    n = 0
    nd = 0
    wi = 0
    for blk in range(NBLK):
        hb = hblk[blk % 2]; hk = f"hblk{blk % 2}"
        P.dma(hb[:], h2T_d.rearrange("(c p) t -> p c t", p=128)[:, :, blk * 1024:(blk + 1) * 1024], writes=[hk])
        for e in range(4):
            wa = w1b[wi % 2]; wb_ = w2b[wi % 2]; k1 = f"w1b{wi % 2}"; k2 = f"w2b{wi % 2}"
            wi += 1
            P.dma(wa[:], w1s[e].rearrange("(c p) n -> p c n", p=128), reads=[f"w1s{e}"], writes=[k1])
            P.dma(wb_[:], w2s[e].rearrange("(c p) n -> p c n", p=128), reads=[f"w2s{e}"], writes=[k2])
            for half in range(2):
                hs = slice(half * 512, (half + 1) * 512)
                for fc in range(8):
                    pg = psGt[n % 2]; pl = psLn[n % 2]; g_ = gsb[n % 2]; s_ = sgb[n % 2]; l_ = lsb[n % 2]
                    i2 = n % 2
                    for kc in range(8):
                        P.op("pe", lambda e_, pg=pg, kc=kc, fc=fc, wa=wa, hb=hb, hs=hs: e_.matmul(
                            pg[:, :], lhsT=wa[:, kc, fc * 128:(fc + 1) * 128], rhs=hb[:, kc, hs], start=(kc == 0), stop=(kc == 7)),
                            reads=[hk, k1], writes=[f"psGt{i2}"], inc=(kc == 7))
                    for kc in range(8):
                        P.op("pe", lambda e_, pl=pl, kc=kc, fc=fc, wa=wa, hb=hb, hs=hs: e_.matmul(
                            pl[:, :], lhsT=wa[:, kc, D + fc * 128:D + (fc + 1) * 128], rhs=hb[:, kc, hs], start=(kc == 0), stop=(kc == 7)),
                            reads=[hk, k1], writes=[f"psLn{i2}"], inc=(kc == 7))
                    P.op("dve", lambda e_, pg=pg, g_=g_, e=e, fc=fc: e_.tensor_scalar(out=g_[:], in0=pg[:, :], scalar1=b1[:, e, fc:fc + 1], scalar2=SW_LIMIT,
                                                                                    op0=ALU.add, op1=ALU.min),
                         reads=[f"psGt{i2}", "b1"], writes=[f"gsb{i2}"])
                    P.op("act", lambda e_, g_=g_, s_=s_: e_.activation(out=s_[:], in_=g_[:], func=AF.Sigmoid, scale=SW_ALPHA),
                         reads=[f"gsb{i2}"], writes=[f"sgb{i2}"])
                    P.op("dve", lambda e_, pl=pl, l_=l_, e=e, fc=fc: e_.tensor_scalar(out=l_[:], in0=pl[:, :], scalar1=b1[:, e, 8 + fc:9 + fc], scalar2=SW_LIMIT,
                                                                                    op0=ALU.add, op1=ALU.min),
                         reads=[f"psLn{i2}", "b1"], writes=[f"lsb{i2}"])
                    P.op("dve", lambda e_, l_=l_: e_.tensor_scalar(out=l_[:], in0=l_[:], scalar1=-SW_LIMIT, scalar2=1.0, op0=ALU.max, op1=ALU.add),
                         reads=[f"lsb{i2}"], writes=[f"lsb{i2}"])
                    P.op("pool", lambda e_, g_=g_, s_=s_: e_.tensor_tensor(out=g_[:], in0=g_[:], in1=s_[:], op=ALU.mult),
                         reads=[f"gsb{i2}", f"sgb{i2}"], writes=[f"gsb{i2}"])
                    P.op("dve", lambda e_, g_=g_, l_=l_, fc=fc, hs=hs: e_.tensor_tensor(out=actT[:, fc, hs], in0=g_[:], in1=l_[:], op=ALU.mult),
                         reads=[f"gsb{i2}", f"lsb{i2}"], writes=[f"actT{fc}_{half}"])
                    n += 1
            for tl in range(8):
                half = tl // 4
                for dh in range(2):
                    pd = psDn[nd % 2]; i3 = nd % 2
                    for fc in range(8):
                        P.op("pe", lambda e_, pd=pd, fc=fc, tl=tl, dh=dh, wb_=wb_: e_.matmul(
                            pd[:, :], lhsT=actT[:, fc, tl * 128:(tl + 1) * 128], rhs=wb_[:, fc, dh * 512:(dh + 1) * 512], start=(fc == 0), stop=False),
                            reads=[f"actT{fc}_{half}", k2], writes=[f"psDn{i3}"], inc=False)
                    P.op("pe", lambda e_, pd=pd, e=e, dh=dh: e_.matmul(pd[:, :], lhsT=onesb[:], rhs=b2b[:, e, dh * 512:(dh + 1) * 512], start=False, stop=True),
                         reads=["onesb", "b2b"], writes=[f"psDn{i3}"])
                    wcol = ws[:, blk * 8 + tl, e:e + 1]
                    av = acc[:, tl, dh * 512:(dh + 1) * 512]
                    if e == 0:
                        P.op("dve", lambda e_, pd=pd, wcol=wcol, av=av: e_.tensor_scalar(out=av, in0=pd[:, :], scalar1=wcol, scalar2=None, op0=ALU.mult),
                             reads=[f"psDn{i3}", "ws"], writes=[f"acc{tl}"])
                    else:
                        P.op("dve", lambda e_, pd=pd, wcol=wcol, av=av: e_.scalar_tensor_tensor(out=av, in0=pd[:, :], scalar=wcol, in1=av, op0=ALU.mult, op1=ALU.add),
                             reads=[f"psDn{i3}", "ws", f"acc{tl}"], writes=[f"acc{tl}"])
                    nd += 1
        for tl in range(8):
            ab = accb[:, tl % 2, :]
            if tl % 2 == 0:
                P.op("act", lambda e_, tl=tl, ab=ab: e_.copy(out=ab, in_=acc[:, tl, :]), reads=[f"acc{tl}"], writes=[f"accb{tl % 2}"])
            else:
                P.op("pool", lambda e_, tl=tl, ab=ab: e_.tensor_copy(out=ab, in_=acc[:, tl, :]), reads=[f"acc{tl}"], writes=[f"accb{tl % 2}"])
            toks.append(P.dma(part_o[blk * 1024 + tl * 128:blk * 1024 + (tl + 1) * 128, :], ab,
                              reads=[f"accb{tl % 2}"], writes=[f"parto{blk}_{tl}"]))
    return P.finish(toks)


OFF_FOURIER = 1024
OFF_Z = 2048
OFF_XBC = 3072
OFF_DT = OFF_XBC + 2048
OFF_GATE = OFF_DT + 32
DEBUG = {}
_PROGS = {}


def _prog(name, fn):
    return fn()


def fT(v):
    return np.ascontiguousarray(np.asarray(v, np.float32).reshape(8, 128).T)


def pack_ts(lat, cx):
    out = np.zeros((NCORE, TS_T) + lat.shape[2:], lat.dtype)
    for b in range(B):
        for q in range(4):
            out[b * 4 + q, :2048] = lat[b, q * 2048:(q + 1) * 2048]
            out[b * 4 + q, 2048:2112] = cx[b, q * 64:(q + 1) * 64]
    return out


def unpack_ts(ts):
    lat = np.zeros((B, SEQ) + ts.shape[2:], ts.dtype)
    cx = np.zeros((B, CTX) + ts.shape[2:], ts.dtype)
    for b in range(B):
        for q in range(4):
            lat[b, q * 2048:(q + 1) * 2048] = ts[b * 4 + q, :2048]
            cx[b, q * 64:(q + 1) * 64] = ts[b * 4 + q, 2048:2112]
    return lat, cx


def pad_blocks(hT, segs, nb):
    out = np.zeros((hT.shape[0], nb, 260), hT.dtype)
    for bk in range(nb):
        s0 = bk * 256
        seg = [s_ for s_ in segs if s_[0] <= s0 < s_[1]][0]
        lo, hi = max(seg[0], s0 - 2), min(seg[1], s0 + 258)
        out[:, bk, (lo - (s0 - 2)):(hi - (s0 - 2))] = hT[:, lo:hi]
    return out


def mod_pair(mod, l, b, j):
    return np.stack([fT(mod[l, b, j]), fT(mod[l, 2, j])], -1)


def run_s1(lat_ts, mod, l, norm_w, parts=None, g2_layer=None, final_w=None):
    ident = np.eye(128, dtype=np.float32)
    fw = np.broadcast_to(np.asarray(final_w if final_w is not None else np.ones(D), np.float32), (128, D)).copy()
    maps = []
    for core in range(NCORE):
        b = core // 4
        m = {"xin": lat_ts[core], "nwT": fT(norm_w), "scT": mod_pair(mod, l, b, 1), "shT": mod_pair(mod, l, b, 0),
             "ident": ident, "fwb": fw}
        if parts is not None:
            m["part"] = parts[core]
            m["g2b"] = np.stack([np.broadcast_to(mod[g2_layer, b, 5], (128, D)), np.broadcast_to(mod[g2_layer, 2, 5], (128, D))]).astype(np.float32)
        maps.append(m)
    res = run_spmd(build_s1(parts is not None), maps)
    hT = np.stack([r["hT_o"] for r in res])
    if parts is not None:
        return hT, np.stack([r["lat_o"] for r in res]), np.stack([r["fin_o"] for r in res])
    return hT, lat_ts, None


def build_s2(gw, nlat):
    nc = build_s2a(gw, None, "a_")
    nc = build_s2b(gw, nc, "b_")
    nc = build_s2c(nlat, nc, "c_")
    return nc


def run_s2(hT_ts, l, inp):
    h_lat, h_ctx = unpack_ts(np.ascontiguousarray(hT_ts.transpose(0, 2, 1)))
    w_in = inp["w_in"][l]
    c6, m4 = ssd_consts()
    MAl, MCl = fourier_consts(64)
    MAc, MCc = fourier_consts(2)
    FB = fourier_fb()
    mapsa, mapsb, mapsc = [], [], []
    for core in range(NCORE):
        b, g = core // 4, core % 4
        lat_c = h_lat[b].reshape(128, 64, D).transpose(1, 0, 2).reshape(SEQ, D)
        ctx_c = h_ctx[b].reshape(128, 2, D).transpose(1, 0, 2).reshape(CTX, D)
        hTc = np.ascontiguousarray(np.concatenate([lat_c, ctx_c], 0).T)
        ma = {"hTc": hTc, "wp": np.ascontiguousarray(w_in[:, g * 256:(g + 1) * 256]),
              "pw": np.ascontiguousarray(inp["pool_w"][l, g]), "pscT": np.ascontiguousarray(inp["pool_scale"][l, g * 256:(g + 1) * 256].reshape(2, 128).T)}
        ma.update(pool_consts(POOL_WINDOWS[g], 64))
        mapsa.append(ma)
        mapsb.append({"hTc": hTc, "wf": np.ascontiguousarray(w_in[:, OFF_FOURIER + g * 256:OFF_FOURIER + (g + 1) * 256]),
                      "MA_l": MAl, "MA_c": MAc, "MC_l": MCl, "MC_c": MCc, "FB": FB})
        hT_cl = np.ascontiguousarray(np.concatenate([h_ctx[b], h_lat[b]], 0).T)
        xcols = np.r_[np.arange(g * 256, (g + 1) * 256), 1024 + np.arange(g * 128, (g + 1) * 128), 1536 + np.arange(g * 128, (g + 1) * 128)]
        dcols = np.r_[4 * g + np.arange(4), 16 + 4 * g + np.arange(4)]
        wdt = w_in[:, OFF_DT + dcols]
        wdt40 = np.zeros((D, 40), np.float32); wdt40[:, 0:8] = wdt; wdt40[:, 32:40] = wdt
        dtb = inp["dt_bias"][l].reshape(32)[dcols]
        dtb40 = np.zeros((40, 1), np.float32); dtb40[0:8, 0] = dtb; dtb40[32:40, 0] = dtb
        alog40 = np.zeros((40, 1), np.float32); alog40[32:40, 0] = inp["a_log"][l].reshape(32)[dcols]
        mapsc.append({"hTp": pad_blocks(hT_cl, [(0, CTX), (CTX, CTX + SEQ)], (CTX + SEQ) // 256),
                      "wxbc": np.ascontiguousarray(w_in[:, OFF_XBC + xcols]), "wz": np.ascontiguousarray(w_in[:, OFF_Z + g * 256:OFF_Z + (g + 1) * 256]),
                      "wdt40": wdt40, "convwT": np.ascontiguousarray(inp["conv_w"][l][:, xcols].reshape(5, 4, 128).transpose(2, 1, 0)),
                      "convbT": np.ascontiguousarray(inp["conv_b"][l][xcols].reshape(4, 128).T), "dtb40": dtb40, "alog40": alog40,
                      "dskb": np.broadcast_to(inp["d_skip"][l][None, 4 * g:4 * g + 4], (128, 4)).copy(),
                      "snwT": np.ascontiguousarray(inp["ssm_norm_w"][l][g * 256:(g + 1) * 256].reshape(2, 128).T), "c6": c6, "m4": m4})
    ra = rb = rc = run_spmd(build_s2(64, SEQ), [dict(list(a.items()) + list(b_.items()) + list(c_.items())) for a, b_, c_ in zip(mapsa, mapsb, mapsc)])
    y_lat = np.zeros((B, 3, SEQ, D), NBF); y_ctx = np.zeros((B, 3, CTX, D), NBF)
    ssq_lat = np.zeros((B, 4, SEQ), np.float32); ssq_ctx = np.zeros((B, 4, CTX), np.float32)
    for core in range(NCORE):
        b, g = core // 4, core % 4
        cs = slice(g * 256, (g + 1) * 256)
        ya = ra[core]["yaT_o"].T
        y_lat[b, 0, :, cs] = ya[:SEQ].reshape(64, 128, 256).transpose(1, 0, 2).reshape(SEQ, 256)
        y_ctx[b, 0, :, cs] = ya[SEQ:].reshape(2, 128, 256).transpose(1, 0, 2).reshape(CTX, 256)
        yb = rb[core]["ybT_o"].T
        y_lat[b, 1, :, cs] = yb[:SEQ]; y_ctx[b, 1, :, cs] = yb[SEQ:]
        yc = rc[core]["ycT_o"].T
        y_ctx[b, 2, :, cs] = yc[:CTX]; y_lat[b, 2, :, cs] = yc[CTX:]
        sq = rc[core]["ssq_o"][0]
        ssq_ctx[b, g] = sq[:CTX]; ssq_lat[b, g] = sq[CTX:]
    DEBUG[f"L{l}_y_lat"] = y_lat; DEBUG[f"L{l}_y_ctx"] = y_ctx; DEBUG[f"L{l}_ssq_lat"] = ssq_lat
    yT_ts = np.zeros((NCORE, 3, D, TS_T), NBF)
    ssq_ts = np.zeros((NCORE, 4, TS_T), np.float32)
    for core in range(NCORE):
        b, q = core // 4, core % 4
        for k in range(3):
            yT_ts[core, k, :, :2048] = y_lat[b, k, q * 2048:(q + 1) * 2048].T
            yT_ts[core, k, :, 2048:2112] = y_ctx[b, k, q * 64:(q + 1) * 64].T
        ssq_ts[core, :, :2048] = ssq_lat[b, :, q * 2048:(q + 1) * 2048]
        ssq_ts[core, :, 2048:2112] = ssq_ctx[b, :, q * 64:(q + 1) * 64]
    return yT_ts, ssq_ts


def run_s3(lat_ts, hT_ts, yT_ts, ssq_ts, mod, l, inp):
    ident = np.eye(128, dtype=np.float32)
    ones4 = np.ones((4, 128), np.float32)
    wg = np.ascontiguousarray(inp["w_in"][l][:, OFF_GATE:])
    rbb = np.broadcast_to(inp["router_b"][l], (128, 32)).astype(np.float32).copy()
    maps = []
    for core in range(NCORE):
        b = core // 4
        g1b = np.stack([np.broadcast_to(mod[l, b, 2], (128, D)), np.broadcast_to(mod[l, 2, 2], (128, D))]).astype(np.float32)
        maps.append({"lat": lat_ts[core], "hT": hT_ts[core], "yT": yT_ts[core], "ssq4": ssq_ts[core], "wg": wg, "wb": inp["w_branch"][l],
                     "wo": inp["w_out"][l], "g1b": g1b, "nwT": fT(inp["norm2_w"][l]), "scT": mod_pair(mod, l, b, 4), "shT": mod_pair(mod, l, b, 3),
                     "rw": inp["router_w"][l], "rbb": rbb, "ident": ident, "ones4": ones4})
    res = run_spmd(build_s3(), maps)
    return (np.stack([r["lat_o"] for r in res]), np.stack([r["h2T_o"] for r in res]), np.stack([r["wgt_o"] for r in res]))


def run_s4(h2T_ts, wgt_ts, l, inp, skip_ctx=False):
    NL = NCORE * 2048
    ntok = NL if skip_ctx else NTOK_ALL
    h2T_all = np.concatenate([h2T_ts[c][:, :2048] for c in range(NCORE)] + [h2T_ts[c][:, 2048:] for c in range(NCORE)], axis=1)
    wgt_all = np.concatenate([wgt_ts[c][:2048] for c in range(NCORE)] + [wgt_ts[c][2048:] for c in range(NCORE)], axis=0)
    h2T_all = np.ascontiguousarray(h2T_all[:, :ntok]); wgt_all = wgt_all[:ntok]
    maps = []
    for c in range(NCORE):
        es = slice(4 * c, 4 * c + 4)
        maps.append({"h2T": h2T_all, "wsel": np.ascontiguousarray(wgt_all[:, es].reshape(ntok // 128, 128, 4).transpose(1, 0, 2)),
                     "w1": np.ascontiguousarray(inp["moe_w1"][l, es]), "b1T": np.ascontiguousarray(inp["moe_b1"][l, es].reshape(4, 16, 128).transpose(2, 0, 1)),
                     "w2": np.ascontiguousarray(inp["moe_w2"][l, es]), "b2": np.ascontiguousarray(inp["moe_b2"][l, es][None])})
    res = run_spmd(build_s4(ntok), maps)
    parts = np.zeros((NCORE, NCORE, TS_T, D), NBF)
    for c in range(NCORE):
        p = res[c]["part_o"]
        for t in range(NCORE):
            parts[t, c, :2048] = p[t * 2048:(t + 1) * 2048]
            if not skip_ctx:
                parts[t, c, 2048:] = p[NL + t * 128:NL + (t + 1) * 128]
    return parts


def kernel(**inputs):
    inp = {k: np.asarray(v) for k, v in inputs.items()}
    mod = run_s0(inp["c"], inp["c_ctx"], inp["w_ada"], inp["b_ada"])
    DEBUG["mod"] = mod
    lat_ts = pack_ts(inp["x"].astype(np.float32), inp["ctx"].astype(np.float32))
    parts = None
    for l in range(2):
        hT_ts, lat_ts, _ = run_s1(lat_ts, mod, l, inp["norm1_w"][l], parts=parts, g2_layer=(l - 1 if parts is not None else None))
        DEBUG[f"L{l}_hT"] = hT_ts
        yT_ts, ssq_ts = run_s2(hT_ts, l, inp)
        lat_ts, h2T_ts, wgt_ts = run_s3(lat_ts, hT_ts, yT_ts, ssq_ts, mod, l, inp)
        DEBUG[f"L{l}_lat2"] = lat_ts; DEBUG[f"L{l}_h2T"] = h2T_ts; DEBUG[f"L{l}_wgt"] = wgt_ts
        parts = run_s4(h2T_ts, wgt_ts, l, inp, skip_ctx=(l == 1))
    _, lat_f, fin = run_s1(lat_ts, mod, 1, inp["norm1_w"][1], parts=parts, g2_layer=1, final_w=inp["final_norm_w"])
    DEBUG["lat_final"] = lat_f
    out, _ = unpack_ts(fin)
    return out.astype(np.float32)
```
